# Optimizing a Trainium2 kernel written in Bass

```python
import math
import jax
import jax.numpy as jnp
from jax import lax
import numpy as np

D_MODEL = 1024
BATCH = 2
SEQ = 8192
DEPTH = 2

GRID_W = 64
CTX_LEN = 256

N_MIXERS = 2
N_SSM_LAYERS = (DEPTH + 1) // 2
N_ATT_LAYERS = DEPTH // 2
N_MOD = 6

S5_GROUP = 16
S5_GROUPS = D_MODEL // S5_GROUP
S5_STATE = 64
S5_DT_MIN = 1e-3
S5_DT_MAX = 1e-1

MLA_HEADS = 8
MLA_NOPE = 128
MLA_ROPE = 64
MLA_V = 128
MLA_Q_LORA = 384
MLA_KV_LORA = 256
ROPE_BASE = 10000.0
Q_BLOCK = 128

PEER_HEADS = 8
PEER_NKEYS = 128
PEER_EXPERTS = PEER_NKEYS * PEER_NKEYS
PEER_DK = 128
PEER_TOPK = 16
PEER_TOKEN_BLOCK = 128

ALPHA = (2 * DEPTH) ** 0.25
BETA = (8 * DEPTH) ** -0.25
LN_EPS = 1e-5
RMS_EPS = 1e-6

kernel_name = 'hybrid_s5_mla_peer_diffusion_block'


def layer_norm(x, g, b):
    xf = x.astype(jnp.float32)
    mu = jnp.mean(xf, axis=-1, keepdims=True)
    var = jnp.mean(jnp.square(xf - mu), axis=-1, keepdims=True)
    return (xf - mu) * lax.rsqrt(var + LN_EPS) * g + b


def rms_norm(x, g):
    xf = x.astype(jnp.float32)
    return xf * lax.rsqrt(jnp.mean(jnp.square(xf), axis=-1, keepdims=True) + RMS_EPS) * g


def axial_rope_angles(n_tokens):
    rows = n_tokens // GRID_W
    r, col = jnp.meshgrid(jnp.arange(rows, dtype=jnp.float32),
                          jnp.arange(GRID_W, dtype=jnp.float32), indexing='ij')
    n_freq = MLA_ROPE // 4
    inv = ROPE_BASE ** (-jnp.arange(n_freq, dtype=jnp.float32) / n_freq)
    ang = jnp.concatenate([r.reshape(-1, 1) * inv, col.reshape(-1, 1) * inv], axis=-1)
    return jnp.cos(ang), jnp.sin(ang)


def apply_rope(x, cos, sin):
    half = x.shape[-1] // 2
    x1, x2 = x[..., :half], x[..., half:]
    return jnp.concatenate([x1 * cos - x2 * sin, x2 * cos + x1 * sin], axis=-1)


def s5_discretize(a_re, a_im, log_dt):
    a_re = a_re.astype(jnp.float32)
    a_im = a_im.astype(jnp.float32)
    dt = jnp.exp(log_dt.astype(jnp.float32))[:, None]
    mag = jnp.exp(a_re * dt)
    ab_re = mag * jnp.cos(a_im * dt)
    ab_im = mag * jnp.sin(a_im * dt)
    nr, ni = ab_re - 1.0, ab_im
    den = a_re * a_re + a_im * a_im
    f_re = (nr * a_re + ni * a_im) / den
    f_im = (ni * a_re - nr * a_im) / den
    return ab_re, ab_im, f_re, f_im


def _complex_affine_combine(e1, e2):
    a1r, a1i, b1r, b1i = e1
    a2r, a2i, b2r, b2i = e2
    return (a2r * a1r - a2i * a1i,
            a2r * a1i + a2i * a1r,
            a2r * b1r - a2i * b1i + b2r,
            a2r * b1i + a2i * b1r + b2i)


def complex_linear_scan(ab_re, ab_im, bu_re, bu_im, h0, reverse):
    if h0 is not None:
        first = -1 if reverse else 0
        h0_re, h0_im = h0
        bu_re = bu_re.at[first].add(ab_re * h0_re - ab_im * h0_im)
        bu_im = bu_im.at[first].add(ab_re * h0_im + ab_im * h0_re)
    n = bu_re.shape[0]
    a_re = jnp.broadcast_to(ab_re, (n, 1) + ab_re.shape)
    a_im = jnp.broadcast_to(ab_im, (n, 1) + ab_im.shape)
    _, _, h_re, h_im = lax.associative_scan(_complex_affine_combine, (a_re, a_im, bu_re, bu_im),
                                            reverse=reverse, axis=0)
    return h_re, h_im


def s5_direction(u_lat, u_ctx, a_re, a_im, log_dt, b_re, b_im, c_re, c_im, reverse, need_ctx_out):
    ab_re, ab_im, f_re, f_im = s5_discretize(a_re, a_im, log_dt)
    b_re = b_re.astype(jnp.float32)
    b_im = b_im.astype(jnp.float32)
    c_re = c_re.astype(jnp.float32)
    c_im = c_im.astype(jnp.float32)

    def drive(u):
        bu_re = jnp.einsum('lbgc,gpc->lbgp', u, b_re)
        bu_im = jnp.einsum('lbgc,gpc->lbgp', u, b_im)
        return f_re * bu_re - f_im * bu_im, f_re * bu_im + f_im * bu_re

    def readout(h_re, h_im):
        return (jnp.einsum('lbgp,gcp->lbgc', h_re, c_re)
                - jnp.einsum('lbgp,gcp->lbgc', h_im, c_im))

    hc_re, hc_im = complex_linear_scan(ab_re, ab_im, *drive(u_ctx), None, reverse)
    last = 0 if reverse else -1
    hl_re, hl_im = complex_linear_scan(ab_re, ab_im, *drive(u_lat),
                                       (hc_re[last], hc_im[last]), reverse)
    y_ctx = readout(hc_re, hc_im) if need_ctx_out else None
    return readout(hl_re, hl_im), y_ctx


def s5_output(y, w_glu, w_o):
    y = jax.nn.gelu(y, approximate=False)
    return (y * jax.nn.sigmoid(y @ w_glu)) @ w_o


def s5_mixer(h_lat, h_ctx, a_re, a_im, log_dt, b_re, b_im, c_re, c_im, d, w_glu, w_o, need_ctx_out):
    def to_groups(h):
        bsz, n, _ = h.shape
        return h.astype(jnp.float32).transpose(1, 0, 2).reshape(n, bsz, S5_GROUPS, S5_GROUP)

    def from_groups(y):
        n, bsz = y.shape[:2]
        return y.reshape(n, bsz, D_MODEL).transpose(1, 0, 2)

    u_lat, u_ctx = to_groups(h_lat), to_groups(h_ctx)
    yf_l, yf_c = s5_direction(u_lat, u_ctx, a_re[0], a_im[0], log_dt[0], b_re[0], b_im[0],
                              c_re[0], c_im[0], False, need_ctx_out)
    yb_l, yb_c = s5_direction(u_lat, u_ctx, a_re[1], a_im[1], log_dt[1], b_re[1], b_im[1],
                              c_re[1], c_im[1], True, need_ctx_out)
    o_lat = s5_output(from_groups(yf_l + yb_l) + d * h_lat, w_glu, w_o)
    o_ctx = None
    if need_ctx_out:
        o_ctx = s5_output(from_groups(yf_c + yb_c) + d * h_ctx, w_glu, w_o)
    return o_lat, o_ctx


def mla_queries(h, w_dq, q_norm, w_uq, rope):
    bsz, n, _ = h.shape
    q = (rms_norm(h @ w_dq, q_norm) @ w_uq).reshape(bsz, n, MLA_HEADS, MLA_NOPE + MLA_ROPE)
    q_nope, q_rope = q[..., :MLA_NOPE], q[..., MLA_NOPE:]
    if rope is not None:
        cos, sin = rope
        q_rope = apply_rope(q_rope, cos[None, :, None, :], sin[None, :, None, :])
    return q_nope, q_rope


def mla_keys_values(h, w_dkv, kv_norm, w_ukv, rope):
    bsz, n, _ = h.shape
    kv = h @ w_dkv
    c_kv = rms_norm(kv[..., :MLA_KV_LORA], kv_norm)
    k_rope = kv[..., MLA_KV_LORA:]
    if rope is not None:
        cos, sin = rope
        k_rope = apply_rope(k_rope, cos[None], sin[None])
    kvu = (c_kv @ w_ukv).reshape(bsz, n, MLA_HEADS, MLA_NOPE + MLA_V)
    return kvu[..., :MLA_NOPE], k_rope, kvu[..., MLA_NOPE:]


def mla_attend(q_nope, q_rope, k_nope, k_rope, v):
    scale = (MLA_NOPE + MLA_ROPE) ** -0.5
    s = (jnp.einsum('bqhd,bkhd->bhqk', q_nope, k_nope)
         + jnp.einsum('bqhr,bkr->bhqk', q_rope, k_rope))
    p = jax.nn.softmax(s.astype(jnp.float32) * scale, axis=-1).astype(v.dtype)
    return jnp.einsum('bhqk,bkhd->bqhd', p, v)


def mla_attend_blocked(q_nope, q_rope, k_nope, k_rope, v):
    bsz, n = q_nope.shape[:2]
    nb = n // Q_BLOCK
    qn = q_nope.reshape(bsz, nb, Q_BLOCK, MLA_HEADS, MLA_NOPE).transpose(1, 0, 2, 3, 4)
    qr = q_rope.reshape(bsz, nb, Q_BLOCK, MLA_HEADS, MLA_ROPE).transpose(1, 0, 2, 3, 4)
    out = lax.map(lambda qs: mla_attend(qs[0], qs[1], k_nope, k_rope, v), (qn, qr))
    return out.transpose(1, 0, 2, 3, 4).reshape(bsz, n, MLA_HEADS, MLA_V)


def mla_mixer(h_lat, h_ctx, w_dq, q_norm, w_uq, w_dkv, kv_norm, w_ukv, w_o, need_ctx_out):
    bsz, n_lat, _ = h_lat.shape
    rope = axial_rope_angles(n_lat)
    kn_c, kr_c, v_c = mla_keys_values(h_ctx, w_dkv, kv_norm, w_ukv, None)
    kn_l, kr_l, v_l = mla_keys_values(h_lat, w_dkv, kv_norm, w_ukv, rope)
    qn_l, qr_l = mla_queries(h_lat, w_dq, q_norm, w_uq, rope)
    k_nope = jnp.concatenate([kn_l, kn_c], axis=1)
    k_rope = jnp.concatenate([kr_l, kr_c], axis=1)
    v = jnp.concatenate([v_l, v_c], axis=1)
    o_lat = mla_attend_blocked(qn_l, qr_l, k_nope, k_rope, v).reshape(bsz, n_lat, MLA_HEADS * MLA_V) @ w_o
    o_ctx = None
    if need_ctx_out:
        qn_c, qr_c = mla_queries(h_ctx, w_dq, q_norm, w_uq, None)
        o_ctx = mla_attend(qn_c, qr_c, kn_c, kr_c, v_c).reshape(bsz, h_ctx.shape[1], MLA_HEADS * MLA_V) @ w_o
    return o_lat, o_ctx


def peer_route(t, w_q, keys):
    n_tok = t.shape[0]
    q = (t @ w_q).reshape(n_tok, PEER_HEADS, 2, PEER_DK // 2)
    s = jnp.einsum('thsd,hsnd->thsn', q, keys).astype(jnp.float32)
    sv, si = lax.top_k(s, PEER_TOPK)
    cand = sv[:, :, 0, :, None] + sv[:, :, 1, None, :]
    cv, ci = lax.top_k(cand.reshape(n_tok, PEER_HEADS, PEER_TOPK * PEER_TOPK), PEER_TOPK)
    i1 = jnp.take_along_axis(si[:, :, 0], ci // PEER_TOPK, axis=-1)
    i2 = jnp.take_along_axis(si[:, :, 1], ci % PEER_TOPK, axis=-1)
    e = i1 * PEER_NKEYS + i2
    g = jax.nn.softmax(cv, axis=-1)
    return e.reshape(n_tok, -1), g.reshape(n_tok, -1)


def peer_channel(h, w_q, keys, u_tab, v_tab):
    bsz, n, d = h.shape
    t = h.reshape(bsz * n, d)
    idx, gate = peer_route(t, w_q, keys)
    nb = t.shape[0] // PEER_TOKEN_BLOCK

    def block(args):
        hb, ib, gb = args
        act = jax.nn.gelu(jnp.einsum('td,tkd->tk', hb, u_tab[ib]), approximate=False) * gb
        return jnp.einsum('tk,tkd->td', act, v_tab[ib])

    out = lax.map(block, (t.reshape(nb, PEER_TOKEN_BLOCK, d),
                          idx.reshape(nb, PEER_TOKEN_BLOCK, -1),
                          gate.reshape(nb, PEER_TOKEN_BLOCK, -1)))
    return out.reshape(bsz, n, d)


def setup_inputs(seed: int = 0) -> dict:
    key = jax.random.key(seed)
    ks = jax.random.split(key, 32)
    f32 = jnp.float32

    def nrm(k, shape, std):
        return jax.random.normal(k, shape, f32) * std

    D = D_MODEL
    NS, NA = N_SSM_LAYERS, N_ATT_LAYERS
    G, P, GC = S5_GROUPS, S5_STATE, S5_GROUP
    n_idx = jnp.arange(P, dtype=f32)
    return {
        'x': nrm(ks[0], (BATCH, SEQ, D), 1.0),
        'c': nrm(ks[1], (BATCH, D), 1.0),
        'ctx': nrm(ks[2], (BATCH, CTX_LEN, D), 1.0),
        'c_ctx': nrm(ks[3], (D,), 1.0),
        'ada_w': nrm(ks[4], (DEPTH, D, N_MOD * D), 0.5 * D ** -0.5),
        'ada_b': nrm(ks[5], (DEPTH, N_MOD * D), 0.02),
        'ln_g': 1.0 + nrm(ks[6], (DEPTH, 2, D), 0.02),
        'ln_b': nrm(ks[7], (DEPTH, 2, D), 0.02),
        's5_a_re': -0.5 + nrm(ks[8], (NS, 2, G, P), 0.01),
        's5_a_im': math.pi * n_idx + nrm(ks[9], (NS, 2, G, P), 0.01),
        's5_log_dt': jax.random.uniform(ks[10], (NS, 2, G), f32, math.log(S5_DT_MIN), math.log(S5_DT_MAX)),
        's5_b_re': nrm(ks[11], (NS, 2, G, P, GC), (2 * GC) ** -0.5),
        's5_b_im': nrm(ks[12], (NS, 2, G, P, GC), (2 * GC) ** -0.5),
        's5_c_re': nrm(ks[13], (NS, 2, G, GC, P), P ** -0.5),
        's5_c_im': nrm(ks[14], (NS, 2, G, GC, P), P ** -0.5),
        's5_d': nrm(ks[15], (NS, D), 1.0),
        's5_w_glu': nrm(ks[16], (NS, D, D), D ** -0.5),
        's5_w_o': nrm(ks[17], (NS, D, D), BETA * D ** -0.5),
        'mla_w_dq': nrm(ks[18], (NA, D, MLA_Q_LORA), D ** -0.5),
        'mla_q_norm': 1.0 + nrm(ks[19], (NA, MLA_Q_LORA), 0.02),
        'mla_w_uq': nrm(ks[20], (NA, MLA_Q_LORA, MLA_HEADS * (MLA_NOPE + MLA_ROPE)), MLA_Q_LORA ** -0.5),
        'mla_w_dkv': nrm(ks[21], (NA, D, MLA_KV_LORA + MLA_ROPE), D ** -0.5),
        'mla_kv_norm': 1.0 + nrm(ks[22], (NA, MLA_KV_LORA), 0.02),
        'mla_w_ukv': nrm(ks[23], (NA, MLA_KV_LORA, MLA_HEADS * (MLA_NOPE + MLA_V)), MLA_KV_LORA ** -0.5),
        'mla_w_o': nrm(ks[24], (NA, MLA_HEADS * MLA_V, D), BETA * (MLA_HEADS * MLA_V) ** -0.5),
        'peer_w_q': nrm(ks[25], (DEPTH, D, PEER_HEADS * PEER_DK), D ** -0.5),
        'peer_keys': nrm(ks[26], (DEPTH, PEER_HEADS, 2, PEER_NKEYS, PEER_DK // 2), (PEER_DK // 2) ** -0.5),
        'peer_u': nrm(ks[27], (DEPTH, PEER_EXPERTS, D), D ** -0.5),
        'peer_v': nrm(ks[28], (DEPTH, PEER_EXPERTS, D), BETA),
    }


def reference(x, c, ctx, c_ctx, ada_w, ada_b, ln_g, ln_b,
              s5_a_re, s5_a_im, s5_log_dt, s5_b_re, s5_b_im, s5_c_re, s5_c_im, s5_d, s5_w_glu, s5_w_o,
              mla_w_dq, mla_q_norm, mla_w_uq, mla_w_dkv, mla_kv_norm, mla_w_ukv, mla_w_o,
              peer_w_q, peer_keys, peer_u, peer_v):
    bsz, n_lat, d = x.shape
    h_lat, h_ctx = x, ctx
    sc = jax.nn.silu(c)
    scc = jax.nn.silu(c_ctx)
    for i in range(DEPTH):
        last = i == DEPTH - 1
        mod_l = (sc @ ada_w[i] + ada_b[i]).reshape(bsz, N_MOD, d).transpose(1, 0, 2)[:, :, None, :]
        mod_c = (scc @ ada_w[i] + ada_b[i]).reshape(N_MOD, 1, 1, d)
        m_l = h_lat * (1.0 + mod_l[1]) + mod_l[0]
        m_c = h_ctx * (1.0 + mod_c[1]) + mod_c[0]
        j = i // N_MIXERS
        if i % N_MIXERS == 0:
            o_l, o_c = s5_mixer(m_l, m_c, s5_a_re[j], s5_a_im[j], s5_log_dt[j], s5_b_re[j], s5_b_im[j],
                                s5_c_re[j], s5_c_im[j], s5_d[j], s5_w_glu[j], s5_w_o[j], not last)
        else:
            o_l, o_c = mla_mixer(m_l, m_c, mla_w_dq[j], mla_q_norm[j], mla_w_uq[j], mla_w_dkv[j],
                                 mla_kv_norm[j], mla_w_ukv[j], mla_w_o[j], not last)
        h_lat = layer_norm(ALPHA * h_lat + mod_l[2] * o_l, ln_g[i, 0], ln_b[i, 0])
        f_l = peer_channel(h_lat * (1.0 + mod_l[4]) + mod_l[3], peer_w_q[i], peer_keys[i], peer_u[i], peer_v[i])
        h_lat = layer_norm(ALPHA * h_lat + mod_l[5] * f_l, ln_g[i, 1], ln_b[i, 1])
        if not last:
            h_ctx = layer_norm(ALPHA * h_ctx + mod_c[2] * o_c, ln_g[i, 0], ln_b[i, 0])
            f_c = peer_channel(h_ctx * (1.0 + mod_c[4]) + mod_c[3], peer_w_q[i], peer_keys[i], peer_u[i], peer_v[i])
            h_ctx = layer_norm(ALPHA * h_ctx + mod_c[5] * f_c, ln_g[i, 1], ln_b[i, 1])
    return h_lat.astype(x.dtype)
```

```python
import math
import numpy as np
import ml_dtypes
from contextlib import ExitStack
import concourse.bass as bass
import concourse.mybir as mybir
from concourse.bass_utils import run_bass_kernel_spmd

F32 = mybir.dt.float32
BF16 = mybir.dt.bfloat16
U32 = mybir.dt.uint32
I32 = mybir.dt.int32
AF = mybir.ActivationFunctionType
ALU = mybir.AluOpType
AX = mybir.AxisListType

NCORES = 8
D = 1024
ALPHA = (2 * 2) ** 0.25
LN_EPS = 1e-5
RMS_EPS = 1e-6
NT = 17
TPC = NT * 128


class Prog:
    def __init__(self):
        self.nc = bass.Bass("TRN2", target_bir_lowering=False)
        self.es = ExitStack()
        nc = self.nc
        self.eng = {'pe': nc.tensor, 'dve': nc.vector, 'act': nc.scalar, 'pool': nc.gpsimd, 'sp': nc.sync}
        self.sems, self.cnt = {}, {}
        self.seen = {e: {} for e in self.eng}
        self.lastw, self.readers = {}, {}
        for e in ('pe', 'dve', 'act', 'pool'):
            self._sem('E_' + e)
        self.npsum = 0

    def _sem(self, key):
        if key not in self.sems:
            self.sems[key] = self.es.enter_context(self.nc.semaphore(key))
            self.cnt[key] = 0
        return self.sems[key]

    def din(self, name, shape, dt=F32):
        return self.nc.dram_tensor(name, list(shape), dt, kind="ExternalInput").ap()

    def dout(self, name, shape, dt=F32):
        return self.nc.dram_tensor(name, list(shape), dt, kind="ExternalOutput").ap()

    def sb(self, name, shape, dt=F32):
        return self.es.enter_context(self.nc.sbuf_tensor("sb_" + name, list(shape), dt))

    def ps(self, name, shape, dt=F32):
        return self.es.enter_context(self.nc.psum_tensor(name, list(shape), dt))

    def _deps(self, reads, writes):
        deps = {}

        def add(k, v):
            if not k.startswith('E_'):
                v = max(v, self.cnt[k])
            if deps.get(k, 0) < v:
                deps[k] = v
        for b in reads:
            if b in self.lastw:
                add(*self.lastw[b])
        for b in writes:
            if b in self.lastw:
                add(*self.lastw[b])
            for k, v in self.readers.get(b, {}).items():
                add(k, v)
        return deps

    def _wait(self, e, deps, skip=None):
        for k, v in deps.items():
            if k == skip or self.seen[e].get(k, 0) >= v:
                continue
            self.eng[e].wait_ge(self.sems[k], v)
            self.seen[e][k] = v

    def _commit(self, k, v, reads, writes):
        for b in writes:
            self.lastw[b] = (k, v)
            self.readers[b] = {}
        for b in reads:
            r = self.readers.setdefault(b, {})
            if r.get(k, 0) < v:
                r[k] = v

    def op(self, e, fn, reads=(), writes=()):
        key = 'E_' + e
        self._wait(e, self._deps(reads, writes), skip=key if e == 'pe' else None)
        ins = fn(self.eng[e])
        self.cnt[key] += 1
        ins.then_inc(self.sems[key], 1)
        self._commit(key, self.cnt[key], reads, writes)

    def dma(self, q, semkey, fn, reads=(), writes=()):
        self._sem(semkey)
        self._wait(q, self._deps(reads, writes))
        ins = fn(self.eng[q])
        self.cnt[semkey] += 16
        ins.then_inc(self.sems[semkey], 16)
        self._commit(semkey, self.cnt[semkey], reads, writes)

    def finish(self):
        for k, v in self.cnt.items():
            if v > 0 and self.seen['sp'].get(k, 0) < v:
                self.nc.sync.wait_ge(self.sems[k], v)

    def run(self, in_maps):
        res = run_bass_kernel_spmd(self.nc, in_maps, core_ids=list(range(NCORES)))
        return res.results

    def load(self, semkey, dst, src, wid, rid=None, q='sp'):
        self.dma(q, semkey, lambda e: e.dma_start(out=dst, in_=src), reads=(rid,) if rid else (), writes=(wid,))

    def store(self, semkey, dst, src, rid, wid=None, q='sp'):
        self.dma(q, semkey, lambda e: e.dma_start(out=dst, in_=src), reads=(rid,), writes=(wid,) if wid else ())

    def tt(self, e, out, a, b, op, r, w):
        self.op(e, lambda g: g.tensor_tensor(out=out, in0=a, in1=b, op=op), reads=r, writes=w)

    def ts(self, e, out, a, s1, s2, op0, op1, r, w):
        if op1 is None:
            self.op(e, lambda g: g.tensor_scalar(out=out, in0=a, scalar1=s1, scalar2=None, op0=op0), reads=r, writes=w)
        else:
            self.op(e, lambda g: g.tensor_scalar(out=out, in0=a, scalar1=s1, scalar2=s2, op0=op0, op1=op1), reads=r, writes=w)

    def stt(self, out, a, s, b, op0, op1, r, w):
        self.op('dve', lambda g: g.scalar_tensor_tensor(out=out, in0=a, scalar=s, in1=b, op0=op0, op1=op1), reads=r, writes=w)

    def actf(self, out, a, func, r, w, bias=None, scale=None, accum=None):
        kw = {}
        if bias is not None:
            kw['bias'] = bias
        if scale is not None:
            kw['scale'] = scale
        if accum is not None:
            kw['accum_out'] = accum
        self.op('act', lambda g: g.activation(out=out, in_=a, func=func, **kw), reads=r, writes=w)

    def cp(self, e, out, a, r, w):
        if e == 'act':
            self.actf(out, a, AF.Copy, r, w)
        else:
            self.op(e, lambda g: g.tensor_copy(out=out, in_=a), reads=r, writes=w)

    def mm(self, out, lhsT, rhs, start, stop, r, w):
        self.op('pe', lambda g: g.matmul(out, lhsT, rhs, start=start, stop=stop), reads=r, writes=w)

    def tr(self, out, in_, ident, r, w):
        self.op('pe', lambda g: g.transpose(out, in_, ident), reads=r, writes=w)


class PsumRing:
    def __init__(self, P, banks, dt=F32):
        if not hasattr(P, "banks"):
            P.banks = {}
        for i in banks:
            if i not in P.banks:
                P.banks[i] = P.ps(f"psb{i}", [128, 512], dt)
        self.b = list(banks)
        self.P = P
        self.i = 0

    def get(self):
        j = self.b[self.i]
        self.i = (self.i + 1) % len(self.b)
        return self.P.banks[j], f"psb{j}"


def build_mods():
    P = Prog()
    cT = P.din("cT", [128, 8, 3])
    aw = P.din("aw", [2, 1024, 768])
    ab = P.din("ab", [2, 3, 768])
    out = P.dout("mods", [2, 3, 768])
    s = P.sb("s", [128, 8, 3])
    w = [P.sb(f"w{i}", [128, 8, 768]) for i in range(2)]
    bsb = P.sb("bsb", [3, 2, 768])
    res = P.sb("res", [3, 2, 768])
    ring = PsumRing(P, range(4))
    P.load("ld_c", s[:], cT, "s")
    for i in range(2):
        P.load(f"ld_w{i}", w[i][:], aw[i].rearrange("(kc p) n -> p kc n", p=128), f"w{i}")
        P.load("ld_b", bsb[:, i, :], ab[i], "bsb")
    P.actf(s[:], s[:], AF.Silu, ["s"], ["s"])
    for i in range(2):
        for nb in range(2):
            pt, pid = ring.get()
            for kc in range(8):
                P.mm(pt[0:3, 0:384], s[:, kc, :], w[i][:, kc, nb * 384:(nb + 1) * 384], kc == 0, kc == 7,
                     ["s", f"w{i}"], [pid])
            P.tt('dve', res[:, i, nb * 384:(nb + 1) * 384], pt[0:3, 0:384], bsb[:, i, nb * 384:(nb + 1) * 384],
                 ALU.add, [pid, "bsb"], ["res"])
    for i in range(2):
        P.store("st", out[i], res[:, i, :], "res")
    P.finish()
    return P


def run_mods(c, c_ctx, ada_w, ada_b):
    P = build_mods()
    vec = np.stack([c[0], c[1], c_ctx], axis=1)
    cT = np.ascontiguousarray(vec.reshape(8, 128, 3).transpose(1, 0, 2))
    maps = []
    for k in range(NCORES):
        sl = slice(768 * k, 768 * (k + 1))
        maps.append({"cT": cT, "aw": np.ascontiguousarray(ada_w[:, :, sl]),
                     "ab": np.ascontiguousarray(np.broadcast_to(ada_b[:, None, sl], (2, 3, 768)))})
    res = P.run(maps)
    return np.concatenate([r["mods"] for r in res], axis=2)


SEQ_T = 256 + 8192
CH = 512
TWO_PI = 2.0 * math.pi
MAGIC = 12582912.0


def sincos(P, th, n, tmp, sin_out, cos_out, ids):
    nc_ = P
    for which, outt in ((0, sin_out), (1, cos_out)):
        x, u, k, y, m = (tmp[:, j, :n] for j in range(5))
        r = list(ids)
        if which == 0:
            P.cp('dve', x, th, r, r)
        else:
            P.ts('dve', x, th, math.pi / 2, None, ALU.add, None, r, r)
        P.ts('dve', u, x, 1.0 / TWO_PI, None, ALU.mult, None, r, r)
        P.ts('dve', k, u, MAGIC, None, ALU.add, None, r, r)
        P.ts('dve', k, k, MAGIC, None, ALU.subtract, None, r, r)
        P.stt(y, k, -TWO_PI, x, ALU.mult, ALU.add, r, r)
        P.ts('dve', m, y, math.pi, None, ALU.is_gt, None, r, r)
        P.stt(y, m, -TWO_PI, y, ALU.mult, ALU.add, r, r)
        P.ts('dve', m, y, -math.pi, None, ALU.is_lt, None, r, r)
        P.stt(y, m, TWO_PI, y, ALU.mult, ALU.add, r, r)
        P.ts('dve', y, y, math.pi, -math.pi, ALU.min, ALU.max, r, r)
        P.actf(outt, y, AF.Sin, r, r)


def build_s5():
    P = Prog()
    xf = P.din("xf", [128, 2, SEQ_T])
    xb = P.din("xb", [128, 2, SEQ_T])
    msc = P.din("msc", [128, 3])
    msh = P.din("msh", [128, 3])
    a_re_d = P.din("a_re", [128, 8])
    a_im_d = P.din("a_im", [128, 8])
    ldt_d = P.din("ldt", [128, 8])
    bre_d = P.din("bre", [128, 8, 16])
    bim_d = P.din("bim", [128, 8, 16])
    cre_d = P.din("cre", [128, 8, 16])
    cim_d = P.din("cim", [128, 8, 16])
    dvec_d = P.din("dvec", [128, 1])
    ident_d = P.din("ident", [128, 128])
    yf = P.dout("yf", [128, 2, SEQ_T])
    yb = P.dout("yb", [128, 2, SEQ_T])

    ident = P.sb("ident", [128, 128])
    sc1 = P.sb("sc1", [128, 3])
    sh = P.sb("sh", [128, 3])
    dv = P.sb("dv", [128, 1])
    are = P.sb("are", [128, 8]); aim = P.sb("aim", [128, 8]); ldt = P.sb("ldt", [128, 8])
    bre = P.sb("bre", [128, 8, 16]); bim = P.sb("bim", [128, 8, 16])
    cre = P.sb("cre", [128, 8, 16]); cim = P.sb("cim", [128, 8, 16])
    prm = P.sb("prm", [128, 16, 8])
    tmp5 = P.sb("tmp5", [128, 5, 8])
    Wre = P.sb("Wre", [128, 8, 128]); Wim = P.sb("Wim", [128, 8, 128])
    Cre = P.sb("Cre", [128, 8, 128]); nCre = P.sb("nCre", [128, 8, 128]); nCim = P.sb("nCim", [128, 8, 128])
    bfull = P.sb("bfull", [128, 2, 128])
    tb = P.sb("tb", [128, 2, 16])
    ctab = P.sb("ctab", [128, 8, CH]); stab = P.sb("stab", [128, 8, CH]); rB = P.sb("rB", [128, 8, CH])
    ones = P.sb("ones", [128, CH])
    En = P.sb("En", [128, 8, 2, 2])
    cur = P.sb("cur", [128, 4])
    st = P.sb("st", [128, 4, 2])
    ring = PsumRing(P, range(2, 8))
    yring = PsumRing(P, range(0, 2))

    for dst, src, nm in ((ident, ident_d, "ident"), (sc1, msc, "sc1"), (sh, msh, "sh"), (dv, dvec_d, "dv"),
                         (are, a_re_d, "are"), (aim, a_im_d, "aim"), (ldt, ldt_d, "ldt"), (bre, bre_d, "bre"),
                         (bim, bim_d, "bim"), (cre, cre_d, "cre"), (cim, cim_d, "cim")):
        P.load("ld_par", dst[:], src, nm)
    R = ["prm"]
    P.ts('dve', sc1[:], sc1[:], 1.0, None, ALU.add, None, ["sc1"], ["sc1"])
    P.op('dve', lambda g: g.memset(ones[:], 1.0), writes=["ones"])
    dt_, mag, th, sn, cs, abr, abi, den, fre, fim, t0, t1 = (prm[:, j, :] for j in range(12))
    P.actf(dt_, ldt[:], AF.Exp, ["ldt"], R)
    P.tt('dve', t0, are[:], dt_, ALU.mult, ["are"] + R, R)
    P.actf(mag, t0, AF.Exp, R, R)
    P.tt('dve', th, aim[:], dt_, ALU.mult, ["aim"] + R, R)
    sincos(P, th, 8, tmp5, sn, cs, R + ["tmp5"])
    P.tt('dve', abr, mag, cs, ALU.mult, R, R)
    P.tt('dve', abi, mag, sn, ALU.mult, R, R)
    P.tt('dve', den, are[:], are[:], ALU.mult, ["are"] + R, R)
    P.tt('dve', t0, aim[:], aim[:], ALU.mult, ["aim"] + R, R)
    P.tt('dve', den, den, t0, ALU.add, R, R)
    P.op('dve', lambda g: g.reciprocal(out=den, in_=den), reads=R, writes=R)
    P.ts('dve', t0, abr, -1.0, None, ALU.add, None, R, R)
    P.tt('dve', fre, t0, are[:], ALU.mult, ["are"] + R, R)
    P.tt('dve', t1, abi, aim[:], ALU.mult, ["aim"] + R, R)
    P.tt('dve', fre, fre, t1, ALU.add, R, R)
    P.tt('dve', fre, fre, den, ALU.mult, R, R)
    P.tt('dve', fim, abi, are[:], ALU.mult, ["are"] + R, R)
    P.tt('dve', t1, t0, aim[:], ALU.mult, ["aim"] + R, R)
    P.tt('dve', fim, fim, t1, ALU.subtract, R, R)
    P.tt('dve', fim, fim, den, ALU.mult, R, R)
    for j in range(8):
        pair = j % 4
        for which, Wdst in ((0, Wre), (1, Wim)):
            P.op('dve', lambda g: g.memset(bfull[:, which, :], 0.0), writes=["bfull"])
            if which == 0:
                P.ts('dve', tb[:, 0, :], bim[:, j, :], fim[:, j:j + 1], None, ALU.mult, None, ["bim"] + R, ["tb"])
                src2 = bre
                op1 = ALU.subtract
            else:
                P.ts('dve', tb[:, 0, :], bre[:, j, :], fim[:, j:j + 1], None, ALU.mult, None, ["bre"] + R, ["tb"])
                src2 = bim
                op1 = ALU.add
            P.stt(tb[:, 1, :], src2[:, j, :], fre[:, j:j + 1], tb[:, 0, :], ALU.mult, op1, ["bre", "bim", "tb"] + R, ["tb"])
            for g2 in range(2):
                c0 = 32 * pair + 16 * g2
                P.cp('dve', bfull[64 * g2:64 * g2 + 64, which, c0:c0 + 16], tb[64 * g2:64 * g2 + 64, 1, :], ["tb"], ["bfull"])
            pt, pid = ring.get()
            P.tr(pt[:, 0:128], bfull[:, which, :], ident[:], ["bfull", "ident"], [pid])
            P.cp('act', Wdst[:, j, :], pt[:, 0:128], [pid], ["W"])
        for srcc, dsts in ((cre, (Cre, nCre)), (cim, (None, nCim))):
            for dd, sgn in zip(dsts, (1.0, -1.0)):
                if dd is None:
                    continue
                P.op('dve', lambda g: g.memset(dd[:, j, :], 0.0), writes=["W"])
                for g2 in range(2):
                    c0 = 32 * pair + 16 * g2
                    P.ts('dve', dd[64 * g2:64 * g2 + 64, j, c0:c0 + 16], srcc[64 * g2:64 * g2 + 64, j, :], sgn, None,
                         ALU.mult, None, ["cre", "cim"], ["W"])
        P.op('dve', lambda g: g.memset(ctab[:, j, 0:1], 1.0), writes=["tab"])
        P.op('dve', lambda g: g.memset(stab[:, j, 0:1], 0.0), writes=["tab"])
        P.cp('dve', cur[:, 0:1], cs[:, j:j + 1], R, ["cur"])
        P.cp('dve', cur[:, 1:2], sn[:, j:j + 1], R, ["cur"])
        n = 1
        while n <= CH:
            if n in (256, 512):
                wi = 0 if n == 256 else 1
                P.cp('dve', En[:, j, wi, :], cur[:, 0:2], ["cur"], ["En"])
            if n == CH:
                break
            cn, snn = cur[:, 0:1], cur[:, 1:2]
            T = ["tab", "cur", "tmpt"]
            P.ts('dve', rB[:, j, 0:n], stab[:, j, 0:n], snn, None, ALU.mult, None, T, ["tmpt"])
            P.stt(ctab[:, j, n:2 * n], ctab[:, j, 0:n], cn, rB[:, j, 0:n], ALU.mult, ALU.subtract, T, ["tab"])
            P.ts('dve', rB[:, j, 0:n], ctab[:, j, 0:n], snn, None, ALU.mult, None, T, ["tmpt"])
            P.stt(stab[:, j, n:2 * n], stab[:, j, 0:n], cn, rB[:, j, 0:n], ALU.mult, ALU.add, T, ["tab"])
            P.tt('dve', cur[:, 2:3], cn, cn, ALU.mult, ["cur"], ["cur"])
            P.tt('dve', cur[:, 3:4], snn, snn, ALU.mult, ["cur"], ["cur"])
            P.tt('dve', cur[:, 3:4], cur[:, 2:3], cur[:, 3:4], ALU.subtract, ["cur"], ["cur"])
            P.tt('dve', cur[:, 2:3], cn, snn, ALU.mult, ["cur"], ["cur"])
            P.ts('dve', cur[:, 1:2], cur[:, 2:3], 2.0, None, ALU.mult, None, ["cur"], ["cur"])
            P.cp('dve', cur[:, 0:1], cur[:, 3:4], ["cur"], ["cur"])
            n *= 2
        P.ts('dve', rB[:, j, :], ones[:], mag[:, j:j + 1], None, ALU.mult, None, ["ones", "tmpt"] + R, ["rB", "tmpt"])

    xs = [P.sb(f"xs{i}", [128, CH]) for i in range(2)]
    ms = [P.sb(f"ms{i}", [128, CH]) for i in range(2)]
    ys = [P.sb(f"ys{i}", [128, CH]) for i in range(2)]
    NW = 2
    va = [P.sb(f"va{i}", [128, 4, CH]) for i in range(NW)]
    G = [P.sb(f"G{i}", [128, 2, CH]) for i in range(NW)]
    X = [P.sb(f"X{i}", [128, 4, CH]) for i in range(NW)]
    it = 0
    wk = 0
    chunks = [(0, 256)] + [(256 + 512 * i, 512) for i in range(16)]
    for d_ in range(2):
        xin = xf if d_ == 0 else xb
        yout = yf if d_ == 0 else yb
        for b in range(2):
            P.op('dve', lambda g: g.memset(st[:], 0.0), writes=["st"])
            for ci, (t0_, n) in enumerate(chunks):
                sl = it % 2
                it += 1
                mcol = 2 if ci == 0 else b
                P.load(f"ld_x{sl}", xs[sl][:, :n], xin[:, b, t0_:t0_ + n], f"xs{sl}")
                P.ts('dve', ms[sl][:, :n], xs[sl][:, :n], sc1[:, mcol:mcol + 1], sh[:, mcol:mcol + 1], ALU.mult, ALU.add,
                     [f"xs{sl}", "sc1", "sh"], [f"ms{sl}"])
                yp, yid = yring.get()
                for pair in range(4):
                    j = d_ * 4 + pair
                    w_ = wk % NW
                    wk += 1
                    pa, aid = ring.get()
                    pb, bid = ring.get()
                    P.mm(pa[:, :n], Wre[:, j, :], ms[sl][:, :n], True, True, ["W", f"ms{sl}"], [aid])
                    P.mm(pb[:, :n], Wim[:, j, :], ms[sl][:, :n], True, True, ["W", f"ms{sl}"], [bid])
                    c_, s_ = ctab[:, j, :n], stab[:, j, :n]
                    V, VI = va[w_], f"va{w_}"
                    P.tt('dve', V[:, 0, :n], pa[:, :n], c_, ALU.mult, [aid, "tab"], [VI])
                    P.tt('dve', V[:, 1, :n], pb[:, :n], s_, ALU.mult, [bid, "tab"], [VI])
                    P.tt('dve', V[:, 0, :n], V[:, 0, :n], V[:, 1, :n], ALU.add, [VI], [VI])
                    P.tt('dve', V[:, 2, :n], pb[:, :n], c_, ALU.mult, [bid, "tab"], [VI])
                    P.tt('dve', V[:, 3, :n], pa[:, :n], s_, ALU.mult, [aid, "tab"], [VI])
                    P.tt('dve', V[:, 2, :n], V[:, 2, :n], V[:, 3, :n], ALU.subtract, [VI], [VI])
                    GG, GI = G[w_], f"G{w_}"
                    P.op('dve', lambda g: g.tensor_tensor_scan(out=GG[:, 0, :n], data0=rB[:, j, :n], data1=V[:, 0, :n],
                                                               initial=st[:, pair, 0:1], op0=ALU.mult, op1=ALU.add),
                         reads=["rB", VI, "st"], writes=[GI])
                    P.op('dve', lambda g: g.tensor_tensor_scan(out=GG[:, 1, :n], data0=rB[:, j, :n], data1=V[:, 2, :n],
                                                               initial=st[:, pair, 1:2], op0=ALU.mult, op1=ALU.add),
                         reads=["rB", VI, "st"], writes=[GI])
                    wi = 0 if n == 256 else 1
                    cn, snn = En[:, j, wi, 0:1], En[:, j, wi, 1:2]
                    P.ts('dve', cur[:, 0:1], GG[:, 1, n - 1:n], snn, None, ALU.mult, None, [GI, "En"], ["cur"])
                    P.stt(st[:, pair, 0:1], GG[:, 0, n - 1:n], cn, cur[:, 0:1], ALU.mult, ALU.subtract, [GI, "En", "cur"], ["st"])
                    P.ts('dve', cur[:, 1:2], GG[:, 0, n - 1:n], snn, None, ALU.mult, None, [GI, "En"], ["cur"])
                    P.stt(st[:, pair, 1:2], GG[:, 1, n - 1:n], cn, cur[:, 1:2], ALU.mult, ALU.add, [GI, "En", "cur"], ["st"])
                    XX, XI = X[w_], f"X{w_}"
                    P.tt('pool', XX[:, 0, :n], GG[:, 0, :n], c_, ALU.mult, [GI, "tab"], [XI])
                    P.tt('pool', XX[:, 1, :n], GG[:, 1, :n], s_, ALU.mult, [GI, "tab"], [XI])
                    P.tt('pool', XX[:, 2, :n], GG[:, 1, :n], c_, ALU.mult, [GI, "tab"], [XI])
                    P.tt('pool', XX[:, 3, :n], GG[:, 0, :n], s_, ALU.mult, [GI, "tab"], [XI])
                    for q_, Wm in enumerate((Cre, nCre, nCim, nCim)):
                        P.mm(yp[:, :n], Wm[:, j, :], XX[:, q_, :n], pair == 0 and q_ == 0, pair == 3 and q_ == 3,
                             ["W", XI], [yid])
                if d_ == 0:
                    P.stt(ys[sl][:, :n], ms[sl][:, :n], dv[:, 0:1], yp[:, :n], ALU.mult, ALU.add, [f"ms{sl}", "dv", yid], [f"ys{sl}"])
                else:
                    P.cp('act', ys[sl][:, :n], yp[:, :n], [yid], [f"ys{sl}"])
                P.store(f"st_y{sl}", yout[:, b, t0_:t0_ + n], ys[sl][:, :n], f"ys{sl}")
    P.finish()
    return P


def run_s5(x, ctx, mods, p):
    P = build_s5()
    maps = []
    ident = np.eye(128, dtype=np.float32)
    seq_f = np.concatenate([ctx, x], axis=1)
    seq_b = np.concatenate([ctx[:, ::-1], x[:, ::-1]], axis=1)
    for k in range(NCORES):
        fs = slice(128 * k, 128 * k + 128)
        gs = slice(8 * k, 8 * k + 8)

        def st(a):
            return np.ascontiguousarray(a[:, gs].reshape(2, 4, 2, 64).transpose(2, 3, 0, 1).reshape(128, 8))

        def bl(a):
            return np.ascontiguousarray(a.reshape(2, 4, 2, 64, 16).transpose(2, 3, 0, 1, 4).reshape(128, 8, 16))
        maps.append({
            "xf": np.ascontiguousarray(seq_f[:, :, fs].transpose(2, 0, 1)),
            "xb": np.ascontiguousarray(seq_b[:, :, fs].transpose(2, 0, 1)),
            "msc": np.ascontiguousarray(mods[0, :, 1024 + 128 * k:1024 + 128 * k + 128].T),
            "msh": np.ascontiguousarray(mods[0, :, 128 * k:128 * k + 128].T),
            "a_re": st(p['s5_a_re'][0]), "a_im": st(p['s5_a_im'][0]),
            "ldt": st(np.broadcast_to(p['s5_log_dt'][0][:, :, None], (2, 64, 64))),
            "bre": bl(p['s5_b_re'][0][:, gs]), "bim": bl(p['s5_b_im'][0][:, gs]),
            "cre": bl(p['s5_c_re'][0][:, gs].transpose(0, 1, 3, 2)), "cim": bl(p['s5_c_im'][0][:, gs].transpose(0, 1, 3, 2)),
            "dvec": np.ascontiguousarray(p['s5_d'][0, fs].reshape(128, 1)),
            "ident": ident,
        })
    res = P.run(maps)
    yf = np.concatenate([r["yf"] for r in res], axis=0).transpose(1, 2, 0)
    yb = np.concatenate([r["yb"] for r in res], axis=0).transpose(1, 2, 0)
    yf_c, yf_l = yf[:, :256], yf[:, 256:]
    yb_c, yb_l = yb[:, :256][:, ::-1], yb[:, 256:][:, ::-1]
    return yf_l, yf_c, yb_l, yb_c


def transpose_tile(P, ring, src, sid, dst, did, nk, ident, width=128):
    for kc in range(nk):
        pt, pid = ring.get()
        P.tr(pt[:, 0:128], src[:, kc * 128:(kc + 1) * 128], ident[:], [sid, "ident"], [pid])
        P.cp('act' if kc % 2 == 0 else 'dve', dst[:, kc, :], pt[:, 0:128], [pid], [did])


def linear(P, ring, xT, xid, W, wid, nk, N, cb, bs=512):
    nb = 0
    c0 = 0
    while c0 < N:
        n = min(bs, N - c0)
        pt, pid = ring.get()
        for kc in range(nk):
            P.mm(pt[:, 0:n], xT[:, kc, :], W[:, kc, c0:c0 + n], kc == 0, kc == nk - 1, [xid, wid], [pid])
        cb(nb, c0, pt[:, 0:n], pid, n)
        c0 += n
        nb += 1


def layernorm(P, x, xid, gbc, bbc, out, oid, stats, mv):
    for c in range(2):
        P.op('dve', lambda g: g.bn_stats(out=stats[:, c, :], in_=x[:, c * 512:(c + 1) * 512]), reads=[xid], writes=["lnst"])
    P.op('dve', lambda g: g.bn_aggr(out=mv[:, 0:2], in_=stats[:].rearrange("p a b -> p (a b)")), reads=["lnst"], writes=["lnmv"])
    P.ts('dve', mv[:, 2:3], mv[:, 1:2], LN_EPS, None, ALU.add, None, ["lnmv"], ["lnmv"])
    P.actf(mv[:, 2:3], mv[:, 2:3], AF.Sqrt, ["lnmv"], ["lnmv"])
    P.op('dve', lambda g: g.reciprocal(out=mv[:, 3:4], in_=mv[:, 2:3]), reads=["lnmv"], writes=["lnmv"])
    P.ts('dve', x, x, mv[:, 0:1], mv[:, 3:4], ALU.subtract, ALU.mult, [xid, "lnmv"], [xid])
    P.tt('pool', x, x, gbc, ALU.mult, [xid, "lng"], [xid])
    P.tt('pool', out, x, bbc, ALU.add, [xid, "lnb"], [oid])


def build_s5out(nt=NT):
    P = Prog()
    x_d = P.din("x", [nt * 128, D]); yf_d = P.din("yf", [nt * 128, D]); yb_d = P.din("yb", [nt * 128, D])
    gL_d = P.din("gateL", [128, D]); gC_d = P.din("gateC", [128, D])
    wg_d = P.din("wglu", [D, D]); wo_d = P.din("wo", [D, D])
    lng_d = P.din("lng", [128, D]); lnb_d = P.din("lnb", [128, D]); id_d = P.din("ident", [128, 128])
    h1_d = P.dout("h1", [nt * 128, D])
    ident = P.sb("ident", [128, 128]); gL = P.sb("gL", [128, D]); gC = P.sb("gC", [128, D])
    wg = P.sb("wg", [128, 8, D]); wo = P.sb("wo", [128, 8, D]); lng = P.sb("lng", [128, D]); lnb = P.sb("lnb", [128, D])
    P.load("ld_c", ident[:], id_d, "ident"); P.load("ld_c", gL[:], gL_d, "gL"); P.load("ld_c", gC[:], gC_d, "gC")
    P.load("ld_c", lng[:], lng_d, "lng"); P.load("ld_c", lnb[:], lnb_d, "lnb")
    P.load("ld_wg", wg[:], wg_d.rearrange("(kc p) n -> p kc n", p=128), "wg")
    P.load("ld_wo", wo[:], wo_d.rearrange("(kc p) n -> p kc n", p=128), "wo")
    xs = [P.sb(f"x{i}", [128, D]) for i in range(2)]
    ya = [P.sb(f"ya{i}", [128, D]) for i in range(2)]
    yb_ = [P.sb(f"yb{i}", [128, D]) for i in range(2)]
    A = P.sb("A", [128, D]); B = P.sb("B", [128, D]); C = P.sb("C", [128, D]); T = P.sb("T", [128, 8, 128])
    O = [P.sb(f"O{i}", [128, D]) for i in range(2)]
    stats = P.sb("stats", [128, 2, 6]); mv = P.sb("mv", [128, 4])
    tring = PsumRing(P, range(0, 4)); lring = PsumRing(P, range(4, 8))
    for t in range(nt):
        s = t % 2
        rows = slice(t * 128, (t + 1) * 128)
        P.load(f"ld_x{s}", xs[s][:], x_d[rows, :], f"x{s}")
        P.load(f"ld_ya{s}", ya[s][:], yf_d[rows, :], f"ya{s}")
        P.load(f"ld_yb{s}", yb_[s][:], yb_d[rows, :], f"yb{s}")
        gate = gC if t == 16 else gL
        gid = "gC" if t == 16 else "gL"
        P.tt('dve', A[:], ya[s][:], yb_[s][:], ALU.add, [f"ya{s}", f"yb{s}"], ["A"])
        P.actf(B[:], A[:], AF.Gelu, ["A"], ["B"])
        transpose_tile(P, tring, B, "B", T, "T", 8, ident)
        linear(P, lring, T, "T", wg, "wg", 8, D,
               lambda nb, c0, ps, pid, n: P.actf(C[:, c0:c0 + n], ps, AF.Sigmoid, [pid], ["C"]))
        P.tt('dve', C[:], C[:], B[:], ALU.mult, ["B", "C"], ["C"])
        transpose_tile(P, tring, C, "C", T, "T", 8, ident)
        linear(P, lring, T, "T", wo, "wo", 8, D,
               lambda nb, c0, ps, pid, n: P.tt('dve', A[:, c0:c0 + n], ps, gate[:, c0:c0 + n], ALU.mult, [pid, gid], ["A"]))
        P.stt(A[:], xs[s][:], ALPHA, A[:], ALU.mult, ALU.add, [f"x{s}", "A"], ["A"])
        layernorm(P, A[:], "A", lng[:], lnb[:], O[s][:], f"O{s}", stats, mv)
        P.store(f"st_o{s}", h1_d[rows, :], O[s][:], f"O{s}")
    P.finish()
    return P


def bc(v):
    return np.ascontiguousarray(np.broadcast_to(v[None, :], (128, v.shape[0])).astype(np.float32))


def tok_shard(lat, ctxv, k):
    b, j = k // 4, k % 4
    out = np.zeros((TPC, lat.shape[-1]), dtype=lat.dtype)
    out[:2048] = lat[b, 2048 * j:2048 * (j + 1)]
    out[2048:2048 + 64] = ctxv[b, 64 * j:64 * (j + 1)]
    return out


def tok_unshard(slabs, feat):
    lat = np.zeros((2, 8192, feat), dtype=slabs[0].dtype)
    ctxv = np.zeros((2, 256, feat), dtype=slabs[0].dtype)
    for k in range(NCORES):
        b, j = k // 4, k % 4
        lat[b, 2048 * j:2048 * (j + 1)] = slabs[k][:2048]
        if slabs[k].shape[0] > 2048:
            ctxv[b, 64 * j:64 * (j + 1)] = slabs[k][2048:2048 + 64]
    return lat, ctxv


def run_s5out(x, ctx, ys, mods, p):
    yf_l, yf_c, yb_l, yb_c = ys
    P = build_s5out()
    ident = np.eye(128, dtype=np.float32)
    maps = []
    for k in range(NCORES):
        b = k // 4
        maps.append({
            "x": tok_shard(x, ctx, k), "yf": tok_shard(yf_l, yf_c, k), "yb": tok_shard(yb_l, yb_c, k),
            "gateL": bc(mods[0, b, 2048:3072]), "gateC": bc(mods[0, 2, 2048:3072]),
            "wglu": p['s5_w_glu'][0], "wo": p['s5_w_o'][0],
            "lng": bc(p['ln_g'][0, 0]), "lnb": bc(p['ln_b'][0, 0]), "ident": ident,
        })
    res = P.run(maps)
    return tok_unshard([r["h1"] for r in res], D)


NEG = -1.0e30


def build_peer(nt):
    P = Prog()
    h_d = P.din("h", [nt * 128, D])
    mL_d = P.din("modL", [128, 3, D]); mC_d = P.din("modC", [128, 3, D])
    wq_d = P.din("wq", [D, D]); kbd_d = P.din("kbd", [128, 8, 256])
    u_d = P.din("u", [16384, D]); v_d = P.din("v", [16384, D])
    lng_d = P.din("lng", [128, D]); lnb_d = P.din("lnb", [128, D]); id_d = P.din("ident", [128, 128])
    io_d = P.din("iota16", [128, 16])
    o_d = P.dout("o", [nt * 128, D])
    ident = P.sb("ident", [128, 128]); mL = P.sb("mL", [128, 3, D]); mC = P.sb("mC", [128, 3, D])
    wq = P.sb("wq", [128, 8, D]); kbd = P.sb("kbd", [128, 8, 256]); lng = P.sb("lng", [128, D]); lnb = P.sb("lnb", [128, D])
    iota = P.sb("iota", [128, 16])
    for dst, src, nm in ((ident, id_d, "ident"), (mL, mL_d, "mL"), (mC, mC_d, "mC"), (kbd, kbd_d, "kbd"),
                         (lng, lng_d, "lng"), (lnb, lnb_d, "lnb"), (iota, io_d, "iota")):
        P.load("ld_c", dst[:], src, nm)
    P.load("ld_wq", wq[:], wq_d.rearrange("(kc p) n -> p kc n", p=128), "wq")
    P.ts('dve', mL[:, 1, :], mL[:, 1, :], 1.0, None, ALU.add, None, ["mL"], ["mL"])
    P.ts('dve', mC[:, 1, :], mC[:, 1, :], 1.0, None, ALU.add, None, ["mC"], ["mC"])
    hs = [P.sb(f"h{i}", [128, D]) for i in range(2)]
    M = P.sb("M", [128, D]); T = P.sb("T", [128, 8, 128]); Q = P.sb("Q", [128, D])
    sc = P.sb("sc", [128, 16, 128]); sc2 = P.sb("sc2", [128, 16, 128])
    sv = P.sb("sv", [128, 16, 16]); si = P.sb("si", [128, 16, 16], U32); sif = P.sb("sif", [128, 16, 16])
    cand = P.sb("cand", [128, 8, 256]); cand2 = P.sb("cand2", [128, 8, 256])
    cv = P.sb("cv", [128, 8, 16]); ci = P.sb("ci", [128, 8, 16], U32); cab = P.sb("cab", [128, 2, 8, 16], U32)
    cabf = P.sb("cabf", [128, 2, 8, 16])
    OH = P.sb("OH", [128, 8, 256])
    i12 = P.sb("i12", [128, 2, 8, 16]); ef = P.sb("ef", [128, 128]); eidx = P.sb("eidx", [128, 128], U32)
    gt = P.sb("gt", [128, 8, 16]); gs = P.sb("gs", [128, 8]); dots = P.sb("dots", [128, 128]); actv = P.sb("actv", [128, 128])
    NS = 4
    ug = [P.sb(f"ug{i}", [128, D]) for i in range(NS)]
    vg = [P.sb(f"vg{i}", [128, D]) for i in range(NS)]
    junk = P.sb("junk", [128, D]); acc = P.sb("acc", [128, D])
    O = [P.sb(f"O{i}", [128, D]) for i in range(2)]
    stats = P.sb("stats", [128, 2, 6]); mv = P.sb("mv", [128, 4])
    tring = PsumRing(P, range(0, 4)); lring = PsumRing(P, range(4, 8))
    sv4 = sv[:].rearrange("p (h s) k -> p h s k", s=2)
    sif4 = sif[:].rearrange("p (h s) k -> p h s k", s=2)
    B4 = [128, 8, 16, 16]
    for t in range(nt):
        s = t % 2
        rows = slice(t * 128, (t + 1) * 128)
        mod, mid = (mC, "mC") if t == 16 else (mL, "mL")
        P.load(f"ld_h{s}", hs[s][:], h_d[rows, :], f"h{s}")
        P.tt('dve', M[:], hs[s][:], mod[:, 1, :], ALU.mult, [f"h{s}", mid], ["M"])
        P.tt('dve', M[:], M[:], mod[:, 0, :], ALU.add, ["M", mid], ["M"])
        transpose_tile(P, tring, M, "M", T, "T", 8, ident)
        linear(P, lring, T, "T", wq, "wq", 8, D,
               lambda nb, c0, ps, pid, n: P.cp('act', Q[:, c0:c0 + n], ps, [pid], ["Q"]))
        transpose_tile(P, tring, Q, "Q", T, "T", 8, ident)
        for hh in range(8):
            pt, pid = lring.get()
            P.mm(pt[:, 0:256], T[:, hh, :], kbd[:, hh, :], True, True, ["T", "kbd"], [pid])
            P.cp('act', sc[:, 2 * hh:2 * hh + 2, :], pt[:, 0:256].rearrange("p (s n) -> p s n", s=2), [pid], ["sc"])
        for blk in range(16):
            P.op('dve', lambda g: g.max(out=sv[:, blk, 0:8], in_=sc[:, blk, :]), reads=["sc"], writes=["sv"])
            P.op('dve', lambda g: g.max_index(out=si[:, blk, 0:8], in_max=sv[:, blk, 0:8], in_values=sc[:, blk, :]),
                 reads=["sc", "sv"], writes=["si"])
            P.op('dve', lambda g: g.match_replace(out=sc2[:, blk, :], in_to_replace=sv[:, blk, 0:8], in_values=sc[:, blk, :],
                                                  imm_value=NEG), reads=["sc", "sv"], writes=["sc2"])
            P.op('dve', lambda g: g.max(out=sv[:, blk, 8:16], in_=sc2[:, blk, :]), reads=["sc2"], writes=["sv"])
            P.op('dve', lambda g: g.max_index(out=si[:, blk, 8:16], in_max=sv[:, blk, 8:16], in_values=sc2[:, blk, :]),
                 reads=["sc2", "sv"], writes=["si"])
        P.cp('dve', sif[:], si[:], ["si"], ["sif"])
        P.tt('dve', cand[:].rearrange("p h (a b) -> p h a b", b=16), sv4[:, :, 0, :].unsqueeze(3).to_broadcast(B4),
             sv4[:, :, 1, :].unsqueeze(2).to_broadcast(B4), ALU.add, ["sv"], ["cand"])
        for hh in range(8):
            P.op('dve', lambda g: g.max(out=cv[:, hh, 0:8], in_=cand[:, hh, :]), reads=["cand"], writes=["cv"])
            P.op('dve', lambda g: g.max_index(out=ci[:, hh, 0:8], in_max=cv[:, hh, 0:8], in_values=cand[:, hh, :]),
                 reads=["cand", "cv"], writes=["ci"])
            P.op('dve', lambda g: g.match_replace(out=cand2[:, hh, :], in_to_replace=cv[:, hh, 0:8], in_values=cand[:, hh, :],
                                                  imm_value=NEG), reads=["cand", "cv"], writes=["cand2"])
            P.op('dve', lambda g: g.max(out=cv[:, hh, 8:16], in_=cand2[:, hh, :]), reads=["cand2"], writes=["cv"])
            P.op('dve', lambda g: g.max_index(out=ci[:, hh, 8:16], in_max=cv[:, hh, 8:16], in_values=cand2[:, hh, :]),
                 reads=["cand2", "cv"], writes=["ci"])
        P.op('dve', lambda g: g.tensor_single_scalar(out=cab[:, 0], in_=ci[:], scalar=4, op=ALU.logical_shift_right),
             reads=["ci"], writes=["cab"])
        P.op('dve', lambda g: g.tensor_single_scalar(out=cab[:, 1], in_=ci[:], scalar=15, op=ALU.bitwise_and),
             reads=["ci"], writes=["cab"])
        P.cp('dve', cabf[:], cab[:], ["cab"], ["cabf"])
        OH4 = OH[:].rearrange("p h (a b) -> p h a b", b=16)
        for w_ in range(2):
            P.tt('dve', OH4, iota[:].unsqueeze(1).unsqueeze(1).to_broadcast(B4), cabf[:, w_].unsqueeze(3).to_broadcast(B4),
                 ALU.is_equal, ["iota", "cabf"], ["OH"])
            P.tt('dve', OH4, OH4, sif4[:, :, w_, :].unsqueeze(2).to_broadcast(B4), ALU.mult, ["OH", "sif"], ["OH"])
            P.op('dve', lambda g: g.tensor_reduce(out=i12[:, w_], in_=OH4, axis=AX.X, op=ALU.add), reads=["OH"], writes=["i12"])
        P.stt(ef[:].rearrange("p (h k) -> p h k", k=16), i12[:, 0], 128.0, i12[:, 1], ALU.mult, ALU.add, ["i12"], ["ef"])
        P.cp('dve', eidx[:], ef[:], ["ef"], ["eidx"])
        P.tt('dve', gt[:], cv[:], cv[:, :, 0:1].to_broadcast([128, 8, 16]), ALU.subtract, ["cv"], ["gt"])
        P.actf(gt[:], gt[:], AF.Exp, ["gt"], ["gt"])
        P.op('dve', lambda g: g.tensor_reduce(out=gs[:], in_=gt[:], axis=AX.X, op=ALU.add), reads=["gt"], writes=["gs"])
        P.op('dve', lambda g: g.reciprocal(out=gs[:], in_=gs[:]), reads=["gs"], writes=["gs"])
        P.tt('dve', gt[:], gt[:], gs[:].unsqueeze(2).to_broadcast([128, 8, 16]), ALU.mult, ["gt", "gs"], ["gt"])
        for k in range(128):
            sl = k % NS
            P.dma('pool', f"gu{sl}", lambda e: e.indirect_dma_start(
                out=ug[sl][:], out_offset=None, in_=u_d[:, :],
                in_offset=bass.IndirectOffsetOnAxis(ap=eidx[:, k:k + 1], axis=0)), reads=["eidx"], writes=[f"ug{sl}"])
            P.op('dve', lambda g: g.scalar_tensor_tensor(out=junk[:], in0=ug[sl][:], scalar=1.0, in1=M[:], op0=ALU.mult,
                                                         op1=ALU.mult, accum_out=dots[:, k:k + 1]),
                 reads=[f"ug{sl}", "M"], writes=["junk", "dots"])
        P.actf(actv[:], dots[:], AF.Gelu, ["dots"], ["actv"])
        P.tt('dve', actv[:], actv[:], gt[:].rearrange("p h k -> p (h k)"), ALU.mult, ["actv", "gt"], ["actv"])
        for k in range(128):
            sl = k % NS
            P.dma('pool', f"gv{sl}", lambda e: e.indirect_dma_start(
                out=vg[sl][:], out_offset=None, in_=v_d[:, :],
                in_offset=bass.IndirectOffsetOnAxis(ap=eidx[:, k:k + 1], axis=0)), reads=["eidx"], writes=[f"vg{sl}"])
            if k == 0:
                P.ts('dve', acc[:], vg[sl][:], actv[:, 0:1], None, ALU.mult, None, [f"vg{sl}", "actv"], ["acc"])
            else:
                P.stt(acc[:], vg[sl][:], actv[:, k:k + 1], acc[:], ALU.mult, ALU.add, [f"vg{sl}", "actv", "acc"], ["acc"])
        P.tt('dve', acc[:], acc[:], mod[:, 2, :], ALU.mult, ["acc", mid], ["acc"])
        P.stt(acc[:], hs[s][:], ALPHA, acc[:], ALU.mult, ALU.add, [f"h{s}", "acc"], ["acc"])
        layernorm(P, acc[:], "acc", lng[:], lnb[:], O[s][:], f"O{s}", stats, mv)
        P.store(f"st_o{s}", o_d[rows, :], O[s][:], f"O{s}")
    P.finish()
    return P


def run_peer(h_lat, h_ctx, mods, layer, p, with_ctx):
    nt = NT if with_ctx else 16
    P = build_peer(nt)
    ident = np.eye(128, dtype=np.float32)
    iota16 = bc(np.arange(16, dtype=np.float32))
    keys = p['peer_keys'][layer]
    kbd = np.zeros((128, 8, 256), dtype=np.float32)
    for s in range(2):
        kbd[64 * s:64 * s + 64, :, 128 * s:128 * s + 128] = keys[:, s].transpose(2, 0, 1)
    maps = []
    for k in range(NCORES):
        b = k // 4
        slab = tok_shard(h_lat, h_ctx, k)[:nt * 128]
        ml = np.stack([bc(mods[layer, b, 3072:4096]), bc(mods[layer, b, 4096:5120]), bc(mods[layer, b, 5120:6144])], axis=1)
        mc = np.stack([bc(mods[layer, 2, 3072:4096]), bc(mods[layer, 2, 4096:5120]), bc(mods[layer, 2, 5120:6144])], axis=1)
        maps.append({"h": slab, "modL": np.ascontiguousarray(ml), "modC": np.ascontiguousarray(mc),
                     "wq": p['peer_w_q'][layer], "kbd": kbd, "u": p['peer_u'][layer], "v": p['peer_v'][layer],
                     "lng": bc(p['ln_g'][layer, 1]), "lnb": bc(p['ln_b'][layer, 1]), "ident": ident, "iota16": iota16})
    res = P.run(maps)
    return tok_unshard([r["o"] for r in res], D)


def rmsnorm(P, x, xid, n, gbc, gid, out, oid, junk, ss):
    P.actf(junk, x, AF.Square, [xid], ["rjunk"], accum=ss[:, 0:1])
    P.ts('dve', ss[:, 1:2], ss[:, 0:1], 1.0 / n, RMS_EPS, ALU.mult, ALU.add, ["rjunk"], ["rss"])
    P.actf(ss[:, 1:2], ss[:, 1:2], AF.Sqrt, ["rss"], ["rss"])
    P.op('dve', lambda g: g.reciprocal(out=ss[:, 2:3], in_=ss[:, 1:2]), reads=["rss"], writes=["rss"])
    P.stt(out, x, ss[:, 2:3], gbc, ALU.mult, ALU.mult, [xid, "rss", gid], [oid])


def rope(P, x1, x2, cosb, sinb, o1, o2, t, rid, wid):
    P.tt('dve', t[0], x1, cosb, ALU.mult, rid, ["ropet"])
    P.tt('dve', t[1], x2, sinb, ALU.mult, rid, ["ropet"])
    P.tt('dve', t[2], x2, cosb, ALU.mult, rid, ["ropet"])
    P.tt('dve', t[3], x1, sinb, ALU.mult, rid, ["ropet"])
    P.tt('dve', o1, t[0], t[1], ALU.subtract, ["ropet"], wid)
    P.tt('dve', o2, t[2], t[3], ALU.add, ["ropet"], wid)


def build_mlaproj(nt=NT):
    P = Prog()
    h_d = P.din("h", [nt * 128, D])
    mL_d = P.din("modL", [128, 2, D]); mC_d = P.din("modC", [128, 2, D])
    wdq_d = P.din("wdq", [D, 384]); qn_d = P.din("qn", [128, 384]); wuq_d = P.din("wuq", [384, 1536])
    wdkv_d = P.din("wdkv", [D, 320]); kvn_d = P.din("kvn", [128, 256]); wukv_d = P.din("wukv", [256, 2048])
    cos_d = P.din("cos", [nt * 128, 32]); sin_d = P.din("sin", [nt * 128, 32]); id_d = P.din("ident", [128, 128])
    q_o = P.dout("q", [nt * 128, 1536], BF16); kvu_o = P.dout("kvu", [nt * 128, 2048], BF16); kr_o = P.dout("kr", [nt * 128, 64], BF16)
    ident = P.sb("ident", [128, 128]); mL = P.sb("mL", [128, 2, D]); mC = P.sb("mC", [128, 2, D])
    wdq = P.sb("wdq", [128, 8, 384]); qn = P.sb("qn", [128, 384]); wuq = P.sb("wuq", [128, 3, 1536])
    wdkv = P.sb("wdkv", [128, 8, 320]); kvn = P.sb("kvn", [128, 256]); wukv = P.sb("wukv", [128, 2, 2048])
    for dst, src, nm in ((ident, id_d, "ident"), (mL, mL_d, "mL"), (mC, mC_d, "mC"), (qn, qn_d, "qn"), (kvn, kvn_d, "kvn")):
        P.load("ld_c", dst[:], src, nm)
    for dst, src, nm in ((wdq, wdq_d, "wdq"), (wuq, wuq_d, "wuq"), (wdkv, wdkv_d, "wdkv"), (wukv, wukv_d, "wukv")):
        P.load("ld_w", dst[:], src.rearrange("(kc p) n -> p kc n", p=128), nm)
    P.ts('dve', mL[:, 1, :], mL[:, 1, :], 1.0, None, ALU.add, None, ["mL"], ["mL"])
    P.ts('dve', mC[:, 1, :], mC[:, 1, :], 1.0, None, ALU.add, None, ["mC"], ["mC"])
    hs = [P.sb(f"h{i}", [128, D]) for i in range(2)]
    cs = [P.sb(f"cs{i}", [128, 2, 32]) for i in range(2)]
    M = P.sb("M", [128, D]); T = P.sb("T", [128, 8, 128]); T2 = P.sb("T2", [128, 3, 128])
    cq = P.sb("cq", [128, 384]); cqn = P.sb("cqn", [128, 384]); junk = P.sb("junk", [128, 384]); ss = P.sb("ss", [128, 4])
    Qf = P.sb("Qf", [128, 1536]); kv = P.sb("kv", [128, 320]); ckn = P.sb("ckn", [128, 256])
    rt = P.sb("rt", [128, 4, 8, 32])
    QO = [P.sb(f"QO{i}", [128, 1536], BF16) for i in range(2)]
    KVO = [P.sb(f"KVO{i}", [128, 2048], BF16) for i in range(2)]
    KR = [P.sb(f"KR{i}", [128, 64], BF16) for i in range(2)]
    tring = PsumRing(P, range(0, 4)); lring = PsumRing(P, range(4, 8))
    for t in range(nt):
        s = t % 2
        rows = slice(t * 128, (t + 1) * 128)
        mod, mid = (mC, "mC") if t == 16 else (mL, "mL")
        P.load(f"ld_h{s}", hs[s][:], h_d[rows, :], f"h{s}")
        P.load(f"ld_cs{s}", cs[s][:, 0, :], cos_d[rows, :], f"cs{s}")
        P.load(f"ld_cs{s}", cs[s][:, 1, :], sin_d[rows, :], f"cs{s}")
        P.tt('dve', M[:], hs[s][:], mod[:, 1, :], ALU.mult, [f"h{s}", mid], ["M"])
        P.tt('dve', M[:], M[:], mod[:, 0, :], ALU.add, ["M", mid], ["M"])
        transpose_tile(P, tring, M, "M", T, "T", 8, ident)
        linear(P, lring, T, "T", wdq, "wdq", 8, 384, lambda nb, c0, ps, pid, n: P.cp('act', cq[:, c0:c0 + n], ps, [pid], ["cq"]))
        rmsnorm(P, cq[:], "cq", 384, qn[:], "qn", cqn[:], "cqn", junk[:], ss)
        transpose_tile(P, tring, cqn, "cqn", T2, "T2", 3, ident)
        linear(P, lring, T2, "T2", wuq, "wuq", 3, 1536,
               lambda nb, c0, ps, pid, n: P.cp('act' if nb % 2 else 'dve', Qf[:, c0:c0 + n], ps, [pid], ["Qf"]))
        Qf4 = Qf[:].rearrange("p (h d) -> p h d", d=192)
        QO4 = QO[s][:].rearrange("p (h d) -> p h d", d=192)
        cosb = cs[s][:, 0, :].unsqueeze(1).to_broadcast([128, 8, 32])
        sinb = cs[s][:, 1, :].unsqueeze(1).to_broadcast([128, 8, 32])
        P.cp('act', QO4[:, :, 0:128], Qf4[:, :, 0:128], ["Qf"], [f"QO{s}"])
        rope(P, Qf4[:, :, 128:160], Qf4[:, :, 160:192], cosb, sinb, QO4[:, :, 128:160], QO4[:, :, 160:192],
             [rt[:, i] for i in range(4)], ["Qf", f"cs{s}"], [f"QO{s}"])
        P.store(f"st_q{s}", q_o[rows, :], QO[s][:], f"QO{s}")
        linear(P, lring, T, "T", wdkv, "wdkv", 8, 320, lambda nb, c0, ps, pid, n: P.cp('act', kv[:, c0:c0 + n], ps, [pid], ["kv"]))
        rmsnorm(P, kv[:, 0:256], "kv", 256, kvn[:], "kvn", ckn[:], "ckn", junk[:, 0:256], ss)
        rope(P, kv[:, 256:288], kv[:, 288:320], cs[s][:, 0, :], cs[s][:, 1, :], KR[s][:, 0:32], KR[s][:, 32:64],
             [rt[:, i, 0, :] for i in range(4)], ["kv", f"cs{s}"], [f"KR{s}"])
        P.store(f"st_kr{s}", kr_o[rows, :], KR[s][:], f"KR{s}")
        transpose_tile(P, tring, ckn, "ckn", T2, "T2", 2, ident)
        linear(P, lring, T2, "T2", wukv, "wukv", 2, 2048,
               lambda nb, c0, ps, pid, n: P.cp('act' if nb % 2 else 'dve', KVO[s][:, c0:c0 + n], ps, [pid], [f"KVO{s}"]))
        P.store(f"st_kv{s}", kvu_o[rows, :], KVO[s][:], f"KVO{s}")
    P.finish()
    return P


def rope_tables():
    n_freq = 16
    inv = (10000.0 ** (-np.arange(n_freq, dtype=np.float32) / n_freq)).astype(np.float32)
    r, col = np.meshgrid(np.arange(128, dtype=np.float32), np.arange(64, dtype=np.float32), indexing='ij')
    ang = np.concatenate([r.reshape(-1, 1) * inv, col.reshape(-1, 1) * inv], axis=-1).astype(np.float32)
    return np.cos(ang).astype(np.float32), np.sin(ang).astype(np.float32)


def run_mlaproj(h_lat, h_ctx, mods, p):
    P = build_mlaproj()
    ident = np.eye(128, dtype=np.float32)
    cosl, sinl = rope_tables()
    maps = []
    for k in range(NCORES):
        b, j = k // 4, k % 4
        cos = np.ones((TPC, 32), np.float32); sin = np.zeros((TPC, 32), np.float32)
        cos[:2048] = cosl[2048 * j:2048 * (j + 1)]; sin[:2048] = sinl[2048 * j:2048 * (j + 1)]
        ml = np.stack([bc(mods[1, b, 0:1024]), bc(mods[1, b, 1024:2048])], axis=1)
        mc = np.stack([bc(mods[1, 2, 0:1024]), bc(mods[1, 2, 1024:2048])], axis=1)
        maps.append({"h": tok_shard(h_lat, h_ctx, k), "modL": np.ascontiguousarray(ml), "modC": np.ascontiguousarray(mc),
                     "wdq": p['mla_w_dq'][0], "qn": bc(p['mla_q_norm'][0]), "wuq": p['mla_w_uq'][0],
                     "wdkv": p['mla_w_dkv'][0], "kvn": bc(p['mla_kv_norm'][0]), "wukv": p['mla_w_ukv'][0],
                     "cos": cos, "sin": sin, "ident": ident})
    res = P.run(maps)
    q_l, _ = tok_unshard([r["q"] for r in res], 1536)
    kvu_l, kvu_c = tok_unshard([r["kvu"] for r in res], 2048)
    kr_l, kr_c = tok_unshard([r["kr"] for r in res], 64)
    return q_l, kvu_l, kvu_c, kr_l, kr_c


NK = 8448
NKT = 66
ATT_SCALE = 192.0 ** -0.5


def build_attn():
    P = Prog()
    qT_d = P.din("qT", [8, 128, 2048], BF16); qrT_d = P.din("qrT", [8, 64, 2048], BF16)
    kT_d = P.din("kT", [8, 128, NK], BF16); krT_d = P.din("krT", [64, NK], BF16)
    v_d = P.din("v", [8, 128, NKT * 129], BF16)
    h_d = P.din("h", [2048, D]); gate_d = P.din("gate", [128, D]); wo_d = P.din("wo", [D, D])
    lng_d = P.din("lng", [128, D]); lnb_d = P.din("lnb", [128, D]); id_d = P.din("ident", [128, 128])
    o_d = P.dout("o", [2048, D])
    ident = P.sb("ident", [128, 128]); gate = P.sb("gate", [128, D]); wo = P.sb("wo", [128, 8, D])
    lng = P.sb("lng", [128, D]); lnb = P.sb("lnb", [128, D])
    krTa = P.sb("krTa", [65, NK], BF16)
    for dst, src, nm in ((ident, id_d, "ident"), (gate, gate_d, "gate"), (lng, lng_d, "lng"), (lnb, lnb_d, "lnb")):
        P.load("ld_c", dst[:], src, nm)
    P.load("ld_wo", wo[:], wo_d.rearrange("(kc p) n -> p kc n", p=128), "wo")
    P.load("ld_kr", krTa[0:64, :], krT_d, "krTa")
    P.op('dve', lambda g: g.memset(krTa[64:65, :], 1.0), writes=["krTa"])
    kT = [P.sb(f"kT{i}", [128, NK], BF16) for i in range(2)]
    vh = [P.sb(f"vh{i}", [128, NKT * 129], BF16) for i in range(2)]
    qT = [P.sb(f"qT{i}", [128, 512], BF16) for i in range(2)]
    qrTa = [P.sb(f"qrTa{i}", [65, 512], BF16) for i in range(2)]
    NPT = 3
    PT = [P.sb(f"PT{i}", [128, 512], BF16) for i in range(NPT)]
    mx = P.sb("mx", [128, 20]); nmp = P.sb("nmp", [128, 65]); rden = P.sb("rden", [128, 4])
    otok = P.sb("otok", [128, 4, D])
    hs = [P.sb(f"hs{i}", [128, D]) for i in range(2)]
    A = P.sb("A", [128, D]); T = P.sb("T", [128, 8, 128])
    O = [P.sb(f"O{i}", [128, D]) for i in range(2)]
    stats = P.sb("stats", [128, 2, 6]); mv = P.sb("mv", [128, 4])
    sring = PsumRing(P, range(0, 3)); mring = PsumRing(P, range(3, 4)); oring = PsumRing(P, range(4, 8))
    P.op('dve', lambda g: g.memset(nmp[:], 0.0), writes=["nmp"])
    kblocks = [(512 * i, 512) for i in range(16)] + [(8192, 256)]
    it = 0
    ipt = 0
    tcount = 0
    for qb in range(4):
        for hh in range(8):
            s = it % 2
            it += 1
            P.load(f"ld_k{s}", kT[s][:], kT_d[hh], f"kT{s}")
            P.load(f"ld_v{s}", vh[s][:], v_d[hh], f"vh{s}")
            P.load(f"ld_q{s}", qT[s][:], qT_d[hh, :, qb * 512:(qb + 1) * 512], f"qT{s}")
            P.load(f"ld_qr{s}", qrTa[s][0:64, :], qrT_d[hh, :, qb * 512:(qb + 1) * 512], f"qrTa{s}")
            for qs in range(4):
                qc = slice(qs * 128, (qs + 1) * 128)
                for kb, (k0, n) in enumerate(kblocks):
                    pt, pid = sring.get()
                    P.mm(pt[:, 0:n], qT[s][:, qc], kT[s][:, k0:k0 + n], True, False, [f"qT{s}", f"kT{s}"], [pid])
                    P.mm(pt[:, 0:n], qrTa[s][0:64, qc], krTa[0:64, k0:k0 + n], False, True, [f"qrTa{s}", "krTa"], [pid])
                    P.op('dve', lambda g: g.tensor_reduce(out=mx[:, kb:kb + 1], in_=pt[:, 0:n], axis=AX.X, op=ALU.max),
                         reads=[pid], writes=["mx"])
                P.op('dve', lambda g: g.tensor_reduce(out=mx[:, 17:18], in_=mx[:, 0:17], axis=AX.X, op=ALU.max),
                     reads=["mx"], writes=["mx"])
                P.ts('dve', nmp[:, 64:65], mx[:, 17:18], -1.0, None, ALU.mult, None, ["mx"], ["nmp"])
                pt, pid = mring.get()
                P.mm(pt[0:65, 0:128], nmp[:, :], ident[:], True, True, ["nmp", "ident"], [pid])
                P.cp('act', qrTa[s][64:65, qc], pt[64:65, 0:128], [pid], [f"qrTa{s}"])
            ops = [oring.get() for _ in range(4)]
            for kt in range(NKT):
                kc = slice(kt * 128, (kt + 1) * 128)
                pt, pid = sring.get()
                P.mm(pt[:, 0:512], kT[s][:, kc], qT[s][:, :], True, False, [f"kT{s}", f"qT{s}"], [pid])
                P.mm(pt[:, 0:512], krTa[0:65, kc], qrTa[s][0:65, :], False, True, ["krTa", f"qrTa{s}"], [pid])
                pi = ipt % NPT
                ipt += 1
                P.actf(PT[pi][:], pt[:, 0:512], AF.Exp, [pid], [f"PT{pi}"], scale=ATT_SCALE)
                for qs in range(4):
                    ot, oid = ops[qs]
                    P.mm(ot[:, 0:129], PT[pi][:, qs * 128:(qs + 1) * 128], vh[s][:, kt * 129:(kt + 1) * 129],
                         kt == 0, kt == NKT - 1, [f"PT{pi}", f"vh{s}"], [oid])
            for qs in range(4):
                ot, oid = ops[qs]
                P.op('dve', lambda g: g.reciprocal(out=rden[:, qs:qs + 1], in_=ot[:, 128:129]), reads=[oid], writes=["rden"])
                P.ts('dve', otok[:, qs, hh * 128:(hh + 1) * 128], ot[:, 0:128], rden[:, qs:qs + 1], None, ALU.mult, None,
                     [oid, "rden"], ["otok"])
        for qs in range(4):
            s2 = tcount % 2
            tcount += 1
            rows = slice(qb * 512 + qs * 128, qb * 512 + (qs + 1) * 128)
            P.load(f"ld_h{s2}", hs[s2][:], h_d[rows, :], f"hs{s2}")
            for kc in range(8):
                pt, pid = sring.get()
                P.tr(pt[:, 0:128], otok[:, qs, kc * 128:(kc + 1) * 128], ident[:], ["otok", "ident"], [pid])
                P.cp('act' if kc % 2 == 0 else 'dve', T[:, kc, :], pt[:, 0:128], [pid], ["T"])
            linear(P, sring, T, "T", wo, "wo", 8, D,
                   lambda nb, c0, ps, pid, n: P.tt('dve', A[:, c0:c0 + n], ps, gate[:, c0:c0 + n], ALU.mult, [pid, "gate"], ["A"]))
            P.stt(A[:], hs[s2][:], ALPHA, A[:], ALU.mult, ALU.add, [f"hs{s2}", "A"], ["A"])
            layernorm(P, A[:], "A", lng[:], lnb[:], O[s2][:], f"O{s2}", stats, mv)
            P.store(f"st_o{s2}", o_d[rows, :], O[s2][:], f"O{s2}")
    P.finish()
    return P


def run_attn(h_lat, q_l, kvu_l, kvu_c, kr_l, kr_c, mods, p):
    P = build_attn()
    ident = np.eye(128, dtype=np.float32)
    bf = ml_dtypes.bfloat16
    maps = []
    per_b = []
    for b in range(2):
        kvu = np.concatenate([kvu_l[b], kvu_c[b]], axis=0).reshape(NK, 8, 256)
        kT = np.ascontiguousarray(kvu[:, :, 0:128].transpose(1, 2, 0))
        krT = np.ascontiguousarray(np.concatenate([kr_l[b], kr_c[b]], axis=0).T)
        v = np.ones((8, 128, NKT, 129), dtype=bf)
        v[:, :, :, 0:128] = kvu[:, :, 128:256].reshape(NKT, 128, 8, 128).transpose(2, 1, 0, 3)
        per_b.append((kT, krT, np.ascontiguousarray(v.reshape(8, 128, NKT * 129))))
    for k in range(NCORES):
        b, j = k // 4, k % 4
        q = q_l[b, 2048 * j:2048 * (j + 1)].reshape(2048, 8, 192)
        maps.append({"qT": np.ascontiguousarray(q[:, :, 0:128].transpose(1, 2, 0)),
                     "qrT": np.ascontiguousarray(q[:, :, 128:192].transpose(1, 2, 0)),
                     "kT": per_b[b][0], "krT": per_b[b][1], "v": per_b[b][2],
                     "h": np.ascontiguousarray(h_lat[b, 2048 * j:2048 * (j + 1)]),
                     "gate": bc(mods[1, b, 2048:3072]), "wo": p['mla_w_o'][0],
                     "lng": bc(p['ln_g'][1, 0]), "lnb": bc(p['ln_b'][1, 0]), "ident": ident})
    res = P.run(maps)
    out = np.zeros((2, 8192, D), np.float32)
    for k in range(NCORES):
        b, j = k // 4, k % 4
        out[b, 2048 * j:2048 * (j + 1)] = res[k]["o"]
    return out


def kernel(x, c, ctx, c_ctx, ada_w, ada_b, ln_g, ln_b,
           s5_a_re, s5_a_im, s5_log_dt, s5_b_re, s5_b_im, s5_c_re, s5_c_im, s5_d, s5_w_glu, s5_w_o,
           mla_w_dq, mla_q_norm, mla_w_uq, mla_w_dkv, mla_kv_norm, mla_w_ukv, mla_w_o,
           peer_w_q, peer_keys, peer_u, peer_v):
    p = dict(ln_g=ln_g, ln_b=ln_b, s5_a_re=s5_a_re, s5_a_im=s5_a_im, s5_log_dt=s5_log_dt, s5_b_re=s5_b_re, s5_b_im=s5_b_im,
             s5_c_re=s5_c_re, s5_c_im=s5_c_im, s5_d=s5_d, s5_w_glu=s5_w_glu, s5_w_o=s5_w_o, mla_w_dq=mla_w_dq,
             mla_q_norm=mla_q_norm, mla_w_uq=mla_w_uq, mla_w_dkv=mla_w_dkv, mla_kv_norm=mla_kv_norm, mla_w_ukv=mla_w_ukv,
             mla_w_o=mla_w_o, peer_w_q=peer_w_q, peer_keys=peer_keys, peer_u=peer_u, peer_v=peer_v)
    p = {k: np.asarray(v, dtype=np.float32) for k, v in p.items()}
    x = np.asarray(x, np.float32); ctx = np.asarray(ctx, np.float32)
    mods = run_mods(np.asarray(c, np.float32), np.asarray(c_ctx, np.float32), np.asarray(ada_w, np.float32),
                    np.asarray(ada_b, np.float32))
    ys = run_s5(x, ctx, mods, p)
    h1_l, h1_c = run_s5out(x, ctx, ys, mods, p)
    h2_l, h2_c = run_peer(h1_l, h1_c, mods, 0, p, True)
    q_l, kvu_l, kvu_c, kr_l, kr_c = run_mlaproj(h2_l, h2_c, mods, p)
    h3_l = run_attn(h2_l, q_l, kvu_l, kvu_c, kr_l, kr_c, mods, p)
    h4_l, _ = run_peer(h3_l, h2_c, mods, 1, p, False)
    return h4_l.astype(np.float32)
```

```python
import math
import numpy as np
import ml_dtypes
from contextlib import ExitStack
import concourse.bass as bass
import concourse.mybir as mybir
from concourse.bass_utils import run_bass_kernel_spmd

F32 = mybir.dt.float32
BF16 = mybir.dt.bfloat16
U32 = mybir.dt.uint32
I32 = mybir.dt.int32
AF = mybir.ActivationFunctionType
ALU = mybir.AluOpType
AX = mybir.AxisListType

NCORES = 8
D = 1024
ALPHA = (2 * 2) ** 0.25
LN_EPS = 1e-5
RMS_EPS = 1e-6
NT = 17
TPC = NT * 128


class Prog:
    def __init__(self):
        self.nc = bass.Bass("TRN2", target_bir_lowering=False)
        self.es = ExitStack()
        self.semes = ExitStack()
        nc = self.nc
        self.eng = {'pe': nc.tensor, 'dve': nc.vector, 'act': nc.scalar, 'pool': nc.gpsimd, 'sp': nc.sync}
        self.sems, self.cnt = {}, {}
        self.seen = {e: {} for e in self.eng}
        self.lastw, self.readers = {}, {}
        for e in ('pe', 'dve', 'act', 'pool'):
            self._sem('E_' + e)
        self.npsum = 0

    def _sem(self, key):
        if key not in self.sems:
            self.sems[key] = self.semes.enter_context(self.nc.semaphore(key))
            self.cnt[key] = 0
        return self.sems[key]

    def din(self, name, shape, dt=F32):
        return self.nc.dram_tensor(name, list(shape), dt, kind="ExternalInput").ap()

    def dout(self, name, shape, dt=F32):
        return self.nc.dram_tensor(name, list(shape), dt, kind="ExternalOutput").ap()

    def sb(self, name, shape, dt=F32):
        return self.es.enter_context(self.nc.sbuf_tensor("sb_" + getattr(self, "pfx", "") + name, list(shape), dt))

    def scratch(self, name, shape, dt=F32):
        return self.nc.dram_tensor(name, list(shape), dt).ap()

    def scope(self, pfx):
        prog = self

        class _S:
            def __enter__(s_):
                s_.old = (prog.es, getattr(prog, "pfx", ""))
                prog.es = ExitStack()
                prog.pfx = pfx + "_"
                return prog

            def __exit__(s_, *a):
                prog.barrier()
                prog.es.close()
                prog.es, prog.pfx = s_.old
                return False
        return _S()

    def barrier(self):
        for e in self.eng:
            for k, v in self.cnt.items():
                if v > 0 and self.seen[e].get(k, 0) < v:
                    self.eng[e].wait_ge(self.sems[k], v)
                    self.seen[e][k] = v

    def ps(self, name, shape, dt=F32):
        return self.es.enter_context(self.nc.psum_tensor(name, list(shape), dt))

    def _deps(self, reads, writes):
        deps = {}

        def add(k, v):
            if not k.startswith('E_'):
                v = max(v, self.cnt[k])
            if deps.get(k, 0) < v:
                deps[k] = v
        for b in reads:
            if b in self.lastw:
                add(*self.lastw[b])
        for b in writes:
            if b in self.lastw:
                add(*self.lastw[b])
            for k, v in self.readers.get(b, {}).items():
                add(k, v)
        return deps

    def _wait(self, e, deps, skip=None):
        for k, v in deps.items():
            if k == skip or self.seen[e].get(k, 0) >= v:
                continue
            self.eng[e].wait_ge(self.sems[k], v)
            self.seen[e][k] = v

    def _commit(self, k, v, reads, writes):
        for b in writes:
            self.lastw[b] = (k, v)
            self.readers[b] = {}
        for b in reads:
            r = self.readers.setdefault(b, {})
            if r.get(k, 0) < v:
                r[k] = v

    def op(self, e, fn, reads=(), writes=()):
        key = 'E_' + e
        self._wait(e, self._deps(reads, writes), skip=key if e == 'pe' else None)
        ins = fn(self.eng[e])
        self.cnt[key] += 1
        ins.then_inc(self.sems[key], 1)
        self._commit(key, self.cnt[key], reads, writes)

    def dma(self, q, semkey, fn, reads=(), writes=()):
        self._sem(semkey)
        self._wait(q, self._deps(reads, writes))
        ins = fn(self.eng[q])
        self.cnt[semkey] += 16
        ins.then_inc(self.sems[semkey], 16)
        self._commit(semkey, self.cnt[semkey], reads, writes)

    def finish(self):
        for k, v in self.cnt.items():
            if v > 0 and self.seen['sp'].get(k, 0) < v:
                self.nc.sync.wait_ge(self.sems[k], v)

    def run(self, in_maps):
        res = run_bass_kernel_spmd(self.nc, in_maps, core_ids=list(range(NCORES)))
        return res.results

    def load(self, semkey, dst, src, wid, rid=None, q='sp'):
        self.dma(q, semkey, lambda e: e.dma_start(out=dst, in_=src), reads=(rid,) if rid else (), writes=(wid,))

    def store(self, semkey, dst, src, rid, wid=None, q='sp'):
        self.dma(q, semkey, lambda e: e.dma_start(out=dst, in_=src), reads=(rid,), writes=(wid,) if wid else ())

    def tt(self, e, out, a, b, op, r, w):
        self.op(e, lambda g: g.tensor_tensor(out=out, in0=a, in1=b, op=op), reads=r, writes=w)

    def ts(self, e, out, a, s1, s2, op0, op1, r, w):
        if op1 is None:
            self.op(e, lambda g: g.tensor_scalar(out=out, in0=a, scalar1=s1, scalar2=None, op0=op0), reads=r, writes=w)
        else:
            self.op(e, lambda g: g.tensor_scalar(out=out, in0=a, scalar1=s1, scalar2=s2, op0=op0, op1=op1), reads=r, writes=w)

    def stt(self, out, a, s, b, op0, op1, r, w):
        self.op('dve', lambda g: g.scalar_tensor_tensor(out=out, in0=a, scalar=s, in1=b, op0=op0, op1=op1), reads=r, writes=w)

    def actf(self, out, a, func, r, w, bias=None, scale=None, accum=None):
        kw = {}
        if bias is not None:
            kw['bias'] = bias
        if scale is not None:
            kw['scale'] = scale
        if accum is not None:
            kw['accum_out'] = accum
        self.op('act', lambda g: g.activation(out=out, in_=a, func=func, **kw), reads=r, writes=w)

    def cp(self, e, out, a, r, w):
        if e == 'act':
            self.actf(out, a, AF.Copy, r, w)
        else:
            self.op(e, lambda g: g.tensor_copy(out=out, in_=a), reads=r, writes=w)

    def mm(self, out, lhsT, rhs, start, stop, r, w):
        self.op('pe', lambda g: g.matmul(out, lhsT, rhs, start=start, stop=stop), reads=r, writes=w)

    def tr(self, out, in_, ident, r, w):
        self.op('pe', lambda g: g.transpose(out, in_, ident), reads=r, writes=w)


class PsumRing:
    def __init__(self, P, banks, dt=F32):
        if not hasattr(P, "banks"):
            P.banks = {}
        for i in banks:
            if i not in P.banks:
                P.banks[i] = P.ps(f"psb{i}", [128, 512], dt)
        self.b = list(banks)
        self.P = P
        self.i = 0

    def get(self):
        j = self.b[self.i]
        self.i = (self.i + 1) % len(self.b)
        return self.P.banks[j], f"psb{j}"


NTB = 66
NROW = NTB * 128
SEQ_T = 256 + 8192
CH = 512
TWO_PI = 2.0 * math.pi
MAGIC = 12582912.0
NEG = -1.0e30
NK = 8448
NKT = 66
ATT_SCALE = 192.0 ** -0.5


def sincos(P, th, n, tmp, sin_out, cos_out, ids):
    for which, outt in ((0, sin_out), (1, cos_out)):
        x, u, k, y, m = (tmp[:, j, :n] for j in range(5))
        r = list(ids)
        if which == 0:
            P.cp('dve', x, th, r, r)
        else:
            P.ts('dve', x, th, math.pi / 2, None, ALU.add, None, r, r)
        P.ts('dve', u, x, 1.0 / TWO_PI, None, ALU.mult, None, r, r)
        P.ts('dve', k, u, MAGIC, None, ALU.add, None, r, r)
        P.ts('dve', k, k, MAGIC, None, ALU.subtract, None, r, r)
        P.stt(y, k, -TWO_PI, x, ALU.mult, ALU.add, r, r)
        P.ts('dve', m, y, math.pi, None, ALU.is_gt, None, r, r)
        P.stt(y, m, -TWO_PI, y, ALU.mult, ALU.add, r, r)
        P.ts('dve', m, y, -math.pi, None, ALU.is_lt, None, r, r)
        P.stt(y, m, TWO_PI, y, ALU.mult, ALU.add, r, r)
        P.ts('dve', y, y, math.pi, -math.pi, ALU.min, ALU.max, r, r)
        P.actf(outt, y, AF.Sin, r, r)


def transpose_tile(P, ring, src, sid, dst, did, nk, ident):
    for kc in range(nk):
        pt, pid = ring.get()
        P.tr(pt[:, 0:128], src[:, kc * 128:(kc + 1) * 128], ident[:], [sid, "ident"], [pid])
        P.cp('act' if kc % 2 == 0 else 'dve', dst[:, kc, :], pt[:, 0:128], [pid], [did])


def linear(P, ring, xT, xid, W, wid, nk, N, cb, bs=512):
    nb = 0
    c0 = 0
    while c0 < N:
        n = min(bs, N - c0)
        pt, pid = ring.get()
        for kc in range(nk):
            P.mm(pt[:, 0:n], xT[:, kc, :], W[:, kc, c0:c0 + n], kc == 0, kc == nk - 1, [xid, wid], [pid])
        cb(nb, c0, pt[:, 0:n], pid, n)
        c0 += n
        nb += 1


def layernorm(P, x, xid, gbc, bbc, out, oid, stats, mv):
    for c in range(2):
        P.op('dve', lambda g: g.bn_stats(out=stats[:, c, :], in_=x[:, c * 512:(c + 1) * 512]), reads=[xid], writes=["lnst"])
    P.op('dve', lambda g: g.bn_aggr(out=mv[:, 0:2], in_=stats[:].rearrange("p a b -> p (a b)")), reads=["lnst"], writes=["lnmv"])
    P.ts('dve', mv[:, 2:3], mv[:, 1:2], LN_EPS, None, ALU.add, None, ["lnmv"], ["lnmv"])
    P.actf(mv[:, 2:3], mv[:, 2:3], AF.Sqrt, ["lnmv"], ["lnmv"])
    P.op('dve', lambda g: g.reciprocal(out=mv[:, 3:4], in_=mv[:, 2:3]), reads=["lnmv"], writes=["lnmv"])
    P.ts('dve', x, x, mv[:, 0:1], mv[:, 3:4], ALU.subtract, ALU.mult, [xid, "lnmv"], [xid])
    P.tt('pool', x, x, gbc, ALU.mult, [xid, "lng"], [xid])
    P.tt('pool', out, x, bbc, ALU.add, [xid, "lnb"], [oid])


def rmsnorm(P, x, xid, n, gbc, gid, out, oid, junk, ss):
    P.actf(junk, x, AF.Square, [xid], ["rjunk"], accum=ss[:, 0:1])
    P.actf(ss[:, 3:4], junk[:, 0:1], AF.Copy, ["rjunk"], ["rjunk"])
    P.ts('dve', ss[:, 1:2], ss[:, 0:1], 1.0 / n, RMS_EPS, ALU.mult, ALU.add, ["rjunk"], ["rss"])
    P.actf(ss[:, 1:2], ss[:, 1:2], AF.Sqrt, ["rss"], ["rss"])
    P.op('dve', lambda g: g.reciprocal(out=ss[:, 2:3], in_=ss[:, 1:2]), reads=["rss"], writes=["rss"])
    P.stt(out, x, ss[:, 2:3], gbc, ALU.mult, ALU.mult, [xid, "rss", gid], [oid])


def rope(P, x1, x2, cosb, sinb, o1, o2, t, rid, wid):
    P.tt('dve', t[0], x1, cosb, ALU.mult, rid, ["ropet"])
    P.tt('dve', t[1], x2, sinb, ALU.mult, rid, ["ropet"])
    P.tt('dve', t[2], x2, cosb, ALU.mult, rid, ["ropet"])
    P.tt('dve', t[3], x1, sinb, ALU.mult, rid, ["ropet"])
    P.tt('dve', o1, t[0], t[1], ALU.subtract, ["ropet"], wid)
    P.tt('dve', o2, t[2], t[3], ALU.add, ["ropet"], wid)


def emit_mods(P, cT_d, aw_d, abB_d, modsB):
    with P.scope("md"):
        s = P.sb("s", [128, 8, 2]); srep = P.sb("srep", [128, 8, 2, 128])
        P.load("ld_c", s[:], cT_d, "s")
        P.actf(s[:], s[:], AF.Silu, ["s"], ["s"])
        P.cp('dve', srep[:], s[:].unsqueeze(3).to_broadcast([128, 8, 2, 128]), ["s"], ["srep"])
        w = [P.sb(f"w{i}", [128, 8, 512]) for i in range(2)]
        bb = [P.sb(f"bb{i}", [128, 512]) for i in range(2)]
        ot = [[P.sb(f"ot{v}{i}", [128, 512]) for i in range(2)] for v in range(2)]
        ring = PsumRing(P, range(8))
        it = 0
        for layer in range(2):
            for nb in range(12):
                sl = it % 2
                it += 1
                cols = slice(nb * 512, (nb + 1) * 512)
                P.load(f"ld_w{sl}", w[sl][:], aw_d[layer, :, cols].rearrange("(kc p) n -> p kc n", p=128), f"w{sl}")
                P.load(f"ld_b{sl}", bb[sl][:], abB_d[:, layer, cols], f"bb{sl}")
                for v in range(2):
                    pt, pid = ring.get()
                    for kc in range(8):
                        P.mm(pt[:, 0:512], srep[:, kc, v, :], w[sl][:, kc, :], kc == 0, kc == 7, ["srep", f"w{sl}"], [pid])
                    P.tt('dve', ot[v][sl][:], pt[:, 0:512], bb[sl][:], ALU.add, [pid, f"bb{sl}"], [f"ot{v}{sl}"])
                    P.store(f"st_m{v}{sl}", modsB[layer, v, :, cols], ot[v][sl][:], f"ot{v}{sl}")


def emit_s5(P, I, YF, YB):
    with P.scope("s5"):
        ident = P.sb("ident", [128, 128]); s = P.sb("s", [128, 8, 2])
        P.load("ld_par", ident[:], I["ident"], "ident")
        P.load("ld_par", s[:], I["cT"], "s")
        P.actf(s[:], s[:], AF.Silu, ["s"], ["s"])
        abT = P.sb("abT", [128, 16]); P.load("ld_par", abT[:], I["abT"], "abT")
        wsl = P.sb("wsl", [128, 2, 8, 128])
        sc1 = P.sb("sc1", [128, 2]); sh = P.sb("sh", [128, 2]); dv = P.sb("dv", [128, 1])
        are = P.sb("are", [128, 8]); aim = P.sb("aim", [128, 8]); ldt = P.sb("ldt", [128, 8])
        bre = P.sb("bre", [128, 8, 16]); bim = P.sb("bim", [128, 8, 16])
        cre = P.sb("cre", [128, 8, 16]); cim = P.sb("cim", [128, 8, 16])
        prm = P.sb("prm", [128, 16, 8]); tmp5 = P.sb("tmp5", [128, 5, 8])
        Wre = P.sb("Wre", [128, 8, 128]); Wim = P.sb("Wim", [128, 8, 128])
        Cre = P.sb("Cre", [128, 8, 128]); nCre = P.sb("nCre", [128, 8, 128]); nCim = P.sb("nCim", [128, 8, 128])
        bfull = P.sb("bfull", [128, 2, 128]); tb = P.sb("tb", [128, 2, 16])
        ctab = P.sb("ctab", [128, 8, CH]); stab = P.sb("stab", [128, 8, CH]); rB = P.sb("rB", [128, 8, CH])
        ones = P.sb("ones", [128, CH]); En = P.sb("En", [128, 8, 2, 2]); cur = P.sb("cur", [128, 4]); st = P.sb("st", [128, 4, 2])
        xs = [P.sb(f"xs{i}", [128, CH]) for i in range(2)]
        ms = [P.sb(f"ms{i}", [128, CH]) for i in range(2)]
        ys = [P.sb(f"ys{i}", [128, CH]) for i in range(2)]
        yt = [P.sb(f"yt{i}", [128, 4, 128]) for i in range(2)]
        NW = 3
        va = [P.sb(f"va{i}", [128, 4, CH]) for i in range(NW)]
        G = [P.sb(f"G{i}", [128, 2, CH]) for i in range(NW)]
        X = [P.sb(f"X{i}", [128, 4, CH]) for i in range(NW)]
        ring = PsumRing(P, range(2, 6)); yring = PsumRing(P, range(0, 2)); trring = PsumRing(P, range(6, 8))
        P.op('dve', lambda g: g.memset(ones[:], 1.0), writes=["ones"])
        R = ["prm"]
        it = 0
        wk = 0
        for fc in range(8):
            for dst, key, nm in ((are, "a_re", "are"), (aim, "a_im", "aim"), (ldt, "ldt", "ldt"), (bre, "bre", "bre"),
                                 (bim, "bim", "bim"), (cre, "cre", "cre"), (cim, "cim", "cim"), (dv, "dvec", "dv")):
                P.load("ld_par", dst[:], I[key][fc], nm)
            for which in range(2):
                c0 = which * 1024 + fc * 128
                P.load("ld_wsl", wsl[:, which], I["aw"][0, :, c0:c0 + 128].rearrange("(kc p) n -> p kc n", p=128), "wsl")
            for which, dstt, addc in ((0, sh, 0.0), (1, sc1, 1.0)):
                pt, pid = ring.get()
                for kc in range(8):
                    P.mm(pt[:, 0:2], wsl[:, which, kc, :], s[:, kc, :], kc == 0, kc == 7, ["wsl", "s"], [pid])
                P.ts('dve', dstt[:], pt[:, 0:2], abT[:, which * 8 + fc:which * 8 + fc + 1], addc, ALU.add, ALU.add,
                     [pid, "abT"], ["scsh"])
            dt_, mag, th, sn, cs, abr, abi, den, fre, fim, t0, t1 = (prm[:, j, :] for j in range(12))
            P.actf(dt_, ldt[:], AF.Exp, ["ldt"], R)
            P.tt('dve', t0, are[:], dt_, ALU.mult, ["are"] + R, R)
            P.actf(mag, t0, AF.Exp, R, R)
            P.tt('dve', th, aim[:], dt_, ALU.mult, ["aim"] + R, R)
            sincos(P, th, 8, tmp5, sn, cs, R + ["tmp5"])
            P.tt('dve', abr, mag, cs, ALU.mult, R, R)
            P.tt('dve', abi, mag, sn, ALU.mult, R, R)
            P.tt('dve', den, are[:], are[:], ALU.mult, ["are"] + R, R)
            P.tt('dve', t0, aim[:], aim[:], ALU.mult, ["aim"] + R, R)
            P.tt('dve', den, den, t0, ALU.add, R, R)
            P.op('dve', lambda g: g.reciprocal(out=den, in_=den), reads=R, writes=R)
            P.ts('dve', t0, abr, -1.0, None, ALU.add, None, R, R)
            P.tt('dve', fre, t0, are[:], ALU.mult, ["are"] + R, R)
            P.tt('dve', t1, abi, aim[:], ALU.mult, ["aim"] + R, R)
            P.tt('dve', fre, fre, t1, ALU.add, R, R)
            P.tt('dve', fre, fre, den, ALU.mult, R, R)
            P.tt('dve', fim, abi, are[:], ALU.mult, ["are"] + R, R)
            P.tt('dve', t1, t0, aim[:], ALU.mult, ["aim"] + R, R)
            P.tt('dve', fim, fim, t1, ALU.subtract, R, R)
            P.tt('dve', fim, fim, den, ALU.mult, R, R)
            for j in range(8):
                pair = j % 4
                for which, Wdst in ((0, Wre), (1, Wim)):
                    P.op('dve', lambda g: g.memset(bfull[:, which, :], 0.0), writes=["bfull"])
                    if which == 0:
                        P.ts('dve', tb[:, 0, :], bim[:, j, :], fim[:, j:j + 1], None, ALU.mult, None, ["bim"] + R, ["tb"])
                        src2, op1 = bre, ALU.subtract
                    else:
                        P.ts('dve', tb[:, 0, :], bre[:, j, :], fim[:, j:j + 1], None, ALU.mult, None, ["bre"] + R, ["tb"])
                        src2, op1 = bim, ALU.add
                    P.stt(tb[:, 1, :], src2[:, j, :], fre[:, j:j + 1], tb[:, 0, :], ALU.mult, op1, ["bre", "bim", "tb"] + R, ["tb"])
                    for g2 in range(2):
                        c0 = 32 * pair + 16 * g2
                        P.cp('dve', bfull[64 * g2:64 * g2 + 64, which, c0:c0 + 16], tb[64 * g2:64 * g2 + 64, 1, :], ["tb"], ["bfull"])
                    pt, pid = ring.get()
                    P.tr(pt[:, 0:128], bfull[:, which, :], ident[:], ["bfull", "ident"], [pid])
                    P.cp('act', Wdst[:, j, :], pt[:, 0:128], [pid], ["W"])
                for srcc, dsts in ((cre, (Cre, nCre)), (cim, (None, nCim))):
                    for dd, sgn in zip(dsts, (1.0, -1.0)):
                        if dd is None:
                            continue
                        P.op('dve', lambda g: g.memset(dd[:, j, :], 0.0), writes=["W"])
                        for g2 in range(2):
                            c0 = 32 * pair + 16 * g2
                            P.ts('dve', dd[64 * g2:64 * g2 + 64, j, c0:c0 + 16], srcc[64 * g2:64 * g2 + 64, j, :], sgn, None,
                                 ALU.mult, None, ["cre", "cim"], ["W"])
                P.op('dve', lambda g: g.memset(ctab[:, j, 0:1], 1.0), writes=["tab"])
                P.op('dve', lambda g: g.memset(stab[:, j, 0:1], 0.0), writes=["tab"])
                P.cp('dve', cur[:, 0:1], cs[:, j:j + 1], R, ["cur"])
                P.cp('dve', cur[:, 1:2], sn[:, j:j + 1], R, ["cur"])
                n = 1
                while n <= CH:
                    if n in (256, 512):
                        wi = 0 if n == 256 else 1
                        P.cp('dve', En[:, j, wi, :], cur[:, 0:2], ["cur"], ["En"])
                    if n == CH:
                        break
                    cn, snn = cur[:, 0:1], cur[:, 1:2]
                    T = ["tab", "cur", "rB"]
                    P.ts('dve', rB[:, j, 0:n], stab[:, j, 0:n], snn, None, ALU.mult, None, T, ["rB"])
                    P.stt(ctab[:, j, n:2 * n], ctab[:, j, 0:n], cn, rB[:, j, 0:n], ALU.mult, ALU.subtract, T, ["tab"])
                    P.ts('dve', rB[:, j, 0:n], ctab[:, j, 0:n], snn, None, ALU.mult, None, T, ["rB"])
                    P.stt(stab[:, j, n:2 * n], stab[:, j, 0:n], cn, rB[:, j, 0:n], ALU.mult, ALU.add, T, ["tab"])
                    P.tt('dve', cur[:, 2:3], cn, cn, ALU.mult, ["cur"], ["cur"])
                    P.tt('dve', cur[:, 3:4], snn, snn, ALU.mult, ["cur"], ["cur"])
                    P.tt('dve', cur[:, 3:4], cur[:, 2:3], cur[:, 3:4], ALU.subtract, ["cur"], ["cur"])
                    P.tt('dve', cur[:, 2:3], cn, snn, ALU.mult, ["cur"], ["cur"])
                    P.ts('dve', cur[:, 1:2], cur[:, 2:3], 2.0, None, ALU.mult, None, ["cur"], ["cur"])
                    P.cp('dve', cur[:, 0:1], cur[:, 3:4], ["cur"], ["cur"])
                    n *= 2
                P.ts('dve', rB[:, j, :], ones[:], mag[:, j:j + 1], None, ALU.mult, None, ["ones", "rB"] + R, ["rB"])
            for d_ in range(2):
                Yd = YF if d_ == 0 else YB
                P.op('dve', lambda g: g.memset(st[:], 0.0), writes=["st"])
                items = [(ci, pair) for ci in range(17) for pair in range(4)]
                cinfo = {}

                def chunk_setup(ci):
                    nonlocal it
                    n = 256 if ci == 0 else 512
                    if ci == 0:
                        t0_, row0, mcol = 0, 8192, 1
                    elif d_ == 0:
                        t0_, row0, mcol = 256 + 512 * (ci - 1), 512 * (ci - 1), 0
                    else:
                        t0_, row0, mcol = 256 + 8192 - 512 * ci, 8192 - 512 * ci, 0
                    sl = it % 2
                    it += 1
                    P.load(f"ld_x{sl}", xs[sl][:, :n], I["xT"][fc, :, t0_:t0_ + n], f"xs{sl}")
                    xin = xs[sl][:, :n] if d_ == 0 else xs[sl][:, n - 1::-1]
                    P.ts('dve', ms[sl][:, :n], xin, sc1[:, mcol:mcol + 1], sh[:, mcol:mcol + 1], ALU.mult, ALU.add,
                         [f"xs{sl}", "scsh"], [f"ms{sl}"])
                    yp, yid = yring.get()
                    cinfo[ci] = (n, row0, sl, yp, yid)

                def drive(ci, pair):
                    n, row0, sl, yp, yid = cinfo[ci]
                    j = d_ * 4 + pair
                    pa, aid = ring.get()
                    pb, bid = ring.get()
                    P.mm(pa[:, :n], Wre[:, j, :], ms[sl][:, :n], True, True, ["W", f"ms{sl}"], [aid])
                    P.mm(pb[:, :n], Wim[:, j, :], ms[sl][:, :n], True, True, ["W", f"ms{sl}"], [bid])
                    return pa, aid, pb, bid

                chunk_setup(0)
                pend = drive(0, 0)
                for ii, (ci, pair) in enumerate(items):
                    n, row0, sl, yp, yid = cinfo[ci]
                    pa, aid, pb, bid = pend
                    if ii + 1 < len(items):
                        nci, npair = items[ii + 1]
                        if npair == 0:
                            chunk_setup(nci)
                        pend = drive(nci, npair)
                    j = d_ * 4 + pair
                    w_ = wk % NW
                    wk += 1
                    c_, s_ = ctab[:, j, :n], stab[:, j, :n]
                    V, VI = va[w_], f"va{w_}"
                    P.tt('dve', V[:, 0, :n], pa[:, :n], c_, ALU.mult, [aid, "tab"], [VI])
                    P.tt('dve', V[:, 1, :n], pb[:, :n], s_, ALU.mult, [bid, "tab"], [VI])
                    P.tt('dve', V[:, 0, :n], V[:, 0, :n], V[:, 1, :n], ALU.add, [VI], [VI])
                    P.tt('dve', V[:, 2, :n], pb[:, :n], c_, ALU.mult, [bid, "tab"], [VI])
                    P.tt('dve', V[:, 3, :n], pa[:, :n], s_, ALU.mult, [aid, "tab"], [VI])
                    P.tt('dve', V[:, 2, :n], V[:, 2, :n], V[:, 3, :n], ALU.subtract, [VI], [VI])
                    GG, GI = G[w_], f"G{w_}"
                    P.op('dve', lambda g: g.tensor_tensor_scan(out=GG[:, 0, :n], data0=rB[:, j, :n], data1=V[:, 0, :n],
                                                               initial=st[:, pair, 0:1], op0=ALU.mult, op1=ALU.add),
                         reads=["rB", VI, "st"], writes=[GI])
                    P.op('dve', lambda g: g.tensor_tensor_scan(out=GG[:, 1, :n], data0=rB[:, j, :n], data1=V[:, 2, :n],
                                                               initial=st[:, pair, 1:2], op0=ALU.mult, op1=ALU.add),
                         reads=["rB", VI, "st"], writes=[GI])
                    wi = 0 if n == 256 else 1
                    cn, snn = En[:, j, wi, 0:1], En[:, j, wi, 1:2]
                    P.ts('dve', cur[:, 0:1], GG[:, 1, n - 1:n], snn, None, ALU.mult, None, [GI, "En"], ["cur"])
                    P.stt(st[:, pair, 0:1], GG[:, 0, n - 1:n], cn, cur[:, 0:1], ALU.mult, ALU.subtract, [GI, "En", "cur"], ["st"])
                    P.ts('dve', cur[:, 1:2], GG[:, 0, n - 1:n], snn, None, ALU.mult, None, [GI, "En"], ["cur"])
                    P.stt(st[:, pair, 1:2], GG[:, 1, n - 1:n], cn, cur[:, 1:2], ALU.mult, ALU.add, [GI, "En", "cur"], ["st"])
                    XX, XI = X[w_], f"X{w_}"
                    P.tt('pool', XX[:, 0, :n], GG[:, 0, :n], c_, ALU.mult, [GI, "tab"], [XI])
                    P.tt('pool', XX[:, 1, :n], GG[:, 1, :n], s_, ALU.mult, [GI, "tab"], [XI])
                    P.tt('pool', XX[:, 2, :n], GG[:, 1, :n], c_, ALU.mult, [GI, "tab"], [XI])
                    P.tt('pool', XX[:, 3, :n], GG[:, 0, :n], s_, ALU.mult, [GI, "tab"], [XI])
                    for q_, Wm in enumerate((Cre, nCre, nCim, nCim)):
                        P.mm(yp[:, :n], Wm[:, j, :], XX[:, q_, :n], pair == 0 and q_ == 0, pair == 3 and q_ == 3,
                             ["W", XI], [yid])
                    if pair == 3:
                        if d_ == 0:
                            P.stt(ys[sl][:, :n], ms[sl][:, :n], dv[:, 0:1], yp[:, :n], ALU.mult, ALU.add, [f"ms{sl}", "dv", yid], [f"ys{sl}"])
                        else:
                            P.cp('dve', ys[sl][:, :n], yp[:, n - 1::-1], [yid], [f"ys{sl}"])
                        nblk = n // 128
                        for bk in range(nblk):
                            pt, pid = trring.get()
                            P.tr(pt[:, 0:128], ys[sl][:, bk * 128:(bk + 1) * 128], ident[:], [f"ys{sl}", "ident"], [pid])
                            P.cp('act', yt[sl][:, bk, :], pt[:, 0:128], [pid], [f"yt{sl}"])
                        P.store(f"st_y{sl}", Yd[row0:row0 + n, fc * 128:(fc + 1) * 128].rearrange("(k p) f -> p k f", p=128),
                                yt[sl][:, 0:nblk, :], f"yt{sl}")


def emit_s5out(P, I, modsB, YF, YB, H1):
    nt = NTB
    with P.scope("so"):
        ident = P.sb("ident", [128, 128]); gL = P.sb("gL", [128, D]); gC = P.sb("gC", [128, D])
        wg = P.sb("wg", [128, 8, D]); wo = P.sb("wo", [128, 8, D]); lng = P.sb("lng", [128, D]); lnb = P.sb("lnb", [128, D])
        P.load("ld_c", ident[:], I["ident"], "ident")
        P.load("ld_c", gL[:], modsB[0, 0, :, 2048:3072], "gL"); P.load("ld_c", gC[:], modsB[0, 1, :, 2048:3072], "gC")
        P.load("ld_c", lng[:], I["lng"][0], "lng"); P.load("ld_c", lnb[:], I["lnb"][0], "lnb")
        P.load("ld_wg", wg[:], I["wglu"].rearrange("(kc p) n -> p kc n", p=128), "wg")
        P.load("ld_wo", wo[:], I["wo_s5"].rearrange("(kc p) n -> p kc n", p=128), "wo")
        xs = [P.sb(f"x{i}", [128, D]) for i in range(2)]
        ya = [P.sb(f"ya{i}", [128, D]) for i in range(2)]
        yb_ = [P.sb(f"yb{i}", [128, D]) for i in range(2)]
        A = P.sb("A", [128, D]); B = P.sb("B", [128, D]); C = P.sb("C", [128, D]); T = P.sb("T", [128, 8, 128])
        O = [P.sb(f"O{i}", [128, D]) for i in range(2)]
        stats = P.sb("stats", [128, 2, 6]); mv = P.sb("mv", [128, 4])
        tring = PsumRing(P, range(0, 4)); lring = PsumRing(P, range(4, 8))
        for t in range(nt):
            s = t % 2
            rows = slice(t * 128, (t + 1) * 128)
            P.load(f"ld_x{s}", xs[s][:], I["x_tok"][rows, :], f"x{s}")
            P.load(f"ld_ya{s}", ya[s][:], YF[rows, :], f"ya{s}")
            P.load(f"ld_yb{s}", yb_[s][:], YB[rows, :], f"yb{s}")
            gate, gid = (gC, "gC") if t >= 64 else (gL, "gL")
            P.tt('dve', A[:], ya[s][:], yb_[s][:], ALU.add, [f"ya{s}", f"yb{s}"], ["A"])
            P.actf(B[:], A[:], AF.Gelu, ["A"], ["B"])
            transpose_tile(P, tring, B, "B", T, "T", 8, ident)
            linear(P, lring, T, "T", wg, "wg", 8, D,
                   lambda nb, c0, ps, pid, n: P.actf(C[:, c0:c0 + n], ps, AF.Sigmoid, [pid], ["C"]))
            P.tt('dve', C[:], C[:], B[:], ALU.mult, ["B", "C"], ["C"])
            transpose_tile(P, tring, C, "C", T, "T", 8, ident)
            linear(P, lring, T, "T", wo, "wo", 8, D,
                   lambda nb, c0, ps, pid, n: P.tt('dve', A[:, c0:c0 + n], ps, gate[:, c0:c0 + n], ALU.mult, [pid, gid], ["A"]))
            P.stt(A[:], xs[s][:], ALPHA, A[:], ALU.mult, ALU.add, [f"x{s}", "A"], ["A"])
            layernorm(P, A[:], "A", lng[:], lnb[:], O[s][:], f"O{s}", stats, mv)
            P.store(f"st_o{s}", H1[rows, :], O[s][:], f"O{s}")


def emit_peer(P, I, modsB, layer, Hin, Hout, nt, pfx, uv_d):
    with P.scope(pfx):
        ident = P.sb("ident", [128, 128]); mL = P.sb("mL", [128, 3, D]); mC = P.sb("mC", [128, 3, D])
        wq = P.sb("wq", [128, 8, D]); kbd = P.sb("kbd", [128, 8, 256]); lng = P.sb("lng", [128, D]); lnb = P.sb("lnb", [128, D])
        iota = P.sb("iota", [128, 16])
        P.load("ld_c", ident[:], I["ident"], "ident")
        P.load("ld_c", mL[:], modsB[layer, 0, :, 3072:6144].rearrange("p (m d) -> p m d", d=D), "mL")
        P.load("ld_c", mC[:], modsB[layer, 1, :, 3072:6144].rearrange("p (m d) -> p m d", d=D), "mC")
        P.load("ld_c", kbd[:], I["kbd"][layer], "kbd")
        P.load("ld_c", lng[:], I["lng"][2 * layer + 1], "lng"); P.load("ld_c", lnb[:], I["lnb"][2 * layer + 1], "lnb")
        P.load("ld_c", iota[:], I["iota16"], "iota")
        P.load("ld_wq", wq[:], I["peer_wq"][layer].rearrange("(kc p) n -> p kc n", p=128), "wq")
        P.ts('dve', mL[:, 1, :], mL[:, 1, :], 1.0, None, ALU.add, None, ["mL"], ["mL"])
        P.ts('dve', mC[:, 1, :], mC[:, 1, :], 1.0, None, ALU.add, None, ["mC"], ["mC"])
        hs = [P.sb("h0", [128, D])] * 2
        identb = P.sb("identb", [128, 128], BF16)
        Pk = [P.sb(f"Pk{i}", [128, D], BF16) for i in range(4)]
        M = P.sb("M", [128, D]); T = P.sb("T", [128, 8, 128]); Q = P.sb("Q", [128, D])
        sc = P.sb("sc", [128, 16, 128])
        sv = P.sb("sv", [128, 16, 16]); si = P.sb("si", [128, 16, 16], U32); sif = P.sb("sif", [128, 16, 16])

        cv = P.sb("cv", [128, 8, 16]); ci = P.sb("ci", [128, 8, 16], U32); cab = P.sb("cab", [128, 2, 8, 16], U32)
        cabf = P.sb("cabf", [128, 2, 8, 16]); OH = sc[:].rearrange("p a b -> p (a b)").rearrange("p (h c) -> p h c", c=256)
        i12 = P.sb("i12", [128, 2, 8, 16]); ef = P.sb("ef", [128, 128]); eidx = P.sb("eidx", [128, 128], U32)
        gt = P.sb("gt", [128, 8, 16]); gs = P.sb("gs", [128, 8]); dots = P.sb("dots", [128, 128]); actv = P.sb("actv", [128, 128])
        NS = 8
        uvg = [P.sb(f"uvg{i}", [128, 2 * D], BF16) for i in range(NS)]
        junk = Q
        acc = P.sb("acc", [128, D])
        sc2t = P.sb("sc2", [128, 16, 128]); candt = P.sb("cand", [128, 8, 256])
        sc2 = sc2t[:]
        cand2 = sc2t[:].rearrange("p a b -> p (a b)").rearrange("p (h c) -> p h c", c=256)
        cand = candt[:]
        O = [P.sb("O0", [128, D])] * 2
        stats = P.sb("stats", [128, 2, 6]); mv = P.sb("mv", [128, 4])
        tring = PsumRing(P, range(0, 4)); lring = PsumRing(P, range(4, 6)); aring = PsumRing(P, range(6, 8))
        P.cp('dve', identb[:], ident[:], ["ident"], ["identb"])
        pa = [P.banks[6], P.banks[7]]
        sv4 = sv[:].rearrange("p (h s) k -> p h s k", s=2)
        sif4 = sif[:].rearrange("p (h s) k -> p h s k", s=2)
        B4 = [128, 8, 16, 16]
        for t in range(nt):
            s = t % 2
            rows = slice(t * 128, (t + 1) * 128)
            mod, mid = (mC, "mC") if t >= 64 else (mL, "mL")
            P.load("ld_h0", hs[s][:], Hin[rows, :], "h0")
            P.tt('dve', M[:], hs[s][:], mod[:, 1, :], ALU.mult, ["h0", mid], ["M"])
            P.tt('dve', M[:], M[:], mod[:, 0, :], ALU.add, ["M", mid], ["M"])
            transpose_tile(P, tring, M, "M", T, "T", 8, ident)
            linear(P, lring, T, "T", wq, "wq", 8, D,
                   lambda nb, c0, ps, pid, n: P.cp('act', Q[:, c0:c0 + n], ps, [pid], ["Q"]))
            transpose_tile(P, tring, Q, "Q", T, "T", 8, ident)
            for hh in range(8):
                pt, pid = lring.get()
                P.mm(pt[:, 0:256], T[:, hh, :], kbd[:, hh, :], True, True, ["T", "kbd"], [pid])
                P.cp('act', sc[:, 2 * hh:2 * hh + 2, :], pt[:, 0:256].rearrange("p (s n) -> p s n", s=2), [pid], ["sc"])
            for blk in range(16):
                P.op('dve', lambda g: g.max(out=sv[:, blk, 0:8], in_=sc[:, blk, :]), reads=["sc"], writes=["sv"])
                P.op('dve', lambda g: g.max_index(out=si[:, blk, 0:8], in_max=sv[:, blk, 0:8], in_values=sc[:, blk, :]),
                     reads=["sc", "sv"], writes=["si"])
                P.op('dve', lambda g: g.match_replace(out=sc2[:, blk, :], in_to_replace=sv[:, blk, 0:8], in_values=sc[:, blk, :],
                                                      imm_value=NEG), reads=["sc", "sv"], writes=["sc2"])
                P.op('dve', lambda g: g.max(out=sv[:, blk, 8:16], in_=sc2[:, blk, :]), reads=["sc2"], writes=["sv"])
                P.op('dve', lambda g: g.max_index(out=si[:, blk, 8:16], in_max=sv[:, blk, 8:16], in_values=sc2[:, blk, :]),
                     reads=["sc2", "sv"], writes=["si"])
            P.cp('dve', sif[:], si[:], ["si"], ["sif"])
            P.tt('dve', cand[:].rearrange("p h (a b) -> p h a b", b=16), sv4[:, :, 0, :].unsqueeze(3).to_broadcast(B4),
                 sv4[:, :, 1, :].unsqueeze(2).to_broadcast(B4), ALU.add, ["sv"], ["cand"])
            for hh in range(8):
                P.op('dve', lambda g: g.max(out=cv[:, hh, 0:8], in_=cand[:, hh, :]), reads=["cand"], writes=["cv"])
                P.op('dve', lambda g: g.max_index(out=ci[:, hh, 0:8], in_max=cv[:, hh, 0:8], in_values=cand[:, hh, :]),
                     reads=["cand", "cv"], writes=["ci"])
                P.op('dve', lambda g: g.match_replace(out=cand2[:, hh, :], in_to_replace=cv[:, hh, 0:8], in_values=cand[:, hh, :],
                                                      imm_value=NEG), reads=["cand", "cv"], writes=["sc2"])
                P.op('dve', lambda g: g.max(out=cv[:, hh, 8:16], in_=cand2[:, hh, :]), reads=["sc2"], writes=["cv"])
                P.op('dve', lambda g: g.max_index(out=ci[:, hh, 8:16], in_max=cv[:, hh, 8:16], in_values=cand2[:, hh, :]),
                     reads=["sc2", "cv"], writes=["ci"])
            P.op('dve', lambda g: g.tensor_single_scalar(out=cab[:, 0], in_=ci[:], scalar=4, op=ALU.logical_shift_right),
                 reads=["ci"], writes=["cab"])
            P.op('dve', lambda g: g.tensor_single_scalar(out=cab[:, 1], in_=ci[:], scalar=15, op=ALU.bitwise_and),
                 reads=["ci"], writes=["cab"])
            P.cp('dve', cabf[:], cab[:], ["cab"], ["cabf"])
            OH4 = OH.rearrange("p h (a b) -> p h a b", b=16)
            for w_ in range(2):
                P.tt('dve', OH4, iota[:].unsqueeze(1).unsqueeze(1).to_broadcast(B4), cabf[:, w_].unsqueeze(3).to_broadcast(B4),
                     ALU.is_equal, ["iota", "cabf"], ["sc"])
                P.tt('dve', OH4, OH4, sif4[:, :, w_, :].unsqueeze(2).to_broadcast(B4), ALU.mult, ["sc", "sif"], ["sc"])
                P.op('dve', lambda g: g.tensor_reduce(out=i12[:, w_], in_=OH4, axis=AX.X, op=ALU.add), reads=["sc"], writes=["i12"])
            P.stt(ef[:].rearrange("p (h k) -> p h k", k=16), i12[:, 0], 128.0, i12[:, 1], ALU.mult, ALU.add, ["i12"], ["ef"])
            P.cp('dve', eidx[:], ef[:], ["ef"], ["eidx"])
            P.tt('dve', gt[:], cv[:], cv[:, :, 0:1].to_broadcast([128, 8, 16]), ALU.subtract, ["cv"], ["gt"])
            P.actf(gt[:], gt[:], AF.Exp, ["gt"], ["gt"])
            P.op('dve', lambda g: g.tensor_reduce(out=gs[:], in_=gt[:], axis=AX.X, op=ALU.add), reads=["gt"], writes=["gs"])
            P.op('dve', lambda g: g.reciprocal(out=gs[:], in_=gs[:]), reads=["gs"], writes=["gs"])
            P.tt('dve', gt[:], gt[:], gs[:].unsqueeze(2).to_broadcast([128, 8, 16]), ALU.mult, ["gt", "gs"], ["gt"])
            GS = 2
            NG = 128 // GS
            gtf = gt[:].rearrange("p h k -> p (h k)")
            for g in range(NG + 2):
                if g < NG:
                    for i in range(GS):
                        k = g * GS + i
                        sl = (g % 4) * GS + i
                        P.dma('pool', f"guv{sl}", lambda e: e.indirect_dma_start(
                            out=uvg[sl][:], out_offset=None, in_=uv_d[:, :],
                            in_offset=bass.IndirectOffsetOnAxis(ap=eidx[:, k:k + 1], axis=0)), reads=["eidx"], writes=[f"uvg{sl}"])
                        P.op('dve', lambda g_: g_.scalar_tensor_tensor(out=junk[:], in0=uvg[sl][:, 0:D], scalar=1.0, in1=M[:],
                                                                       op0=ALU.mult, op1=ALU.mult, accum_out=dots[:, k:k + 1]),
                             reads=[f"uvg{sl}", "M"], writes=[f"dots{g % 4}"])
                elif g == NG:
                    P.op('dve', lambda g_: g_.memset(gs[:, 0:1], 0.0), writes=["dfence"])
                if 1 <= g <= NG:
                    gq = g - 1
                    ks = slice(gq * GS, (gq + 1) * GS)
                    fence = [f"dots{g % 4}"] if g < NG else ["dfence"]
                    P.actf(actv[:, ks], dots[:, ks], AF.Gelu, [f"dots{gq % 4}"] + fence, [f"actv{gq % 4}"])
                if g >= 2:
                    gp = g - 2
                    ks = slice(gp * GS, (gp + 1) * GS)
                    P.tt('dve', actv[:, ks], actv[:, ks], gtf[:, ks], ALU.mult, [f"actv{gp % 4}", "gt"], [f"actv{gp % 4}"])
                    for i in range(GS):
                        k = gp * GS + i
                        sl = (gp % 4) * GS + i
                        pk, pkid = Pk[k % 4], f"Pk{k % 4}"
                        P.actf(pk[:], uvg[sl][:, D:2 * D], AF.Identity, [f"uvg{sl}", f"actv{gp % 4}"], [pkid], scale=actv[:, k:k + 1])
                        for hf in range(2):
                            P.mm(pa[hf][:, 0:512], identb[:], pk[:, hf * 512:(hf + 1) * 512], k == 0, k == 127,
                                 [pkid, "identb"], [f"psb{6 + hf}"])
            for hf in range(2):
                P.tt('dve', acc[:, hf * 512:(hf + 1) * 512], pa[hf][:, 0:512], mod[:, 2, hf * 512:(hf + 1) * 512], ALU.mult,
                     [f"psb{6 + hf}", mid], ["acc"])
            P.stt(acc[:], hs[s][:], ALPHA, acc[:], ALU.mult, ALU.add, ["h0", "acc"], ["acc"])
            layernorm(P, acc[:], "acc", lng[:], lnb[:], O[s][:], "O0", stats, mv)
            P.store("st_o0", Hout[rows, :], O[s][:], "O0")


def emit_cvt(P, src, dst, pfx):
    with P.scope(pfx):
        NSL = 3
        fin = [P.sb(f"fin{i}", [128, 2 * D]) for i in range(NSL)]
        fout = [P.sb(f"fout{i}", [128, 2 * D], BF16) for i in range(NSL)]
        engs = ('dve', 'act', 'pool')
        for r in range(128):
            sl = r % NSL
            rows = slice(r * 128, (r + 1) * 128)
            P.load(f"ld_f{sl}", fin[sl][:], src[rows, :], f"fin{sl}")
            P.cp(engs[r % 3], fout[sl][:], fin[sl][:], [f"fin{sl}"], [f"fout{sl}"])
            P.store(f"st_f{sl}", dst[rows, :], fout[sl][:], f"fout{sl}")


def emit_mlaproj(P, I, modsB, H2, kT_s, krT_s, v_s, qT_s, qrT_s):
    with P.scope("mp"):
        ident = P.sb("ident", [128, 128]); mL = P.sb("mL", [128, 2, D]); mC = P.sb("mC", [128, 2, D])
        wdq = P.sb("wdq", [128, 8, 384]); qn = P.sb("qn", [128, 384]); wuq = P.sb("wuq", [128, 3, 1536])
        wdkv = P.sb("wdkv", [128, 8, 320]); kvn = P.sb("kvn", [128, 256]); wukv = P.sb("wukv", [128, 2, 2048])
        own = P.sb("own", [128, 16], U32)
        P.load("ld_c", ident[:], I["ident"], "ident"); P.load("ld_c", own[:], I["ownidx"], "own")
        P.load("ld_c", mL[:], modsB[1, 0, :, 0:2048].rearrange("p (m d) -> p m d", d=D), "mL")
        P.load("ld_c", mC[:], modsB[1, 1, :, 0:2048].rearrange("p (m d) -> p m d", d=D), "mC")
        P.load("ld_c", qn[:], I["qn"], "qn"); P.load("ld_c", kvn[:], I["kvn"], "kvn")
        for dst, key, nm in ((wdq, "wdq", "wdq"), (wuq, "wuq", "wuq"), (wdkv, "wdkv", "wdkv"), (wukv, "wukv", "wukv")):
            P.load("ld_w", dst[:], I[key].rearrange("(kc p) n -> p kc n", p=128), nm)
        P.ts('dve', mL[:, 1, :], mL[:, 1, :], 1.0, None, ALU.add, None, ["mL"], ["mL"])
        P.ts('dve', mC[:, 1, :], mC[:, 1, :], 1.0, None, ALU.add, None, ["mC"], ["mC"])
        hs = [P.sb(f"h{i}", [128, D]) for i in range(2)]
        cs = [P.sb(f"cs{i}", [128, 2, 32]) for i in range(2)]
        M = P.sb("M", [128, D]); T = P.sb("T", [128, 8, 128]); T2 = P.sb("T2", [128, 3, 128])
        cq = P.sb("cq", [128, 384]); cqn = P.sb("cqn", [128, 384]); junk = P.sb("junk", [128, 384]); ss = P.sb("ss", [128, 4])
        Qf = P.sb("Qf", [128, 1536]); QR = P.sb("QR", [128, 8, 64]); kv = P.sb("kv", [128, 320]); ckn = P.sb("ckn", [128, 256])
        KVf = P.sb("KVf", [128, 2048]); KRf = P.sb("KRf", [128, 64])
        rt = P.sb("rt", [128, 4, 8, 32])
        KT = [P.sb(f"KT{i}", [128, 8, 128], BF16) for i in range(2)]
        KRT = [P.sb(f"KRT{i}", [64, 128], BF16) for i in range(2)]
        V9 = [P.sb(f"V9{i}", [128, 8, 129], BF16) for i in range(2)]
        QT = [P.sb(f"QT{i}", [128, 8, 128], BF16) for i in range(2)]
        QRT = [P.sb(f"QRT{i}", [64, 8, 128], BF16) for i in range(2)]
        for i in range(2):
            P.op('dve', lambda g: g.memset(V9[i][:], 1.0), writes=[f"V9{i}"])
        tring = PsumRing(P, range(0, 4)); lring = PsumRing(P, range(4, 8))
        KVf4 = KVf[:].rearrange("p (h d) -> p h d", d=256)
        for t in range(NTB):
            s = t % 2
            rows = slice(t * 128, (t + 1) * 128)
            cols = slice(t * 128, (t + 1) * 128)
            mod, mid = (mC, "mC") if t >= 64 else (mL, "mL")
            P.load(f"ld_h{s}", hs[s][:], H2[rows, :], f"h{s}")
            P.load(f"ld_cs{s}", cs[s][:, 0, :], I["cos"][rows, :], f"cs{s}")
            P.load(f"ld_cs{s}", cs[s][:, 1, :], I["sin"][rows, :], f"cs{s}")
            P.tt('dve', M[:], hs[s][:], mod[:, 1, :], ALU.mult, [f"h{s}", mid], ["M"])
            P.tt('dve', M[:], M[:], mod[:, 0, :], ALU.add, ["M", mid], ["M"])
            transpose_tile(P, tring, M, "M", T, "T", 8, ident)
            linear(P, lring, T, "T", wdkv, "wdkv", 8, 320, lambda nb, c0, ps, pid, n: P.cp('act', kv[:, c0:c0 + n], ps, [pid], ["kv"]))
            rmsnorm(P, kv[:, 0:256], "kv", 256, kvn[:], "kvn", ckn[:], "ckn", junk[:, 0:256], ss)
            rope(P, kv[:, 256:288], kv[:, 288:320], cs[s][:, 0, :], cs[s][:, 1, :], KRf[:, 0:32], KRf[:, 32:64],
                 [rt[:, i, 0, :] for i in range(4)], ["kv", f"cs{s}"], ["KRf"])
            pt, pid = tring.get()
            P.tr(pt[0:64, 0:128], KRf[:, 0:64], ident[:], ["KRf", "ident"], [pid])
            P.cp('act', KRT[s][:], pt[0:64, 0:128], [pid], [f"KRT{s}"])
            P.store(f"st_kr{s}", krT_s[:, cols], KRT[s][:], f"KRT{s}")
            transpose_tile(P, tring, ckn, "ckn", T2, "T2", 2, ident)
            linear(P, lring, T2, "T2", wukv, "wukv", 2, 2048,
                   lambda nb, c0, ps, pid, n: P.cp('act' if nb % 2 else 'dve', KVf[:, c0:c0 + n], ps, [pid], ["KVf"]))
            for hh in range(8):
                pt, pid = tring.get()
                P.tr(pt[:, 0:128], KVf[:, hh * 256:hh * 256 + 128], ident[:], ["KVf", "ident"], [pid])
                P.cp('act' if hh % 2 else 'dve', KT[s][:, hh, :], pt[:, 0:128], [pid], [f"KT{s}"])
            P.store(f"st_kt{s}", kT_s[:, :, cols].rearrange("h d t -> d h t"), KT[s][:], f"KT{s}")
            P.cp('dve', V9[s][:, :, 0:128], KVf4[:, :, 128:256], ["KVf"], [f"V9{s}"])
            P.store(f"st_v{s}", v_s[:, :, t * 129:(t + 1) * 129].rearrange("h p c -> p h c"), V9[s][:], f"V9{s}")
        for i in range(16):
            s = i % 2
            rows = slice(i * 128, (i + 1) * 128)
            P.dma('pool', f"gh{s}", lambda e: e.indirect_dma_start(
                out=hs[s][:], out_offset=None, in_=H2[:, :],
                in_offset=bass.IndirectOffsetOnAxis(ap=own[:, i:i + 1], axis=0)), reads=["own", "H2"], writes=[f"h{s}"])
            P.load(f"ld_cs{s}", cs[s][:, 0, :], I["cos_own"][rows, :], f"cs{s}")
            P.load(f"ld_cs{s}", cs[s][:, 1, :], I["sin_own"][rows, :], f"cs{s}")
            P.tt('dve', M[:], hs[s][:], mL[:, 1, :], ALU.mult, [f"h{s}", "mL"], ["M"])
            P.tt('dve', M[:], M[:], mL[:, 0, :], ALU.add, ["M", "mL"], ["M"])
            transpose_tile(P, tring, M, "M", T, "T", 8, ident)
            linear(P, lring, T, "T", wdq, "wdq", 8, 384, lambda nb, c0, ps, pid, n: P.cp('act', cq[:, c0:c0 + n], ps, [pid], ["cq"]))
            rmsnorm(P, cq[:], "cq", 384, qn[:], "qn", cqn[:], "cqn", junk[:], ss)
            transpose_tile(P, tring, cqn, "cqn", T2, "T2", 3, ident)
            linear(P, lring, T2, "T2", wuq, "wuq", 3, 1536,
                   lambda nb, c0, ps, pid, n: P.cp('act' if nb % 2 else 'dve', Qf[:, c0:c0 + n], ps, [pid], ["Qf"]))
            Qf4 = Qf[:].rearrange("p (h d) -> p h d", d=192)
            cosb = cs[s][:, 0, :].unsqueeze(1).to_broadcast([128, 8, 32])
            sinb = cs[s][:, 1, :].unsqueeze(1).to_broadcast([128, 8, 32])
            rope(P, Qf4[:, :, 128:160], Qf4[:, :, 160:192], cosb, sinb, QR[:, :, 0:32], QR[:, :, 32:64],
                 [rt[:, j] for j in range(4)], ["Qf", f"cs{s}"], ["QR"])
            for hh in range(8):
                pt, pid = tring.get()
                P.tr(pt[:, 0:128], Qf[:, hh * 192:hh * 192 + 128], ident[:], ["Qf", "ident"], [pid])
                P.cp('act' if hh % 2 else 'dve', QT[s][:, hh, :], pt[:, 0:128], [pid], [f"QT{s}"])
                pt, pid = tring.get()
                P.tr(pt[0:64, 0:128], QR[:, hh, :], ident[:], ["QR", "ident"], [pid])
                P.cp('dve' if hh % 2 else 'act', QRT[s][:, hh, :], pt[0:64, 0:128], [pid], [f"QRT{s}"])
            P.store(f"st_qt{s}", qT_s[:, :, rows].rearrange("h d t -> d h t"), QT[s][:], f"QT{s}")
            P.store(f"st_qr{s}", qrT_s[:, :, rows].rearrange("h d t -> d h t"), QRT[s][:], f"QRT{s}")


def emit_attn(P, I, modsB, H2, kT_d, krT_d, v_d, qT_d, qrT_d, H3):
    with P.scope("at"):
        ident = P.sb("ident", [128, 128]); gate = P.sb("gate", [128, D]); wo = P.sb("wo", [128, 8, D])
        lng = P.sb("lng", [128, D]); lnb = P.sb("lnb", [128, D]); own = P.sb("own", [128, 16], U32)
        krTa = P.sb("krTa", [65, NK], BF16)
        P.load("ld_c", ident[:], I["ident"], "ident"); P.load("ld_c", own[:], I["ownidx"], "own")
        P.load("ld_c", gate[:], modsB[1, 0, :, 2048:3072], "gate")
        P.load("ld_c", lng[:], I["lng"][2], "lng"); P.load("ld_c", lnb[:], I["lnb"][2], "lnb")
        P.load("ld_wo", wo[:], I["wo_mla"].rearrange("(kc p) n -> p kc n", p=128), "wo")
        P.load("ld_kr", krTa[0:64, :], krT_d, "krTa")
        P.op('dve', lambda g: g.memset(krTa[64:65, :], 1.0), writes=["krTa"])
        kT = [P.sb(f"kT{i}", [128, NK], BF16) for i in range(2)]
        vh = [P.sb(f"vh{i}", [128, NKT * 129], BF16) for i in range(2)]
        qT = [P.sb(f"qT{i}", [128, 512], BF16) for i in range(2)]
        qrTa = [P.sb(f"qrTa{i}", [65, 512], BF16) for i in range(2)]
        NPT = 3
        PT = [P.sb(f"PT{i}", [128, 512], BF16) for i in range(NPT)]
        mx = P.sb("mx", [128, 20]); nmp = P.sb("nmp", [128, 65]); rden = P.sb("rden", [128, 4])
        otok = P.sb("otok", [128, 4, D])
        hs = [P.sb("hs0", [128, D])] * 2
        A = P.sb("A", [128, D]); T = P.sb("T", [128, 8, 128])
        O = [P.sb("O0", [128, D])] * 2
        stats = P.sb("stats", [128, 2, 6]); mv = P.sb("mv", [128, 4])
        sring = PsumRing(P, range(0, 3)); mring = PsumRing(P, range(3, 4)); oring = PsumRing(P, range(4, 8))
        P.op('dve', lambda g: g.memset(nmp[:], 0.0), writes=["nmp"])
        kblocks = [(512 * i, 512) for i in range(16)] + [(8192, 256)]
        it = 0
        ipt = 0
        tcount = 0
        for qb in range(4):
            for hh in range(8):
                s = it % 2
                it += 1
                P.load(f"ld_k{s}", kT[s][:], kT_d[hh], f"kT{s}")
                P.load(f"ld_v{s}", vh[s][:], v_d[hh], f"vh{s}")
                P.load(f"ld_q{s}", qT[s][:], qT_d[hh, :, qb * 512:(qb + 1) * 512], f"qT{s}")
                P.load(f"ld_qr{s}", qrTa[s][0:64, :], qrT_d[hh, :, qb * 512:(qb + 1) * 512], f"qrTa{s}")
                for qs in range(4):
                    qc = slice(qs * 128, (qs + 1) * 128)
                    for kb, (k0, n) in enumerate(kblocks):
                        pt, pid = sring.get()
                        P.mm(pt[:, 0:n], qT[s][:, qc], kT[s][:, k0:k0 + n], True, False, [f"qT{s}", f"kT{s}"], [pid])
                        P.mm(pt[:, 0:n], qrTa[s][0:64, qc], krTa[0:64, k0:k0 + n], False, True, [f"qrTa{s}", "krTa"], [pid])
                        P.op('dve', lambda g: g.tensor_reduce(out=mx[:, kb:kb + 1], in_=pt[:, 0:n], axis=AX.X, op=ALU.max),
                             reads=[pid], writes=["mx"])
                    P.op('dve', lambda g: g.tensor_reduce(out=mx[:, 17:18], in_=mx[:, 0:17], axis=AX.X, op=ALU.max),
                         reads=["mx"], writes=["mx"])
                    P.ts('dve', nmp[:, 64:65], mx[:, 17:18], -1.0, None, ALU.mult, None, ["mx"], ["nmp"])
                    pt, pid = mring.get()
                    P.mm(pt[0:65, 0:128], nmp[:, :], ident[:], True, True, ["nmp", "ident"], [pid])
                    P.cp('act', qrTa[s][64:65, qc], pt[64:65, 0:128], [pid], [f"qrTa{s}"])
                ops = [oring.get() for _ in range(4)]
                for kt in range(NKT):
                    kc = slice(kt * 128, (kt + 1) * 128)
                    pt, pid = sring.get()
                    P.mm(pt[:, 0:512], kT[s][:, kc], qT[s][:, :], True, False, [f"kT{s}", f"qT{s}"], [pid])
                    P.mm(pt[:, 0:512], krTa[0:65, kc], qrTa[s][0:65, :], False, True, ["krTa", f"qrTa{s}"], [pid])
                    pi = ipt % NPT
                    ipt += 1
                    P.actf(PT[pi][:], pt[:, 0:512], AF.Exp, [pid], [f"PT{pi}"], scale=ATT_SCALE)
                    for qs in range(4):
                        ot, oid = ops[qs]
                        P.mm(ot[:, 0:129], PT[pi][:, qs * 128:(qs + 1) * 128], vh[s][:, kt * 129:(kt + 1) * 129],
                             kt == 0, kt == NKT - 1, [f"PT{pi}", f"vh{s}"], [oid])
                for qs in range(4):
                    ot, oid = ops[qs]
                    P.op('dve', lambda g: g.reciprocal(out=rden[:, qs:qs + 1], in_=ot[:, 128:129]), reads=[oid], writes=["rden"])
                    P.ts('dve', otok[:, qs, hh * 128:(hh + 1) * 128], ot[:, 0:128], rden[:, qs:qs + 1], None, ALU.mult, None,
                         [oid, "rden"], ["otok"])
            for qs in range(4):
                s2 = tcount % 2
                ti = qb * 4 + qs
                tcount += 1
                rows = slice(ti * 128, (ti + 1) * 128)
                P.dma('pool', "gh0", lambda e: e.indirect_dma_start(
                    out=hs[s2][:], out_offset=None, in_=H2[:, :],
                    in_offset=bass.IndirectOffsetOnAxis(ap=own[:, ti:ti + 1], axis=0)), reads=["own"], writes=["hs0"])
                for kc in range(8):
                    pt, pid = sring.get()
                    P.tr(pt[:, 0:128], otok[:, qs, kc * 128:(kc + 1) * 128], ident[:], ["otok", "ident"], [pid])
                    P.cp('act' if kc % 2 == 0 else 'dve', T[:, kc, :], pt[:, 0:128], [pid], ["T"])
                linear(P, sring, T, "T", wo, "wo", 8, D,
                       lambda nb, c0, ps, pid, n: P.tt('dve', A[:, c0:c0 + n], ps, gate[:, c0:c0 + n], ALU.mult, [pid, "gate"], ["A"]))
                P.stt(A[:], hs[s2][:], ALPHA, A[:], ALU.mult, ALU.add, ["hs0", "A"], ["A"])
                layernorm(P, A[:], "A", lng[:], lnb[:], O[s2][:], "O0", stats, mv)
                P.store("st_o0", H3[rows, :], O[s2][:], "O0")


IN_SPECS = [
    ("cT", [128, 8, 2], F32), ("aw", [2, 1024, 6144], F32), ("abB", [128, 2, 6144], F32), ("abT", [128, 16], F32),
    ("xT", [8, 128, SEQ_T], F32), ("x_tok", [NROW, D], F32),
    ("a_re", [8, 128, 8], F32), ("a_im", [8, 128, 8], F32), ("ldt", [8, 128, 8], F32),
    ("bre", [8, 128, 8, 16], F32), ("bim", [8, 128, 8, 16], F32), ("cre", [8, 128, 8, 16], F32), ("cim", [8, 128, 8, 16], F32),
    ("dvec", [8, 128, 1], F32), ("ident", [128, 128], F32), ("iota16", [128, 16], F32),
    ("wglu", [D, D], F32), ("wo_s5", [D, D], F32), ("lng", [4, 128, D], F32), ("lnb", [4, 128, D], F32),
    ("peer_wq", [2, D, D], F32), ("kbd", [2, 128, 8, 256], F32), ("peer_uv0", [16384, 2 * D], F32), ("peer_uv1", [16384, 2 * D], F32),
    ("wdq", [D, 384], F32), ("qn", [128, 384], F32), ("wuq", [384, 1536], F32), ("wdkv", [D, 320], F32),
    ("kvn", [128, 256], F32), ("wukv", [256, 2048], F32), ("wo_mla", [D, D], F32),
    ("cos", [NROW, 32], F32), ("sin", [NROW, 32], F32), ("cos_own", [2048, 32], F32), ("sin_own", [2048, 32], F32),
    ("ownidx", [128, 16], U32),
]


def build_fused():
    P = Prog()
    I = {name: P.din(name, shape, dt) for name, shape, dt in IN_SPECS}
    out = P.dout("out", [2048, D])
    modsB = P.scratch("modsB", [2, 2, 128, 6144])
    YF = P.scratch("YF", [NROW, D]); YB = P.scratch("YB", [NROW, D])
    H1 = P.scratch("H1", [NROW, D]); H2 = P.scratch("H2", [NROW, D]); H3 = P.scratch("H3", [2048, D])
    kT_s = P.scratch("kT_s", [8, 128, NK], BF16); krT_s = P.scratch("krT_s", [64, NK], BF16)
    v_s = P.scratch("v_s", [8, 128, NKT * 129], BF16)
    qT_s = P.scratch("qT_s", [8, 128, 2048], BF16); qrT_s = P.scratch("qrT_s", [8, 64, 2048], BF16)
    uvb = [P.scratch(f"uvb{l}", [16384, 2 * D], BF16) for l in range(2)]
    for l in range(2):
        emit_cvt(P, I[f"peer_uv{l}"], uvb[l], f"cv{l}")
    emit_mods(P, I["cT"], I["aw"], I["abB"], modsB)
    emit_s5(P, I, YF, YB)
    emit_s5out(P, I, modsB, YF, YB, H1)
    emit_peer(P, I, modsB, 0, H1, H2, NTB, "p0", uvb[0])
    emit_mlaproj(P, I, modsB, H2, kT_s, krT_s, v_s, qT_s, qrT_s)
    emit_attn(P, I, modsB, H2, kT_s, krT_s, v_s, qT_s, qrT_s, H3)
    emit_peer(P, I, modsB, 1, H3, out, 16, "p1", uvb[1])
    P.finish()
    return P


def bc(v):
    return np.ascontiguousarray(np.broadcast_to(v[None, :], (128, v.shape[0])).astype(np.float32))


def rope_tables():
    n_freq = 16
    inv = (10000.0 ** (-np.arange(n_freq, dtype=np.float32) / n_freq)).astype(np.float32)
    r, col = np.meshgrid(np.arange(128, dtype=np.float32), np.arange(64, dtype=np.float32), indexing='ij')
    ang = np.concatenate([r.reshape(-1, 1) * inv, col.reshape(-1, 1) * inv], axis=-1).astype(np.float32)
    return np.cos(ang).astype(np.float32), np.sin(ang).astype(np.float32)


def kernel(x, c, ctx, c_ctx, ada_w, ada_b, ln_g, ln_b,
           s5_a_re, s5_a_im, s5_log_dt, s5_b_re, s5_b_im, s5_c_re, s5_c_im, s5_d, s5_w_glu, s5_w_o,
           mla_w_dq, mla_q_norm, mla_w_uq, mla_w_dkv, mla_kv_norm, mla_w_ukv, mla_w_o,
           peer_w_q, peer_keys, peer_u, peer_v):
    f = lambda a: np.ascontiguousarray(np.asarray(a, dtype=np.float32))
    x, c, ctx, c_ctx, ada_w, ada_b, ln_g, ln_b = map(f, (x, c, ctx, c_ctx, ada_w, ada_b, ln_g, ln_b))
    P = build_fused()
    def st(a):
        return np.ascontiguousarray(f(a).reshape(2, 8, 4, 2, 64).transpose(1, 3, 4, 0, 2).reshape(8, 128, 8))

    def bl(a):
        return np.ascontiguousarray(f(a).reshape(2, 8, 4, 2, 64, 16).transpose(1, 3, 4, 0, 2, 5).reshape(8, 128, 8, 16))
    kbd = np.zeros((2, 128, 8, 256), dtype=np.float32)
    pk = f(peer_keys)
    for l in range(2):
        for s_ in range(2):
            kbd[l, 64 * s_:64 * s_ + 64, :, 128 * s_:128 * s_ + 128] = pk[l, :, s_].transpose(2, 0, 1)
    cosl, sinl = rope_tables()
    cos_all = np.ones((NROW, 32), np.float32); sin_all = np.zeros((NROW, 32), np.float32)
    cos_all[:8192] = cosl; sin_all[:8192] = sinl
    common = {
        "aw": ada_w, "abB": np.ascontiguousarray(np.broadcast_to(ada_b[None], (128, 2, 6144))),
        "abT": np.ascontiguousarray(ada_b[0, :2048].reshape(2, 8, 128).transpose(2, 0, 1).reshape(128, 16)),
        "a_re": st(s5_a_re[0]), "a_im": st(s5_a_im[0]),
        "ldt": st(np.broadcast_to(f(s5_log_dt)[0][:, :, None], (2, 64, 64))),
        "bre": bl(s5_b_re[0]), "bim": bl(s5_b_im[0]),
        "cre": bl(f(s5_c_re)[0].transpose(0, 1, 3, 2)), "cim": bl(f(s5_c_im)[0].transpose(0, 1, 3, 2)),
        "dvec": np.ascontiguousarray(f(s5_d)[0].reshape(8, 128, 1)),
        "ident": np.eye(128, dtype=np.float32), "iota16": bc(np.arange(16, dtype=np.float32)),
        "wglu": f(s5_w_glu)[0], "wo_s5": f(s5_w_o)[0],
        "lng": np.stack([bc(ln_g[0, 0]), bc(ln_g[0, 1]), bc(ln_g[1, 0]), bc(ln_g[1, 1])]),
        "lnb": np.stack([bc(ln_b[0, 0]), bc(ln_b[0, 1]), bc(ln_b[1, 0]), bc(ln_b[1, 1])]),
        "peer_wq": f(peer_w_q), "kbd": kbd, "peer_uv0": np.ascontiguousarray(np.concatenate([f(peer_u)[0], f(peer_v)[0]], axis=1)),
        "peer_uv1": np.ascontiguousarray(np.concatenate([f(peer_u)[1], f(peer_v)[1]], axis=1)),
        "wdq": f(mla_w_dq)[0], "qn": bc(f(mla_q_norm)[0]), "wuq": f(mla_w_uq)[0], "wdkv": f(mla_w_dkv)[0],
        "kvn": bc(f(mla_kv_norm)[0]), "wukv": f(mla_w_ukv)[0], "wo_mla": f(mla_w_o)[0],
        "cos": cos_all, "sin": sin_all,
    }
    per_b = []
    for b in range(2):
        seq = np.concatenate([ctx[b], x[b]], axis=0)
        per_b.append({
            "cT": np.ascontiguousarray(np.stack([c[b], c_ctx], axis=1).reshape(8, 128, 2).transpose(1, 0, 2)),
            "xT": np.ascontiguousarray(seq.T.reshape(8, 128, SEQ_T)),
            "x_tok": np.ascontiguousarray(np.concatenate([x[b], ctx[b]], axis=0)),
        })
    maps = []
    for k in range(NCORES):
        b, j = k // 4, k % 4
        m = dict(common)
        m.update(per_b[b])
        m["cos_own"] = np.ascontiguousarray(cosl[2048 * j:2048 * (j + 1)])
        m["sin_own"] = np.ascontiguousarray(sinl[2048 * j:2048 * (j + 1)])
        m["ownidx"] = np.ascontiguousarray((2048 * j + np.arange(2048, dtype=np.uint32)).reshape(16, 128).T)
        maps.append(m)
    res = P.run(maps)
    out = np.zeros((2, 8192, D), np.float32)
    for k in range(NCORES):
        b, j = k // 4, k % 4
        out[b, 2048 * j:2048 * (j + 1)] = res[k]["out"]
    return out
```

```python
import math
import numpy as np
import ml_dtypes
from contextlib import ExitStack
import concourse.bass as bass
import concourse.mybir as mybir
from concourse.bass_utils import run_bass_kernel_spmd

F32 = mybir.dt.float32
BF16 = mybir.dt.bfloat16
U32 = mybir.dt.uint32
I32 = mybir.dt.int32
AF = mybir.ActivationFunctionType
ALU = mybir.AluOpType
AX = mybir.AxisListType

NCORES = 8
D = 1024
ALPHA = (2 * 2) ** 0.25
LN_EPS = 1e-5
RMS_EPS = 1e-6
NT = 17
TPC = NT * 128


class Prog:
    def __init__(self):
        self.nc = bass.Bass("TRN2", target_bir_lowering=False)
        self.es = ExitStack()
        self.semes = ExitStack()
        nc = self.nc
        self.eng = {'pe': nc.tensor, 'dve': nc.vector, 'act': nc.scalar, 'pool': nc.gpsimd, 'sp': nc.sync}
        self.sems, self.cnt = {}, {}
        self.seen = {e: {} for e in self.eng}
        self.lastw, self.readers = {}, {}
        for e in ('pe', 'dve', 'act', 'pool'):
            self._sem('E_' + e)
        self.npsum = 0

    def _sem(self, key):
        if key not in self.sems:
            self.sems[key] = self.semes.enter_context(self.nc.semaphore(key))
            self.cnt[key] = 0
        return self.sems[key]

    def din(self, name, shape, dt=F32):
        return self.nc.dram_tensor(name, list(shape), dt, kind="ExternalInput").ap()

    def dout(self, name, shape, dt=F32):
        return self.nc.dram_tensor(name, list(shape), dt, kind="ExternalOutput").ap()

    def sb(self, name, shape, dt=F32):
        return self.es.enter_context(self.nc.sbuf_tensor("sb_" + getattr(self, "pfx", "") + name, list(shape), dt))

    def scratch(self, name, shape, dt=F32):
        return self.nc.dram_tensor(name, list(shape), dt).ap()

    def scope(self, pfx):
        prog = self

        class _S:
            def __enter__(s_):
                s_.old = (prog.es, getattr(prog, "pfx", ""))
                prog.es = ExitStack()
                prog.pfx = pfx + "_"
                return prog

            def __exit__(s_, *a):
                prog.barrier()
                prog.es.close()
                prog.es, prog.pfx = s_.old
                return False
        return _S()

    def barrier(self):
        for e in self.eng:
            for k, v in self.cnt.items():
                if v > 0 and self.seen[e].get(k, 0) < v:
                    self.eng[e].wait_ge(self.sems[k], v)
                    self.seen[e][k] = v

    def ps(self, name, shape, dt=F32):
        return self.es.enter_context(self.nc.psum_tensor(name, list(shape), dt))

    def _deps(self, reads, writes):
        deps = {}

        def add(k, v):
            if not k.startswith('E_'):
                v = max(v, self.cnt[k])
            if deps.get(k, 0) < v:
                deps[k] = v
        for b in reads:
            if b in self.lastw:
                add(*self.lastw[b])
        for b in writes:
            if b in self.lastw:
                add(*self.lastw[b])
            for k, v in self.readers.get(b, {}).items():
                add(k, v)
        return deps

    def _wait(self, e, deps, skip=None):
        for k, v in deps.items():
            if k == skip or self.seen[e].get(k, 0) >= v:
                continue
            self.eng[e].wait_ge(self.sems[k], v)
            self.seen[e][k] = v

    def _commit(self, k, v, reads, writes):
        for b in writes:
            self.lastw[b] = (k, v)
            self.readers[b] = {}
        for b in reads:
            r = self.readers.setdefault(b, {})
            if r.get(k, 0) < v:
                r[k] = v

    def op(self, e, fn, reads=(), writes=()):
        key = 'E_' + e
        self._wait(e, self._deps(reads, writes), skip=key if e == 'pe' else None)
        ins = fn(self.eng[e])
        self.cnt[key] += 1
        ins.then_inc(self.sems[key], 1)
        self._commit(key, self.cnt[key], reads, writes)

    def dma(self, q, semkey, fn, reads=(), writes=()):
        self._sem(semkey)
        self._wait(q, self._deps(reads, writes))
        ins = fn(self.eng[q])
        self.cnt[semkey] += 16
        ins.then_inc(self.sems[semkey], 16)
        self._commit(semkey, self.cnt[semkey], reads, writes)

    def finish(self):
        for k, v in self.cnt.items():
            if v > 0 and self.seen['sp'].get(k, 0) < v:
                self.nc.sync.wait_ge(self.sems[k], v)

    def run(self, in_maps):
        res = run_bass_kernel_spmd(self.nc, in_maps, core_ids=list(range(NCORES)))
        return res.results

    def load(self, semkey, dst, src, wid, rid=None, q='sp'):
        self.dma(q, semkey, lambda e: e.dma_start(out=dst, in_=src), reads=(rid,) if rid else (), writes=(wid,))

    def store(self, semkey, dst, src, rid, wid=None, q='sp'):
        self.dma(q, semkey, lambda e: e.dma_start(out=dst, in_=src), reads=(rid,), writes=(wid,) if wid else ())

    def tt(self, e, out, a, b, op, r, w):
        self.op(e, lambda g: g.tensor_tensor(out=out, in0=a, in1=b, op=op), reads=r, writes=w)

    def ts(self, e, out, a, s1, s2, op0, op1, r, w):
        if op1 is None:
            self.op(e, lambda g: g.tensor_scalar(out=out, in0=a, scalar1=s1, scalar2=None, op0=op0), reads=r, writes=w)
        else:
            self.op(e, lambda g: g.tensor_scalar(out=out, in0=a, scalar1=s1, scalar2=s2, op0=op0, op1=op1), reads=r, writes=w)

    def stt(self, out, a, s, b, op0, op1, r, w):
        self.op('dve', lambda g: g.scalar_tensor_tensor(out=out, in0=a, scalar=s, in1=b, op0=op0, op1=op1), reads=r, writes=w)

    def actf(self, out, a, func, r, w, bias=None, scale=None, accum=None):
        kw = {}
        if bias is not None:
            kw['bias'] = bias
        if scale is not None:
            kw['scale'] = scale
        if accum is not None:
            kw['accum_out'] = accum
        self.op('act', lambda g: g.activation(out=out, in_=a, func=func, **kw), reads=r, writes=w)

    def cp(self, e, out, a, r, w):
        if e == 'act':
            self.actf(out, a, AF.Copy, r, w)
        else:
            self.op(e, lambda g: g.tensor_copy(out=out, in_=a), reads=r, writes=w)

    def mm(self, out, lhsT, rhs, start, stop, r, w):
        self.op('pe', lambda g: g.matmul(out, lhsT, rhs, start=start, stop=stop), reads=r, writes=w)

    def tr(self, out, in_, ident, r, w):
        self.op('pe', lambda g: g.transpose(out, in_, ident), reads=r, writes=w)


class PsumRing:
    def __init__(self, P, banks, dt=F32):
        if not hasattr(P, "banks"):
            P.banks = {}
        for i in banks:
            if i not in P.banks:
                P.banks[i] = P.ps(f"psb{i}", [128, 512], dt)
        self.b = list(banks)
        self.P = P
        self.i = 0

    def get(self):
        j = self.b[self.i]
        self.i = (self.i + 1) % len(self.b)
        return self.P.banks[j], f"psb{j}"


NTB = 66
NROW = NTB * 128
SEQ_T = 256 + 8192
CH = 512
TWO_PI = 2.0 * math.pi
MAGIC = 12582912.0
NEG = -1.0e30
NK = 8448
NKT = 66
ATT_SCALE = 192.0 ** -0.5


def sincos(P, th, n, tmp, sin_out, cos_out, ids):
    for which, outt in ((0, sin_out), (1, cos_out)):
        x, u, k, y, m = (tmp[:, j, :n] for j in range(5))
        r = list(ids)
        if which == 0:
            P.cp('dve', x, th, r, r)
        else:
            P.ts('dve', x, th, math.pi / 2, None, ALU.add, None, r, r)
        P.ts('dve', u, x, 1.0 / TWO_PI, None, ALU.mult, None, r, r)
        P.ts('dve', k, u, MAGIC, None, ALU.add, None, r, r)
        P.ts('dve', k, k, MAGIC, None, ALU.subtract, None, r, r)
        P.stt(y, k, -TWO_PI, x, ALU.mult, ALU.add, r, r)
        P.ts('dve', m, y, math.pi, None, ALU.is_gt, None, r, r)
        P.stt(y, m, -TWO_PI, y, ALU.mult, ALU.add, r, r)
        P.ts('dve', m, y, -math.pi, None, ALU.is_lt, None, r, r)
        P.stt(y, m, TWO_PI, y, ALU.mult, ALU.add, r, r)
        P.ts('dve', y, y, math.pi, -math.pi, ALU.min, ALU.max, r, r)
        P.actf(outt, y, AF.Sin, r, r)


def transpose_tile(P, ring, src, sid, dst, did, nk, ident):
    for kc in range(nk):
        pt, pid = ring.get()
        P.tr(pt[:, 0:128], src[:, kc * 128:(kc + 1) * 128], ident[:], [sid, "ident"], [pid])
        P.cp('act' if kc % 2 == 0 else 'dve', dst[:, kc, :], pt[:, 0:128], [pid], [did])


def linear(P, ring, xT, xid, W, wid, nk, N, cb, bs=512):
    nb = 0
    c0 = 0
    while c0 < N:
        n = min(bs, N - c0)
        pt, pid = ring.get()
        for kc in range(nk):
            P.mm(pt[:, 0:n], xT[:, kc, :], W[:, kc, c0:c0 + n], kc == 0, kc == nk - 1, [xid, wid], [pid])
        cb(nb, c0, pt[:, 0:n], pid, n)
        c0 += n
        nb += 1


def layernorm(P, x, xid, gbc, bbc, out, oid, stats, mv):
    for c in range(2):
        P.op('dve', lambda g: g.bn_stats(out=stats[:, c, :], in_=x[:, c * 512:(c + 1) * 512]), reads=[xid], writes=["lnst"])
    P.op('dve', lambda g: g.bn_aggr(out=mv[:, 0:2], in_=stats[:].rearrange("p a b -> p (a b)")), reads=["lnst"], writes=["lnmv"])
    P.ts('dve', mv[:, 2:3], mv[:, 1:2], LN_EPS, None, ALU.add, None, ["lnmv"], ["lnmv"])
    P.actf(mv[:, 2:3], mv[:, 2:3], AF.Sqrt, ["lnmv"], ["lnmv"])
    P.op('dve', lambda g: g.reciprocal(out=mv[:, 3:4], in_=mv[:, 2:3]), reads=["lnmv"], writes=["lnmv"])
    P.ts('dve', x, x, mv[:, 0:1], mv[:, 3:4], ALU.subtract, ALU.mult, [xid, "lnmv"], [xid])
    P.tt('pool', x, x, gbc, ALU.mult, [xid, "lng"], [xid])
    P.tt('pool', out, x, bbc, ALU.add, [xid, "lnb"], [oid])


def rmsnorm(P, x, xid, n, gbc, gid, out, oid, junk, ss):
    P.actf(junk, x, AF.Square, [xid], ["rjunk"], accum=ss[:, 0:1])
    P.actf(ss[:, 3:4], junk[:, 0:1], AF.Copy, ["rjunk"], ["rjunk"])
    P.ts('dve', ss[:, 1:2], ss[:, 0:1], 1.0 / n, RMS_EPS, ALU.mult, ALU.add, ["rjunk"], ["rss"])
    P.actf(ss[:, 1:2], ss[:, 1:2], AF.Sqrt, ["rss"], ["rss"])
    P.op('dve', lambda g: g.reciprocal(out=ss[:, 2:3], in_=ss[:, 1:2]), reads=["rss"], writes=["rss"])
    P.stt(out, x, ss[:, 2:3], gbc, ALU.mult, ALU.mult, [xid, "rss", gid], [oid])


def rope(P, x1, x2, cosb, sinb, o1, o2, t, rid, wid):
    P.tt('dve', t[0], x1, cosb, ALU.mult, rid, ["ropet"])
    P.tt('dve', t[1], x2, sinb, ALU.mult, rid, ["ropet"])
    P.tt('dve', t[2], x2, cosb, ALU.mult, rid, ["ropet"])
    P.tt('dve', t[3], x1, sinb, ALU.mult, rid, ["ropet"])
    P.tt('dve', o1, t[0], t[1], ALU.subtract, ["ropet"], wid)
    P.tt('dve', o2, t[2], t[3], ALU.add, ["ropet"], wid)


def emit_mods(P, cT_d, aw_d, abB_d, modsB):
    with P.scope("md"):
        s = P.sb("s", [128, 8, 2]); srep = P.sb("srep", [128, 8, 2, 128])
        P.load("ld_c", s[:], cT_d, "s")
        P.actf(s[:], s[:], AF.Silu, ["s"], ["s"])
        P.cp('dve', srep[:], s[:].unsqueeze(3).to_broadcast([128, 8, 2, 128]), ["s"], ["srep"])
        w = [P.sb(f"w{i}", [128, 8, 512]) for i in range(2)]
        bb = [P.sb(f"bb{i}", [128, 512]) for i in range(2)]
        ot = [[P.sb(f"ot{v}{i}", [128, 512]) for i in range(2)] for v in range(2)]
        ring = PsumRing(P, range(8))
        it = 0
        for layer in range(2):
            for nb in range(12):
                sl = it % 2
                it += 1
                cols = slice(nb * 512, (nb + 1) * 512)
                P.load(f"ld_w{sl}", w[sl][:], aw_d[layer, :, cols].rearrange("(kc p) n -> p kc n", p=128), f"w{sl}")
                P.load(f"ld_b{sl}", bb[sl][:], abB_d[:, layer, cols], f"bb{sl}")
                for v in range(2):
                    pt, pid = ring.get()
                    for kc in range(8):
                        P.mm(pt[:, 0:512], srep[:, kc, v, :], w[sl][:, kc, :], kc == 0, kc == 7, ["srep", f"w{sl}"], [pid])
                    P.tt('dve', ot[v][sl][:], pt[:, 0:512], bb[sl][:], ALU.add, [pid, f"bb{sl}"], [f"ot{v}{sl}"])
                    P.store(f"st_m{v}{sl}", modsB[layer, v, :, cols], ot[v][sl][:], f"ot{v}{sl}")


def emit_s5(P, I, YF, YB):
    with P.scope("s5"):
        ident = P.sb("ident", [128, 128]); s = P.sb("s", [128, 8, 2])
        P.load("ld_par", ident[:], I["ident"], "ident")
        P.load("ld_par", s[:], I["cT"], "s")
        P.actf(s[:], s[:], AF.Silu, ["s"], ["s"])
        abT = P.sb("abT", [128, 16]); P.load("ld_par", abT[:], I["abT"], "abT")
        wsl = P.sb("wsl", [128, 2, 8, 128])
        sc1 = P.sb("sc1", [128, 2]); sh = P.sb("sh", [128, 2]); dv = P.sb("dv", [128, 1])
        are = P.sb("are", [128, 8]); aim = P.sb("aim", [128, 8]); ldt = P.sb("ldt", [128, 8])
        bre = P.sb("bre", [128, 8, 16]); bim = P.sb("bim", [128, 8, 16])
        cre = P.sb("cre", [128, 8, 16]); cim = P.sb("cim", [128, 8, 16])
        prm = P.sb("prm", [128, 16, 8]); tmp5 = P.sb("tmp5", [128, 5, 8])
        Wre = P.sb("Wre", [128, 8, 128]); Wim = P.sb("Wim", [128, 8, 128])
        Cre = P.sb("Cre", [128, 8, 128]); nCre = P.sb("nCre", [128, 8, 128]); nCim = P.sb("nCim", [128, 8, 128])
        bfull = P.sb("bfull", [128, 2, 128]); tb = P.sb("tb", [128, 2, 16])
        ctab = P.sb("ctab", [128, 8, CH]); stab = P.sb("stab", [128, 8, CH]); rB = P.sb("rB", [128, 8, CH])
        ones = P.sb("ones", [128, CH]); En = P.sb("En", [128, 8, 2, 2]); cur = P.sb("cur", [128, 4]); st = P.sb("st", [128, 4, 2])
        xs = [P.sb(f"xs{i}", [128, CH]) for i in range(2)]
        ms = [P.sb(f"ms{i}", [128, CH]) for i in range(2)]
        ys = [P.sb(f"ys{i}", [128, CH]) for i in range(2)]
        yt = [P.sb(f"yt{i}", [128, 4, 128]) for i in range(2)]
        NW = 3
        va = [P.sb(f"va{i}", [128, 4, CH]) for i in range(NW)]
        G = [P.sb(f"G{i}", [128, 2, CH]) for i in range(NW)]
        X = [P.sb(f"X{i}", [128, 4, CH]) for i in range(NW)]
        ring = PsumRing(P, range(2, 6)); yring = PsumRing(P, range(0, 2)); trring = PsumRing(P, range(6, 8))
        P.op('dve', lambda g: g.memset(ones[:], 1.0), writes=["ones"])
        R = ["prm"]
        it = 0
        wk = 0
        for fc in range(8):
            for dst, key, nm in ((are, "a_re", "are"), (aim, "a_im", "aim"), (ldt, "ldt", "ldt"), (bre, "bre", "bre"),
                                 (bim, "bim", "bim"), (cre, "cre", "cre"), (cim, "cim", "cim"), (dv, "dvec", "dv")):
                P.load("ld_par", dst[:], I[key][fc], nm)
            for which in range(2):
                c0 = which * 1024 + fc * 128
                P.load("ld_wsl", wsl[:, which], I["aw"][0, :, c0:c0 + 128].rearrange("(kc p) n -> p kc n", p=128), "wsl")
            for which, dstt, addc in ((0, sh, 0.0), (1, sc1, 1.0)):
                pt, pid = ring.get()
                for kc in range(8):
                    P.mm(pt[:, 0:2], wsl[:, which, kc, :], s[:, kc, :], kc == 0, kc == 7, ["wsl", "s"], [pid])
                P.ts('dve', dstt[:], pt[:, 0:2], abT[:, which * 8 + fc:which * 8 + fc + 1], addc, ALU.add, ALU.add,
                     [pid, "abT"], ["scsh"])
            dt_, mag, th, sn, cs, abr, abi, den, fre, fim, t0, t1 = (prm[:, j, :] for j in range(12))
            P.actf(dt_, ldt[:], AF.Exp, ["ldt"], R)
            P.tt('dve', t0, are[:], dt_, ALU.mult, ["are"] + R, R)
            P.actf(mag, t0, AF.Exp, R, R)
            P.tt('dve', th, aim[:], dt_, ALU.mult, ["aim"] + R, R)
            sincos(P, th, 8, tmp5, sn, cs, R + ["tmp5"])
            P.tt('dve', abr, mag, cs, ALU.mult, R, R)
            P.tt('dve', abi, mag, sn, ALU.mult, R, R)
            P.tt('dve', den, are[:], are[:], ALU.mult, ["are"] + R, R)
            P.tt('dve', t0, aim[:], aim[:], ALU.mult, ["aim"] + R, R)
            P.tt('dve', den, den, t0, ALU.add, R, R)
            P.op('dve', lambda g: g.reciprocal(out=den, in_=den), reads=R, writes=R)
            P.ts('dve', t0, abr, -1.0, None, ALU.add, None, R, R)
            P.tt('dve', fre, t0, are[:], ALU.mult, ["are"] + R, R)
            P.tt('dve', t1, abi, aim[:], ALU.mult, ["aim"] + R, R)
            P.tt('dve', fre, fre, t1, ALU.add, R, R)
            P.tt('dve', fre, fre, den, ALU.mult, R, R)
            P.tt('dve', fim, abi, are[:], ALU.mult, ["are"] + R, R)
            P.tt('dve', t1, t0, aim[:], ALU.mult, ["aim"] + R, R)
            P.tt('dve', fim, fim, t1, ALU.subtract, R, R)
            P.tt('dve', fim, fim, den, ALU.mult, R, R)
            for j in range(8):
                pair = j % 4
                for which, Wdst in ((0, Wre), (1, Wim)):
                    P.op('dve', lambda g: g.memset(bfull[:, which, :], 0.0), writes=["bfull"])
                    if which == 0:
                        P.ts('dve', tb[:, 0, :], bim[:, j, :], fim[:, j:j + 1], None, ALU.mult, None, ["bim"] + R, ["tb"])
                        src2, op1 = bre, ALU.subtract
                    else:
                        P.ts('dve', tb[:, 0, :], bre[:, j, :], fim[:, j:j + 1], None, ALU.mult, None, ["bre"] + R, ["tb"])
                        src2, op1 = bim, ALU.add
                    P.stt(tb[:, 1, :], src2[:, j, :], fre[:, j:j + 1], tb[:, 0, :], ALU.mult, op1, ["bre", "bim", "tb"] + R, ["tb"])
                    for g2 in range(2):
                        c0 = 32 * pair + 16 * g2
                        P.cp('dve', bfull[64 * g2:64 * g2 + 64, which, c0:c0 + 16], tb[64 * g2:64 * g2 + 64, 1, :], ["tb"], ["bfull"])
                    pt, pid = ring.get()
                    P.tr(pt[:, 0:128], bfull[:, which, :], ident[:], ["bfull", "ident"], [pid])
                    P.cp('act', Wdst[:, j, :], pt[:, 0:128], [pid], ["W"])
                for srcc, dsts in ((cre, (Cre, nCre)), (cim, (None, nCim))):
                    for dd, sgn in zip(dsts, (1.0, -1.0)):
                        if dd is None:
                            continue
                        P.op('dve', lambda g: g.memset(dd[:, j, :], 0.0), writes=["W"])
                        for g2 in range(2):
                            c0 = 32 * pair + 16 * g2
                            P.ts('dve', dd[64 * g2:64 * g2 + 64, j, c0:c0 + 16], srcc[64 * g2:64 * g2 + 64, j, :], sgn, None,
                                 ALU.mult, None, ["cre", "cim"], ["W"])
                P.op('dve', lambda g: g.memset(ctab[:, j, 0:1], 1.0), writes=["tab"])
                P.op('dve', lambda g: g.memset(stab[:, j, 0:1], 0.0), writes=["tab"])
                P.cp('dve', cur[:, 0:1], cs[:, j:j + 1], R, ["cur"])
                P.cp('dve', cur[:, 1:2], sn[:, j:j + 1], R, ["cur"])
                n = 1
                while n <= CH:
                    if n in (256, 512):
                        wi = 0 if n == 256 else 1
                        P.cp('dve', En[:, j, wi, :], cur[:, 0:2], ["cur"], ["En"])
                    if n == CH:
                        break
                    cn, snn = cur[:, 0:1], cur[:, 1:2]
                    T = ["tab", "cur", "rB"]
                    P.ts('dve', rB[:, j, 0:n], stab[:, j, 0:n], snn, None, ALU.mult, None, T, ["rB"])
                    P.stt(ctab[:, j, n:2 * n], ctab[:, j, 0:n], cn, rB[:, j, 0:n], ALU.mult, ALU.subtract, T, ["tab"])
                    P.ts('dve', rB[:, j, 0:n], ctab[:, j, 0:n], snn, None, ALU.mult, None, T, ["rB"])
                    P.stt(stab[:, j, n:2 * n], stab[:, j, 0:n], cn, rB[:, j, 0:n], ALU.mult, ALU.add, T, ["tab"])
                    P.tt('dve', cur[:, 2:3], cn, cn, ALU.mult, ["cur"], ["cur"])
                    P.tt('dve', cur[:, 3:4], snn, snn, ALU.mult, ["cur"], ["cur"])
                    P.tt('dve', cur[:, 3:4], cur[:, 2:3], cur[:, 3:4], ALU.subtract, ["cur"], ["cur"])
                    P.tt('dve', cur[:, 2:3], cn, snn, ALU.mult, ["cur"], ["cur"])
                    P.ts('dve', cur[:, 1:2], cur[:, 2:3], 2.0, None, ALU.mult, None, ["cur"], ["cur"])
                    P.cp('dve', cur[:, 0:1], cur[:, 3:4], ["cur"], ["cur"])
                    n *= 2
                P.ts('dve', rB[:, j, :], ones[:], mag[:, j:j + 1], None, ALU.mult, None, ["ones", "rB"] + R, ["rB"])
            for d_ in range(2):
                Yd = YF if d_ == 0 else YB
                P.op('dve', lambda g: g.memset(st[:], 0.0), writes=["st"])
                items = [(ci, pair) for ci in range(17) for pair in range(4)]
                cinfo = {}

                def chunk_setup(ci):
                    nonlocal it
                    n = 256 if ci == 0 else 512
                    if ci == 0:
                        t0_, row0, mcol = 0, 8192, 1
                    elif d_ == 0:
                        t0_, row0, mcol = 256 + 512 * (ci - 1), 512 * (ci - 1), 0
                    else:
                        t0_, row0, mcol = 256 + 8192 - 512 * ci, 8192 - 512 * ci, 0
                    sl = it % 2
                    it += 1
                    P.load(f"ld_x{sl}", xs[sl][:, :n], I["xT"][fc, :, t0_:t0_ + n], f"xs{sl}")
                    xin = xs[sl][:, :n] if d_ == 0 else xs[sl][:, n - 1::-1]
                    P.ts('dve', ms[sl][:, :n], xin, sc1[:, mcol:mcol + 1], sh[:, mcol:mcol + 1], ALU.mult, ALU.add,
                         [f"xs{sl}", "scsh"], [f"ms{sl}"])
                    yp, yid = yring.get()
                    cinfo[ci] = (n, row0, sl, yp, yid)

                def drive(ci, pair):
                    n, row0, sl, yp, yid = cinfo[ci]
                    j = d_ * 4 + pair
                    pa, aid = ring.get()
                    pb, bid = ring.get()
                    P.mm(pa[:, :n], Wre[:, j, :], ms[sl][:, :n], True, True, ["W", f"ms{sl}"], [aid])
                    P.mm(pb[:, :n], Wim[:, j, :], ms[sl][:, :n], True, True, ["W", f"ms{sl}"], [bid])
                    return pa, aid, pb, bid

                chunk_setup(0)
                pend = drive(0, 0)
                for ii, (ci, pair) in enumerate(items):
                    n, row0, sl, yp, yid = cinfo[ci]
                    pa, aid, pb, bid = pend
                    if ii + 1 < len(items):
                        nci, npair = items[ii + 1]
                        if npair == 0:
                            chunk_setup(nci)
                        pend = drive(nci, npair)
                    j = d_ * 4 + pair
                    w_ = wk % NW
                    wk += 1
                    c_, s_ = ctab[:, j, :n], stab[:, j, :n]
                    V, VI = va[w_], f"va{w_}"
                    P.tt('dve', V[:, 0, :n], pa[:, :n], c_, ALU.mult, [aid, "tab"], [VI])
                    P.tt('dve', V[:, 1, :n], pb[:, :n], s_, ALU.mult, [bid, "tab"], [VI])
                    P.tt('dve', V[:, 0, :n], V[:, 0, :n], V[:, 1, :n], ALU.add, [VI], [VI])
                    P.tt('dve', V[:, 2, :n], pb[:, :n], c_, ALU.mult, [bid, "tab"], [VI])
                    P.tt('dve', V[:, 3, :n], pa[:, :n], s_, ALU.mult, [aid, "tab"], [VI])
                    P.tt('dve', V[:, 2, :n], V[:, 2, :n], V[:, 3, :n], ALU.subtract, [VI], [VI])
                    GG, GI = G[w_], f"G{w_}"
                    P.op('dve', lambda g: g.tensor_tensor_scan(out=GG[:, 0, :n], data0=rB[:, j, :n], data1=V[:, 0, :n],
                                                               initial=st[:, pair, 0:1], op0=ALU.mult, op1=ALU.add),
                         reads=["rB", VI, "st"], writes=[GI])
                    P.op('dve', lambda g: g.tensor_tensor_scan(out=GG[:, 1, :n], data0=rB[:, j, :n], data1=V[:, 2, :n],
                                                               initial=st[:, pair, 1:2], op0=ALU.mult, op1=ALU.add),
                         reads=["rB", VI, "st"], writes=[GI])
                    wi = 0 if n == 256 else 1
                    cn, snn = En[:, j, wi, 0:1], En[:, j, wi, 1:2]
                    P.ts('dve', cur[:, 0:1], GG[:, 1, n - 1:n], snn, None, ALU.mult, None, [GI, "En"], ["cur"])
                    P.stt(st[:, pair, 0:1], GG[:, 0, n - 1:n], cn, cur[:, 0:1], ALU.mult, ALU.subtract, [GI, "En", "cur"], ["st"])
                    P.ts('dve', cur[:, 1:2], GG[:, 0, n - 1:n], snn, None, ALU.mult, None, [GI, "En"], ["cur"])
                    P.stt(st[:, pair, 1:2], GG[:, 1, n - 1:n], cn, cur[:, 1:2], ALU.mult, ALU.add, [GI, "En", "cur"], ["st"])
                    XX, XI = X[w_], f"X{w_}"
                    P.tt('pool', XX[:, 0, :n], GG[:, 0, :n], c_, ALU.mult, [GI, "tab"], [XI])
                    P.tt('pool', XX[:, 1, :n], GG[:, 1, :n], s_, ALU.mult, [GI, "tab"], [XI])
                    P.tt('pool', XX[:, 2, :n], GG[:, 1, :n], c_, ALU.mult, [GI, "tab"], [XI])
                    P.tt('pool', XX[:, 3, :n], GG[:, 0, :n], s_, ALU.mult, [GI, "tab"], [XI])
                    for q_, Wm in enumerate((Cre, nCre, nCim, nCim)):
                        P.mm(yp[:, :n], Wm[:, j, :], XX[:, q_, :n], pair == 0 and q_ == 0, pair == 3 and q_ == 3,
                             ["W", XI], [yid])
                    if pair == 3:
                        if d_ == 0:
                            P.stt(ys[sl][:, :n], ms[sl][:, :n], dv[:, 0:1], yp[:, :n], ALU.mult, ALU.add, [f"ms{sl}", "dv", yid], [f"ys{sl}"])
                        else:
                            P.cp('dve', ys[sl][:, :n], yp[:, n - 1::-1], [yid], [f"ys{sl}"])
                        nblk = n // 128
                        for bk in range(nblk):
                            pt, pid = trring.get()
                            P.tr(pt[:, 0:128], ys[sl][:, bk * 128:(bk + 1) * 128], ident[:], [f"ys{sl}", "ident"], [pid])
                            P.cp('act', yt[sl][:, bk, :], pt[:, 0:128], [pid], [f"yt{sl}"])
                        P.store(f"st_y{sl}", Yd[row0:row0 + n, fc * 128:(fc + 1) * 128].rearrange("(k p) f -> p k f", p=128),
                                yt[sl][:, 0:nblk, :], f"yt{sl}")


def emit_s5out(P, I, modsB, YF, YB, H1):
    nt = NTB
    with P.scope("so"):
        ident = P.sb("ident", [128, 128]); gL = P.sb("gL", [128, D]); gC = P.sb("gC", [128, D])
        wg = P.sb("wg", [128, 8, D]); wo = P.sb("wo", [128, 8, D]); lng = P.sb("lng", [128, D]); lnb = P.sb("lnb", [128, D])
        P.load("ld_c", ident[:], I["ident"], "ident")
        P.load("ld_c", gL[:], modsB[0, 0, :, 2048:3072], "gL"); P.load("ld_c", gC[:], modsB[0, 1, :, 2048:3072], "gC")
        P.load("ld_c", lng[:], I["lng"][0], "lng"); P.load("ld_c", lnb[:], I["lnb"][0], "lnb")
        P.load("ld_wg", wg[:], I["wglu"].rearrange("(kc p) n -> p kc n", p=128), "wg")
        P.load("ld_wo", wo[:], I["wo_s5"].rearrange("(kc p) n -> p kc n", p=128), "wo")
        xs = [P.sb(f"x{i}", [128, D]) for i in range(2)]
        ya = [P.sb(f"ya{i}", [128, D]) for i in range(2)]
        yb_ = [P.sb(f"yb{i}", [128, D]) for i in range(2)]
        A = P.sb("A", [128, D]); B = P.sb("B", [128, D]); C = P.sb("C", [128, D]); T = P.sb("T", [128, 8, 128])
        O = [P.sb(f"O{i}", [128, D]) for i in range(2)]
        stats = P.sb("stats", [128, 2, 6]); mv = P.sb("mv", [128, 4])
        tring = PsumRing(P, range(0, 4)); lring = PsumRing(P, range(4, 8))
        for t in range(nt):
            s = t % 2
            rows = slice(t * 128, (t + 1) * 128)
            P.load(f"ld_x{s}", xs[s][:], I["x_tok"][rows, :], f"x{s}")
            P.load(f"ld_ya{s}", ya[s][:], YF[rows, :], f"ya{s}")
            P.load(f"ld_yb{s}", yb_[s][:], YB[rows, :], f"yb{s}")
            gate, gid = (gC, "gC") if t >= 64 else (gL, "gL")
            P.tt('dve', A[:], ya[s][:], yb_[s][:], ALU.add, [f"ya{s}", f"yb{s}"], ["A"])
            P.actf(B[:], A[:], AF.Gelu, ["A"], ["B"])
            transpose_tile(P, tring, B, "B", T, "T", 8, ident)
            linear(P, lring, T, "T", wg, "wg", 8, D,
                   lambda nb, c0, ps, pid, n: P.actf(C[:, c0:c0 + n], ps, AF.Sigmoid, [pid], ["C"]))
            P.tt('dve', C[:], C[:], B[:], ALU.mult, ["B", "C"], ["C"])
            transpose_tile(P, tring, C, "C", T, "T", 8, ident)
            linear(P, lring, T, "T", wo, "wo", 8, D,
                   lambda nb, c0, ps, pid, n: P.tt('dve', A[:, c0:c0 + n], ps, gate[:, c0:c0 + n], ALU.mult, [pid, gid], ["A"]))
            P.stt(A[:], xs[s][:], ALPHA, A[:], ALU.mult, ALU.add, [f"x{s}", "A"], ["A"])
            layernorm(P, A[:], "A", lng[:], lnb[:], O[s][:], f"O{s}", stats, mv)
            P.store(f"st_o{s}", H1[rows, :], O[s][:], f"O{s}")


def emit_peer(P, I, modsB, layer, Hin, Hout, nt, pfx, uv_d):
    with P.scope(pfx):
        ident = P.sb("ident", [128, 128]); mL = P.sb("mL", [128, 3, D])
        wq = P.sb("wq", [128, 8, D]); kbd = P.sb("kbd", [128, 8, 256]); lng = P.sb("lng", [128, D]); lnb = P.sb("lnb", [128, D])
        iota = P.sb("iota", [128, 16])
        P.load("ld_c", ident[:], I["ident"], "ident")
        P.load("ld_c", mL[:], modsB[layer, 0, :, 3072:6144].rearrange("p (m d) -> p m d", d=D), "mL")
        P.load("ld_c", kbd[:], I["kbd"][layer], "kbd")
        P.load("ld_c", lng[:], I["lng"][2 * layer + 1], "lng"); P.load("ld_c", lnb[:], I["lnb"][2 * layer + 1], "lnb")
        P.load("ld_c", iota[:], I["iota16"], "iota")
        P.load("ld_wq", wq[:], I["peer_wq"][layer].rearrange("(kc p) n -> p kc n", p=128), "wq")
        P.ts('dve', mL[:, 1, :], mL[:, 1, :], 1.0, None, ALU.add, None, ["mL"], ["mL"])
        hs = [P.sb("h0", [128, D])] * 2
        identb = P.sb("identb", [128, 128], BF16)
        Pk = [P.sb(f"Pk{i}", [128, D], BF16) for i in range(4)]
        M = P.sb("M", [128, D]); T = P.sb("T", [128, 8, 128]); Q = P.sb("Q", [128, D])
        sc = P.sb("sc", [128, 16, 128])
        sv = P.sb("sv", [128, 16, 16]); si = P.sb("si", [128, 16, 16], U32); sif = P.sb("sif", [128, 16, 16])

        cv = P.sb("cv", [128, 8, 16]); ci = P.sb("ci", [128, 8, 16], U32); cab = P.sb("cab", [128, 2, 8, 16], U32)
        cabf = P.sb("cabf", [128, 2, 8, 16]); OH = sc[:].rearrange("p a b -> p (a b)").rearrange("p (h c) -> p h c", c=256)
        i12 = P.sb("i12", [128, 2, 8, 16]); ef = P.sb("ef", [128, 128]); eidx = P.sb("eidx", [128, 128], U32)
        gt = P.sb("gt", [128, 8, 16]); gs = P.sb("gs", [128, 8]); dots = P.sb("dots", [128, 128]); actv = P.sb("actv", [128, 128])
        NS = 12
        uvg = [P.sb(f"uvg{i}", [128, 2 * D], BF16) for i in range(NS)]
        junk = Q
        acc = P.sb("acc", [128, D])
        sc2t = P.sb("sc2", [128, 16, 128]); candt = P.sb("cand", [128, 8, 256])
        sc2 = sc2t[:]
        cand2 = sc2t[:].rearrange("p a b -> p (a b)").rearrange("p (h c) -> p h c", c=256)
        cand = candt[:]
        O = [Q] * 2
        stats = P.sb("stats", [128, 2, 6]); mv = P.sb("mv", [128, 4])
        tring = PsumRing(P, range(0, 4)); lring = PsumRing(P, range(4, 6)); aring = PsumRing(P, range(6, 8))
        P.cp('dve', identb[:], ident[:], ["ident"], ["identb"])
        pa = [P.banks[6], P.banks[7]]
        sv4 = sv[:].rearrange("p (h s) k -> p h s k", s=2)
        sif4 = sif[:].rearrange("p (h s) k -> p h s k", s=2)
        B4 = [128, 8, 16, 16]
        for t in range(nt):
            s = t % 2
            rows = slice(t * 128, (t + 1) * 128)
            mod, mid = mL, "mL"
            if t == 64:
                P.load("ld_c", mL[:], modsB[layer, 1, :, 3072:6144].rearrange("p (m d) -> p m d", d=D), "mL")
                P.ts('dve', mL[:, 1, :], mL[:, 1, :], 1.0, None, ALU.add, None, ["mL"], ["mL"])
            P.load("ld_h0", hs[s][:], Hin[rows, :], "h0")
            P.tt('dve', M[:], hs[s][:], mod[:, 1, :], ALU.mult, ["h0", mid], ["M"])
            P.tt('dve', M[:], M[:], mod[:, 0, :], ALU.add, ["M", mid], ["M"])
            transpose_tile(P, tring, M, "M", T, "T", 8, ident)
            linear(P, lring, T, "T", wq, "wq", 8, D,
                   lambda nb, c0, ps, pid, n: P.cp('act', Q[:, c0:c0 + n], ps, [pid], ["Q"]))
            transpose_tile(P, tring, Q, "Q", T, "T", 8, ident)
            for hh in range(8):
                pt, pid = lring.get()
                P.mm(pt[:, 0:256], T[:, hh, :], kbd[:, hh, :], True, True, ["T", "kbd"], [pid])
                P.cp('act', sc[:, 2 * hh:2 * hh + 2, :], pt[:, 0:256].rearrange("p (s n) -> p s n", s=2), [pid], ["sc"])
            for blk in range(16):
                P.op('dve', lambda g: g.max(out=sv[:, blk, 0:8], in_=sc[:, blk, :]), reads=["sc"], writes=["sv"])
                P.op('dve', lambda g: g.max_index(out=si[:, blk, 0:8], in_max=sv[:, blk, 0:8], in_values=sc[:, blk, :]),
                     reads=["sc", "sv"], writes=["si"])
                P.op('dve', lambda g: g.match_replace(out=sc2[:, blk, :], in_to_replace=sv[:, blk, 0:8], in_values=sc[:, blk, :],
                                                      imm_value=NEG), reads=["sc", "sv"], writes=["sc2"])
                P.op('dve', lambda g: g.max(out=sv[:, blk, 8:16], in_=sc2[:, blk, :]), reads=["sc2"], writes=["sv"])
                P.op('dve', lambda g: g.max_index(out=si[:, blk, 8:16], in_max=sv[:, blk, 8:16], in_values=sc2[:, blk, :]),
                     reads=["sc2", "sv"], writes=["si"])
            P.cp('dve', sif[:], si[:], ["si"], ["sif"])
            P.tt('dve', cand[:].rearrange("p h (a b) -> p h a b", b=16), sv4[:, :, 0, :].unsqueeze(3).to_broadcast(B4),
                 sv4[:, :, 1, :].unsqueeze(2).to_broadcast(B4), ALU.add, ["sv"], ["cand"])
            for hh in range(8):
                P.op('dve', lambda g: g.max(out=cv[:, hh, 0:8], in_=cand[:, hh, :]), reads=["cand"], writes=["cv"])
                P.op('dve', lambda g: g.max_index(out=ci[:, hh, 0:8], in_max=cv[:, hh, 0:8], in_values=cand[:, hh, :]),
                     reads=["cand", "cv"], writes=["ci"])
                P.op('dve', lambda g: g.match_replace(out=cand2[:, hh, :], in_to_replace=cv[:, hh, 0:8], in_values=cand[:, hh, :],
                                                      imm_value=NEG), reads=["cand", "cv"], writes=["sc2"])
                P.op('dve', lambda g: g.max(out=cv[:, hh, 8:16], in_=cand2[:, hh, :]), reads=["sc2"], writes=["cv"])
                P.op('dve', lambda g: g.max_index(out=ci[:, hh, 8:16], in_max=cv[:, hh, 8:16], in_values=cand2[:, hh, :]),
                     reads=["sc2", "cv"], writes=["ci"])
            P.op('dve', lambda g: g.tensor_single_scalar(out=cab[:, 0], in_=ci[:], scalar=4, op=ALU.logical_shift_right),
                 reads=["ci"], writes=["cab"])
            P.op('dve', lambda g: g.tensor_single_scalar(out=cab[:, 1], in_=ci[:], scalar=15, op=ALU.bitwise_and),
                 reads=["ci"], writes=["cab"])
            P.cp('dve', cabf[:], cab[:], ["cab"], ["cabf"])
            OH4 = OH.rearrange("p h (a b) -> p h a b", b=16)
            for w_ in range(2):
                P.tt('dve', OH4, iota[:].unsqueeze(1).unsqueeze(1).to_broadcast(B4), cabf[:, w_].unsqueeze(3).to_broadcast(B4),
                     ALU.is_equal, ["iota", "cabf"], ["sc"])
                P.tt('dve', OH4, OH4, sif4[:, :, w_, :].unsqueeze(2).to_broadcast(B4), ALU.mult, ["sc", "sif"], ["sc"])
                P.op('dve', lambda g: g.tensor_reduce(out=i12[:, w_], in_=OH4, axis=AX.X, op=ALU.add), reads=["sc"], writes=["i12"])
            P.stt(ef[:].rearrange("p (h k) -> p h k", k=16), i12[:, 0], 128.0, i12[:, 1], ALU.mult, ALU.add, ["i12"], ["ef"])
            P.cp('dve', eidx[:], ef[:], ["ef"], ["eidx"])
            P.tt('dve', gt[:], cv[:], cv[:, :, 0:1].to_broadcast([128, 8, 16]), ALU.subtract, ["cv"], ["gt"])
            P.actf(gt[:], gt[:], AF.Exp, ["gt"], ["gt"])
            P.op('dve', lambda g: g.tensor_reduce(out=gs[:], in_=gt[:], axis=AX.X, op=ALU.add), reads=["gt"], writes=["gs"])
            P.op('dve', lambda g: g.reciprocal(out=gs[:], in_=gs[:]), reads=["gs"], writes=["gs"])
            P.tt('dve', gt[:], gt[:], gs[:].unsqueeze(2).to_broadcast([128, 8, 16]), ALU.mult, ["gt", "gs"], ["gt"])
            GS = 2
            NG = 128 // GS
            gtf = gt[:].rearrange("p h k -> p (h k)")
            for g in range(NG + 2):
                if g < NG:
                    for i in range(GS):
                        k = g * GS + i
                        sl = (g % 6) * GS + i
                        P.dma('pool', f"guv{sl}", lambda e: e.indirect_dma_start(
                            out=uvg[sl][:], out_offset=None, in_=uv_d[:, :],
                            in_offset=bass.IndirectOffsetOnAxis(ap=eidx[:, k:k + 1], axis=0)), reads=["eidx"], writes=[f"uvg{sl}"])
                        P.op('dve', lambda g_: g_.scalar_tensor_tensor(out=junk[:], in0=uvg[sl][:, 0:D], scalar=1.0, in1=M[:],
                                                                       op0=ALU.mult, op1=ALU.mult, accum_out=dots[:, k:k + 1]),
                             reads=[f"uvg{sl}", "M"], writes=[f"dots{g % 4}"])
                elif g == NG:
                    P.op('dve', lambda g_: g_.memset(gs[:, 0:1], 0.0), writes=["dfence"])
                if 1 <= g <= NG:
                    gq = g - 1
                    ks = slice(gq * GS, (gq + 1) * GS)
                    fence = [f"dots{g % 4}"] if g < NG else ["dfence"]
                    P.actf(actv[:, ks], dots[:, ks], AF.Gelu, [f"dots{gq % 4}"] + fence, [f"actv{gq % 4}"])
                if g >= 2:
                    gp = g - 2
                    ks = slice(gp * GS, (gp + 1) * GS)
                    P.tt('dve', actv[:, ks], actv[:, ks], gtf[:, ks], ALU.mult, [f"actv{gp % 4}", "gt"], [f"actv{gp % 4}"])
                    for i in range(GS):
                        k = gp * GS + i
                        sl = (gp % 6) * GS + i
                        pk, pkid = Pk[k % 4], f"Pk{k % 4}"
                        P.actf(pk[:], uvg[sl][:, D:2 * D], AF.Identity, [f"uvg{sl}", f"actv{gp % 4}"], [pkid], scale=actv[:, k:k + 1])
                        for hf in range(2):
                            P.mm(pa[hf][:, 0:512], identb[:], pk[:, hf * 512:(hf + 1) * 512], k == 0, k == 127,
                                 [pkid, "identb"], [f"psb{6 + hf}"])
            for hf in range(2):
                P.tt('dve', acc[:, hf * 512:(hf + 1) * 512], pa[hf][:, 0:512], mod[:, 2, hf * 512:(hf + 1) * 512], ALU.mult,
                     [f"psb{6 + hf}", mid], ["acc"])
            P.stt(acc[:], hs[s][:], ALPHA, acc[:], ALU.mult, ALU.add, ["h0", "acc"], ["acc"])
            layernorm(P, acc[:], "acc", lng[:], lnb[:], O[s][:], "Q", stats, mv)
            P.store("st_o0", Hout[rows, :], O[s][:], "Q")


def emit_cvt(P, src, dst, pfx):
    with P.scope(pfx):
        NSL = 3
        fin = [P.sb(f"fin{i}", [128, 2 * D]) for i in range(NSL)]
        fout = [P.sb(f"fout{i}", [128, 2 * D], BF16) for i in range(NSL)]
        engs = ('dve', 'act', 'pool')
        for r in range(128):
            sl = r % NSL
            rows = slice(r * 128, (r + 1) * 128)
            P.load(f"ld_f{sl}", fin[sl][:], src[rows, :], f"fin{sl}")
            P.cp(engs[r % 3], fout[sl][:], fin[sl][:], [f"fin{sl}"], [f"fout{sl}"])
            P.store(f"st_f{sl}", dst[rows, :], fout[sl][:], f"fout{sl}")


def emit_mlaproj(P, I, modsB, H2, kT_s, krT_s, v_s, qT_s, qrT_s):
    with P.scope("mp"):
        ident = P.sb("ident", [128, 128]); mL = P.sb("mL", [128, 2, D]); mC = P.sb("mC", [128, 2, D])
        wdq = P.sb("wdq", [128, 8, 384]); qn = P.sb("qn", [128, 384]); wuq = P.sb("wuq", [128, 3, 1536])
        wdkv = P.sb("wdkv", [128, 8, 320]); kvn = P.sb("kvn", [128, 256]); wukv = P.sb("wukv", [128, 2, 2048])
        own = P.sb("own", [128, 16], U32)
        P.load("ld_c", ident[:], I["ident"], "ident"); P.load("ld_c", own[:], I["ownidx"], "own")
        P.load("ld_c", mL[:], modsB[1, 0, :, 0:2048].rearrange("p (m d) -> p m d", d=D), "mL")
        P.load("ld_c", mC[:], modsB[1, 1, :, 0:2048].rearrange("p (m d) -> p m d", d=D), "mC")
        P.load("ld_c", qn[:], I["qn"], "qn"); P.load("ld_c", kvn[:], I["kvn"], "kvn")
        for dst, key, nm in ((wdq, "wdq", "wdq"), (wuq, "wuq", "wuq"), (wdkv, "wdkv", "wdkv"), (wukv, "wukv", "wukv")):
            P.load("ld_w", dst[:], I[key].rearrange("(kc p) n -> p kc n", p=128), nm)
        P.ts('dve', mL[:, 1, :], mL[:, 1, :], 1.0, None, ALU.add, None, ["mL"], ["mL"])
        P.ts('dve', mC[:, 1, :], mC[:, 1, :], 1.0, None, ALU.add, None, ["mC"], ["mC"])
        hs = [P.sb(f"h{i}", [128, D]) for i in range(2)]
        cs = [P.sb(f"cs{i}", [128, 2, 32]) for i in range(2)]
        M = P.sb("M", [128, D]); T = P.sb("T", [128, 8, 128]); T2 = P.sb("T2", [128, 3, 128])
        cq = P.sb("cq", [128, 384]); cqn = P.sb("cqn", [128, 384]); junk = P.sb("junk", [128, 384]); ss = P.sb("ss", [128, 4])
        Qf = P.sb("Qf", [128, 1536]); QR = P.sb("QR", [128, 8, 64]); kv = P.sb("kv", [128, 320]); ckn = P.sb("ckn", [128, 256])
        KVf = P.sb("KVf", [128, 2048]); KRf = P.sb("KRf", [128, 64])
        rt = P.sb("rt", [128, 4, 8, 32])
        KT = [P.sb(f"KT{i}", [128, 8, 128], BF16) for i in range(2)]
        KRT = [P.sb(f"KRT{i}", [64, 128], BF16) for i in range(2)]
        V9 = [P.sb(f"V9{i}", [128, 8, 129], BF16) for i in range(2)]
        QT = [P.sb(f"QT{i}", [128, 8, 128], BF16) for i in range(2)]
        QRT = [P.sb(f"QRT{i}", [64, 8, 128], BF16) for i in range(2)]
        for i in range(2):
            P.op('dve', lambda g: g.memset(V9[i][:], 1.0), writes=[f"V9{i}"])
        tring = PsumRing(P, range(0, 4)); lring = PsumRing(P, range(4, 8))
        KVf4 = KVf[:].rearrange("p (h d) -> p h d", d=256)
        for t in range(NTB):
            s = t % 2
            rows = slice(t * 128, (t + 1) * 128)
            cols = slice(t * 128, (t + 1) * 128)
            mod, mid = (mC, "mC") if t >= 64 else (mL, "mL")
            P.load(f"ld_h{s}", hs[s][:], H2[rows, :], f"h{s}")
            P.load(f"ld_cs{s}", cs[s][:, 0, :], I["cos"][rows, :], f"cs{s}")
            P.load(f"ld_cs{s}", cs[s][:, 1, :], I["sin"][rows, :], f"cs{s}")
            P.tt('dve', M[:], hs[s][:], mod[:, 1, :], ALU.mult, [f"h{s}", mid], ["M"])
            P.tt('dve', M[:], M[:], mod[:, 0, :], ALU.add, ["M", mid], ["M"])
            transpose_tile(P, tring, M, "M", T, "T", 8, ident)
            linear(P, lring, T, "T", wdkv, "wdkv", 8, 320, lambda nb, c0, ps, pid, n: P.cp('act', kv[:, c0:c0 + n], ps, [pid], ["kv"]))
            rmsnorm(P, kv[:, 0:256], "kv", 256, kvn[:], "kvn", ckn[:], "ckn", junk[:, 0:256], ss)
            rope(P, kv[:, 256:288], kv[:, 288:320], cs[s][:, 0, :], cs[s][:, 1, :], KRf[:, 0:32], KRf[:, 32:64],
                 [rt[:, i, 0, :] for i in range(4)], ["kv", f"cs{s}"], ["KRf"])
            pt, pid = tring.get()
            P.tr(pt[0:64, 0:128], KRf[:, 0:64], ident[:], ["KRf", "ident"], [pid])
            P.cp('act', KRT[s][:], pt[0:64, 0:128], [pid], [f"KRT{s}"])
            P.store(f"st_kr{s}", krT_s[:, cols], KRT[s][:], f"KRT{s}")
            transpose_tile(P, tring, ckn, "ckn", T2, "T2", 2, ident)
            linear(P, lring, T2, "T2", wukv, "wukv", 2, 2048,
                   lambda nb, c0, ps, pid, n: P.cp('act' if nb % 2 else 'dve', KVf[:, c0:c0 + n], ps, [pid], ["KVf"]))
            for hh in range(8):
                pt, pid = tring.get()
                P.tr(pt[:, 0:128], KVf[:, hh * 256:hh * 256 + 128], ident[:], ["KVf", "ident"], [pid])
                P.cp('act' if hh % 2 else 'dve', KT[s][:, hh, :], pt[:, 0:128], [pid], [f"KT{s}"])
            P.store(f"st_kt{s}", kT_s[:, :, cols].rearrange("h d t -> d h t"), KT[s][:], f"KT{s}")
            P.cp('dve', V9[s][:, :, 0:128], KVf4[:, :, 128:256], ["KVf"], [f"V9{s}"])
            P.store(f"st_v{s}", v_s[:, :, t * 129:(t + 1) * 129].rearrange("h p c -> p h c"), V9[s][:], f"V9{s}")
        for i in range(16):
            s = i % 2
            rows = slice(i * 128, (i + 1) * 128)
            P.dma('pool', f"gh{s}", lambda e: e.indirect_dma_start(
                out=hs[s][:], out_offset=None, in_=H2[:, :],
                in_offset=bass.IndirectOffsetOnAxis(ap=own[:, i:i + 1], axis=0)), reads=["own", "H2"], writes=[f"h{s}"])
            P.load(f"ld_cs{s}", cs[s][:, 0, :], I["cos_own"][rows, :], f"cs{s}")
            P.load(f"ld_cs{s}", cs[s][:, 1, :], I["sin_own"][rows, :], f"cs{s}")
            P.tt('dve', M[:], hs[s][:], mL[:, 1, :], ALU.mult, [f"h{s}", "mL"], ["M"])
            P.tt('dve', M[:], M[:], mL[:, 0, :], ALU.add, ["M", "mL"], ["M"])
            transpose_tile(P, tring, M, "M", T, "T", 8, ident)
            linear(P, lring, T, "T", wdq, "wdq", 8, 384, lambda nb, c0, ps, pid, n: P.cp('act', cq[:, c0:c0 + n], ps, [pid], ["cq"]))
            rmsnorm(P, cq[:], "cq", 384, qn[:], "qn", cqn[:], "cqn", junk[:], ss)
            transpose_tile(P, tring, cqn, "cqn", T2, "T2", 3, ident)
            linear(P, lring, T2, "T2", wuq, "wuq", 3, 1536,
                   lambda nb, c0, ps, pid, n: P.cp('act' if nb % 2 else 'dve', Qf[:, c0:c0 + n], ps, [pid], ["Qf"]))
            Qf4 = Qf[:].rearrange("p (h d) -> p h d", d=192)
            cosb = cs[s][:, 0, :].unsqueeze(1).to_broadcast([128, 8, 32])
            sinb = cs[s][:, 1, :].unsqueeze(1).to_broadcast([128, 8, 32])
            rope(P, Qf4[:, :, 128:160], Qf4[:, :, 160:192], cosb, sinb, QR[:, :, 0:32], QR[:, :, 32:64],
                 [rt[:, j] for j in range(4)], ["Qf", f"cs{s}"], ["QR"])
            for hh in range(8):
                pt, pid = tring.get()
                P.tr(pt[:, 0:128], Qf[:, hh * 192:hh * 192 + 128], ident[:], ["Qf", "ident"], [pid])
                P.cp('act' if hh % 2 else 'dve', QT[s][:, hh, :], pt[:, 0:128], [pid], [f"QT{s}"])
                pt, pid = tring.get()
                P.tr(pt[0:64, 0:128], QR[:, hh, :], ident[:], ["QR", "ident"], [pid])
                P.cp('dve' if hh % 2 else 'act', QRT[s][:, hh, :], pt[0:64, 0:128], [pid], [f"QRT{s}"])
            P.store(f"st_qt{s}", qT_s[:, :, rows].rearrange("h d t -> d h t"), QT[s][:], f"QT{s}")
            P.store(f"st_qr{s}", qrT_s[:, :, rows].rearrange("h d t -> d h t"), QRT[s][:], f"QRT{s}")


def emit_attn(P, I, modsB, H2, kT_d, krT_d, v_d, qT_d, qrT_d, H3):
    with P.scope("at"):
        ident = P.sb("ident", [128, 128]); gate = P.sb("gate", [128, D]); wo = P.sb("wo", [128, 8, D])
        lng = P.sb("lng", [128, D]); lnb = P.sb("lnb", [128, D]); own = P.sb("own", [128, 16], U32)
        krTa = P.sb("krTa", [65, NK], BF16)
        P.load("ld_c", ident[:], I["ident"], "ident"); P.load("ld_c", own[:], I["ownidx"], "own")
        P.load("ld_c", gate[:], modsB[1, 0, :, 2048:3072], "gate")
        P.load("ld_c", lng[:], I["lng"][2], "lng"); P.load("ld_c", lnb[:], I["lnb"][2], "lnb")
        P.load("ld_wo", wo[:], I["wo_mla"].rearrange("(kc p) n -> p kc n", p=128), "wo")
        P.load("ld_kr", krTa[0:64, :], krT_d, "krTa")
        P.op('dve', lambda g: g.memset(krTa[64:65, :], 1.0), writes=["krTa"])
        kT = [P.sb(f"kT{i}", [128, NK], BF16) for i in range(2)]
        vh = [P.sb(f"vh{i}", [128, NKT * 129], BF16) for i in range(2)]
        qT = [P.sb(f"qT{i}", [128, 512], BF16) for i in range(2)]
        qrTa = [P.sb(f"qrTa{i}", [65, 512], BF16) for i in range(2)]
        NPT = 3
        PT = [P.sb(f"PT{i}", [128, 512], BF16) for i in range(NPT)]
        mx = P.sb("mx", [128, 20]); nmp = P.sb("nmp", [128, 65]); rden = P.sb("rden", [128, 4])
        otok = P.sb("otok", [128, 4, D])
        hs = [P.sb("hs0", [128, D])] * 2
        A = P.sb("A", [128, D]); T = P.sb("T", [128, 8, 128])
        O = [P.sb("O0", [128, D])] * 2
        stats = P.sb("stats", [128, 2, 6]); mv = P.sb("mv", [128, 4])
        sring = PsumRing(P, range(0, 3)); mring = PsumRing(P, range(3, 4)); oring = PsumRing(P, range(4, 8))
        P.op('dve', lambda g: g.memset(nmp[:], 0.0), writes=["nmp"])
        kblocks = [(512 * i, 512) for i in range(16)] + [(8192, 256)]
        it = 0
        ipt = 0
        tcount = 0
        for qb in range(4):
            for hh in range(8):
                s = it % 2
                it += 1
                P.load(f"ld_k{s}", kT[s][:], kT_d[hh], f"kT{s}")
                P.load(f"ld_v{s}", vh[s][:], v_d[hh], f"vh{s}")
                P.load(f"ld_q{s}", qT[s][:], qT_d[hh, :, qb * 512:(qb + 1) * 512], f"qT{s}")
                P.load(f"ld_qr{s}", qrTa[s][0:64, :], qrT_d[hh, :, qb * 512:(qb + 1) * 512], f"qrTa{s}")
                for qs in range(4):
                    qc = slice(qs * 128, (qs + 1) * 128)
                    for kb, (k0, n) in enumerate(kblocks):
                        pt, pid = sring.get()
                        P.mm(pt[:, 0:n], qT[s][:, qc], kT[s][:, k0:k0 + n], True, False, [f"qT{s}", f"kT{s}"], [pid])
                        P.mm(pt[:, 0:n], qrTa[s][0:64, qc], krTa[0:64, k0:k0 + n], False, True, [f"qrTa{s}", "krTa"], [pid])
                        P.op('dve', lambda g: g.tensor_reduce(out=mx[:, kb:kb + 1], in_=pt[:, 0:n], axis=AX.X, op=ALU.max),
                             reads=[pid], writes=["mx"])
                    P.op('dve', lambda g: g.tensor_reduce(out=mx[:, 17:18], in_=mx[:, 0:17], axis=AX.X, op=ALU.max),
                         reads=["mx"], writes=["mx"])
                    P.ts('dve', nmp[:, 64:65], mx[:, 17:18], -1.0, None, ALU.mult, None, ["mx"], ["nmp"])
                    pt, pid = mring.get()
                    P.mm(pt[0:65, 0:128], nmp[:, :], ident[:], True, True, ["nmp", "ident"], [pid])
                    P.cp('act', qrTa[s][64:65, qc], pt[64:65, 0:128], [pid], [f"qrTa{s}"])
                ops = [oring.get() for _ in range(4)]
                for kt in range(NKT):
                    kc = slice(kt * 128, (kt + 1) * 128)
                    pt, pid = sring.get()
                    P.mm(pt[:, 0:512], kT[s][:, kc], qT[s][:, :], True, False, [f"kT{s}", f"qT{s}"], [pid])
                    P.mm(pt[:, 0:512], krTa[0:65, kc], qrTa[s][0:65, :], False, True, ["krTa", f"qrTa{s}"], [pid])
                    pi = ipt % NPT
                    ipt += 1
                    P.actf(PT[pi][:], pt[:, 0:512], AF.Exp, [pid], [f"PT{pi}"], scale=ATT_SCALE)
                    for qs in range(4):
                        ot, oid = ops[qs]
                        P.mm(ot[:, 0:129], PT[pi][:, qs * 128:(qs + 1) * 128], vh[s][:, kt * 129:(kt + 1) * 129],
                             kt == 0, kt == NKT - 1, [f"PT{pi}", f"vh{s}"], [oid])
                for qs in range(4):
                    ot, oid = ops[qs]
                    P.op('dve', lambda g: g.reciprocal(out=rden[:, qs:qs + 1], in_=ot[:, 128:129]), reads=[oid], writes=["rden"])
                    P.ts('dve', otok[:, qs, hh * 128:(hh + 1) * 128], ot[:, 0:128], rden[:, qs:qs + 1], None, ALU.mult, None,
                         [oid, "rden"], ["otok"])
            for qs in range(4):
                s2 = tcount % 2
                ti = qb * 4 + qs
                tcount += 1
                rows = slice(ti * 128, (ti + 1) * 128)
                P.dma('pool', "gh0", lambda e: e.indirect_dma_start(
                    out=hs[s2][:], out_offset=None, in_=H2[:, :],
                    in_offset=bass.IndirectOffsetOnAxis(ap=own[:, ti:ti + 1], axis=0)), reads=["own"], writes=["hs0"])
                for kc in range(8):
                    pt, pid = sring.get()
                    P.tr(pt[:, 0:128], otok[:, qs, kc * 128:(kc + 1) * 128], ident[:], ["otok", "ident"], [pid])
                    P.cp('act' if kc % 2 == 0 else 'dve', T[:, kc, :], pt[:, 0:128], [pid], ["T"])
                linear(P, sring, T, "T", wo, "wo", 8, D,
                       lambda nb, c0, ps, pid, n: P.tt('dve', A[:, c0:c0 + n], ps, gate[:, c0:c0 + n], ALU.mult, [pid, "gate"], ["A"]))
                P.stt(A[:], hs[s2][:], ALPHA, A[:], ALU.mult, ALU.add, ["hs0", "A"], ["A"])
                layernorm(P, A[:], "A", lng[:], lnb[:], O[s2][:], "O0", stats, mv)
                P.store("st_o0", H3[rows, :], O[s2][:], "O0")


IN_SPECS = [
    ("cT", [128, 8, 2], F32), ("aw", [2, 1024, 6144], F32), ("abB", [128, 2, 6144], F32), ("abT", [128, 16], F32),
    ("xT", [8, 128, SEQ_T], F32), ("x_tok", [NROW, D], F32),
    ("a_re", [8, 128, 8], F32), ("a_im", [8, 128, 8], F32), ("ldt", [8, 128, 8], F32),
    ("bre", [8, 128, 8, 16], F32), ("bim", [8, 128, 8, 16], F32), ("cre", [8, 128, 8, 16], F32), ("cim", [8, 128, 8, 16], F32),
    ("dvec", [8, 128, 1], F32), ("ident", [128, 128], F32), ("iota16", [128, 16], F32),
    ("wglu", [D, D], F32), ("wo_s5", [D, D], F32), ("lng", [4, 128, D], F32), ("lnb", [4, 128, D], F32),
    ("peer_wq", [2, D, D], F32), ("kbd", [2, 128, 8, 256], F32), ("peer_uv0", [16384, 2 * D], F32), ("peer_uv1", [16384, 2 * D], F32),
    ("wdq", [D, 384], F32), ("qn", [128, 384], F32), ("wuq", [384, 1536], F32), ("wdkv", [D, 320], F32),
    ("kvn", [128, 256], F32), ("wukv", [256, 2048], F32), ("wo_mla", [D, D], F32),
    ("cos", [NROW, 32], F32), ("sin", [NROW, 32], F32), ("cos_own", [2048, 32], F32), ("sin_own", [2048, 32], F32),
    ("ownidx", [128, 16], U32),
]


def build_fused():
    P = Prog()
    I = {name: P.din(name, shape, dt) for name, shape, dt in IN_SPECS}
    out = P.dout("out", [2048, D])
    modsB = P.scratch("modsB", [2, 2, 128, 6144])
    YF = P.scratch("YF", [NROW, D]); YB = P.scratch("YB", [NROW, D])
    H1 = P.scratch("H1", [NROW, D]); H2 = P.scratch("H2", [NROW, D]); H3 = P.scratch("H3", [2048, D])
    kT_s = P.scratch("kT_s", [8, 128, NK], BF16); krT_s = P.scratch("krT_s", [64, NK], BF16)
    v_s = P.scratch("v_s", [8, 128, NKT * 129], BF16)
    qT_s = P.scratch("qT_s", [8, 128, 2048], BF16); qrT_s = P.scratch("qrT_s", [8, 64, 2048], BF16)
    uvb = [P.scratch(f"uvb{l}", [16384, 2 * D], BF16) for l in range(2)]
    for l in range(2):
        emit_cvt(P, I[f"peer_uv{l}"], uvb[l], f"cv{l}")
    emit_mods(P, I["cT"], I["aw"], I["abB"], modsB)
    emit_s5(P, I, YF, YB)
    emit_s5out(P, I, modsB, YF, YB, H1)
    emit_peer(P, I, modsB, 0, H1, H2, NTB, "p0", uvb[0])
    emit_mlaproj(P, I, modsB, H2, kT_s, krT_s, v_s, qT_s, qrT_s)
    emit_attn(P, I, modsB, H2, kT_s, krT_s, v_s, qT_s, qrT_s, H3)
    emit_peer(P, I, modsB, 1, H3, out, 16, "p1", uvb[1])
    P.finish()
    return P


def bc(v):
    return np.ascontiguousarray(np.broadcast_to(v[None, :], (128, v.shape[0])).astype(np.float32))


def rope_tables():
    n_freq = 16
    inv = (10000.0 ** (-np.arange(n_freq, dtype=np.float32) / n_freq)).astype(np.float32)
    r, col = np.meshgrid(np.arange(128, dtype=np.float32), np.arange(64, dtype=np.float32), indexing='ij')
    ang = np.concatenate([r.reshape(-1, 1) * inv, col.reshape(-1, 1) * inv], axis=-1).astype(np.float32)
    return np.cos(ang).astype(np.float32), np.sin(ang).astype(np.float32)


def kernel(x, c, ctx, c_ctx, ada_w, ada_b, ln_g, ln_b,
           s5_a_re, s5_a_im, s5_log_dt, s5_b_re, s5_b_im, s5_c_re, s5_c_im, s5_d, s5_w_glu, s5_w_o,
           mla_w_dq, mla_q_norm, mla_w_uq, mla_w_dkv, mla_kv_norm, mla_w_ukv, mla_w_o,
           peer_w_q, peer_keys, peer_u, peer_v):
    f = lambda a: np.ascontiguousarray(np.asarray(a, dtype=np.float32))
    x, c, ctx, c_ctx, ada_w, ada_b, ln_g, ln_b = map(f, (x, c, ctx, c_ctx, ada_w, ada_b, ln_g, ln_b))
    P = build_fused()
    def st(a):
        return np.ascontiguousarray(f(a).reshape(2, 8, 4, 2, 64).transpose(1, 3, 4, 0, 2).reshape(8, 128, 8))

    def bl(a):
        return np.ascontiguousarray(f(a).reshape(2, 8, 4, 2, 64, 16).transpose(1, 3, 4, 0, 2, 5).reshape(8, 128, 8, 16))
    kbd = np.zeros((2, 128, 8, 256), dtype=np.float32)
    pk = f(peer_keys)
    for l in range(2):
        for s_ in range(2):
            kbd[l, 64 * s_:64 * s_ + 64, :, 128 * s_:128 * s_ + 128] = pk[l, :, s_].transpose(2, 0, 1)
    cosl, sinl = rope_tables()
    cos_all = np.ones((NROW, 32), np.float32); sin_all = np.zeros((NROW, 32), np.float32)
    cos_all[:8192] = cosl; sin_all[:8192] = sinl
    common = {
        "aw": ada_w, "abB": np.ascontiguousarray(np.broadcast_to(ada_b[None], (128, 2, 6144))),
        "abT": np.ascontiguousarray(ada_b[0, :2048].reshape(2, 8, 128).transpose(2, 0, 1).reshape(128, 16)),
        "a_re": st(s5_a_re[0]), "a_im": st(s5_a_im[0]),
        "ldt": st(np.broadcast_to(f(s5_log_dt)[0][:, :, None], (2, 64, 64))),
        "bre": bl(s5_b_re[0]), "bim": bl(s5_b_im[0]),
        "cre": bl(f(s5_c_re)[0].transpose(0, 1, 3, 2)), "cim": bl(f(s5_c_im)[0].transpose(0, 1, 3, 2)),
        "dvec": np.ascontiguousarray(f(s5_d)[0].reshape(8, 128, 1)),
        "ident": np.eye(128, dtype=np.float32), "iota16": bc(np.arange(16, dtype=np.float32)),
        "wglu": f(s5_w_glu)[0], "wo_s5": f(s5_w_o)[0],
        "lng": np.stack([bc(ln_g[0, 0]), bc(ln_g[0, 1]), bc(ln_g[1, 0]), bc(ln_g[1, 1])]),
        "lnb": np.stack([bc(ln_b[0, 0]), bc(ln_b[0, 1]), bc(ln_b[1, 0]), bc(ln_b[1, 1])]),
        "peer_wq": f(peer_w_q), "kbd": kbd, "peer_uv0": np.ascontiguousarray(np.concatenate([f(peer_u)[0], f(peer_v)[0]], axis=1)),
        "peer_uv1": np.ascontiguousarray(np.concatenate([f(peer_u)[1], f(peer_v)[1]], axis=1)),
        "wdq": f(mla_w_dq)[0], "qn": bc(f(mla_q_norm)[0]), "wuq": f(mla_w_uq)[0], "wdkv": f(mla_w_dkv)[0],
        "kvn": bc(f(mla_kv_norm)[0]), "wukv": f(mla_w_ukv)[0], "wo_mla": f(mla_w_o)[0],
        "cos": cos_all, "sin": sin_all,
    }
    per_b = []
    for b in range(2):
        seq = np.concatenate([ctx[b], x[b]], axis=0)
        per_b.append({
            "cT": np.ascontiguousarray(np.stack([c[b], c_ctx], axis=1).reshape(8, 128, 2).transpose(1, 0, 2)),
            "xT": np.ascontiguousarray(seq.T.reshape(8, 128, SEQ_T)),
            "x_tok": np.ascontiguousarray(np.concatenate([x[b], ctx[b]], axis=0)),
        })
    maps = []
    for k in range(NCORES):
        b, j = k // 4, k % 4
        m = dict(common)
        m.update(per_b[b])
        m["cos_own"] = np.ascontiguousarray(cosl[2048 * j:2048 * (j + 1)])
        m["sin_own"] = np.ascontiguousarray(sinl[2048 * j:2048 * (j + 1)])
        m["ownidx"] = np.ascontiguousarray((2048 * j + np.arange(2048, dtype=np.uint32)).reshape(16, 128).T)
        maps.append(m)
    res = P.run(maps)
    out = np.zeros((2, 8192, D), np.float32)
    for k in range(NCORES):
        b, j = k // 4, k % 4
        out[b, 2048 * j:2048 * (j + 1)] = res[k]["out"]
    return out
```

```python
import math
import numpy as np
import ml_dtypes
from contextlib import ExitStack
import concourse.bass as bass
import concourse.mybir as mybir
from concourse.bass_utils import run_bass_kernel_spmd

F32 = mybir.dt.float32
BF16 = mybir.dt.bfloat16
U32 = mybir.dt.uint32
I32 = mybir.dt.int32
AF = mybir.ActivationFunctionType
ALU = mybir.AluOpType
AX = mybir.AxisListType

NCORES = 8
D = 1024
ALPHA = (2 * 2) ** 0.25
LN_EPS = 1e-5
RMS_EPS = 1e-6
NT = 17
TPC = NT * 128


class Prog:
    def __init__(self):
        self.nc = bass.Bass("TRN2", target_bir_lowering=False)
        self.es = ExitStack()
        self.semes = ExitStack()
        nc = self.nc
        self.eng = {'pe': nc.tensor, 'dve': nc.vector, 'act': nc.scalar, 'pool': nc.gpsimd, 'sp': nc.sync}
        self.sems, self.cnt = {}, {}
        self.seen = {e: {} for e in self.eng}
        self.lastw, self.readers = {}, {}
        for e in ('pe', 'dve', 'act', 'pool'):
            self._sem('E_' + e)
        self.npsum = 0

    def _sem(self, key):
        if key not in self.sems:
            self.sems[key] = self.semes.enter_context(self.nc.semaphore(key))
            self.cnt[key] = 0
        return self.sems[key]

    def din(self, name, shape, dt=F32):
        return self.nc.dram_tensor(name, list(shape), dt, kind="ExternalInput").ap()

    def dout(self, name, shape, dt=F32):
        return self.nc.dram_tensor(name, list(shape), dt, kind="ExternalOutput").ap()

    def sb(self, name, shape, dt=F32):
        return self.es.enter_context(self.nc.sbuf_tensor("sb_" + getattr(self, "pfx", "") + name, list(shape), dt))

    def scratch(self, name, shape, dt=F32):
        return self.nc.dram_tensor(name, list(shape), dt).ap()

    def scope(self, pfx):
        prog = self

        class _S:
            def __enter__(s_):
                s_.old = (prog.es, getattr(prog, "pfx", ""))
                prog.es = ExitStack()
                prog.pfx = pfx + "_"
                return prog

            def __exit__(s_, *a):
                prog.barrier()
                prog.es.close()
                prog.es, prog.pfx = s_.old
                return False
        return _S()

    def barrier(self):
        for e in self.eng:
            for k, v in self.cnt.items():
                if v > 0 and self.seen[e].get(k, 0) < v:
                    self.eng[e].wait_ge(self.sems[k], v)
                    self.seen[e][k] = v

    def ps(self, name, shape, dt=F32):
        return self.es.enter_context(self.nc.psum_tensor(name, list(shape), dt))

    def _deps(self, reads, writes):
        deps = {}

        def add(k, v):
            if not k.startswith('E_'):
                v = max(v, self.cnt[k])
            if deps.get(k, 0) < v:
                deps[k] = v
        for b in reads:
            if b in self.lastw:
                add(*self.lastw[b])
        for b in writes:
            if b in self.lastw:
                add(*self.lastw[b])
            for k, v in self.readers.get(b, {}).items():
                add(k, v)
        return deps

    def _wait(self, e, deps, skip=None):
        for k, v in deps.items():
            if k == skip or self.seen[e].get(k, 0) >= v:
                continue
            self.eng[e].wait_ge(self.sems[k], v)
            self.seen[e][k] = v

    def _commit(self, k, v, reads, writes):
        for b in writes:
            self.lastw[b] = (k, v)
            self.readers[b] = {}
        for b in reads:
            r = self.readers.setdefault(b, {})
            if r.get(k, 0) < v:
                r[k] = v

    def op(self, e, fn, reads=(), writes=()):
        key = 'E_' + e
        self._wait(e, self._deps(reads, writes), skip=key if e == 'pe' else None)
        ins = fn(self.eng[e])
        self.cnt[key] += 1
        ins.then_inc(self.sems[key], 1)
        self._commit(key, self.cnt[key], reads, writes)

    def dma(self, q, semkey, fn, reads=(), writes=()):
        self._sem(semkey)
        self._wait(q, self._deps(reads, writes))
        ins = fn(self.eng[q])
        self.cnt[semkey] += 16
        ins.then_inc(self.sems[semkey], 16)
        self._commit(semkey, self.cnt[semkey], reads, writes)

    def finish(self):
        for k, v in self.cnt.items():
            if v > 0 and self.seen['sp'].get(k, 0) < v:
                self.nc.sync.wait_ge(self.sems[k], v)

    def run(self, in_maps):
        res = run_bass_kernel_spmd(self.nc, in_maps, core_ids=list(range(NCORES)))
        return res.results

    def load(self, semkey, dst, src, wid, rid=None, q='sp'):
        self.dma(q, semkey, lambda e: e.dma_start(out=dst, in_=src), reads=(rid,) if rid else (), writes=(wid,))

    def store(self, semkey, dst, src, rid, wid=None, q='sp'):
        self.dma(q, semkey, lambda e: e.dma_start(out=dst, in_=src), reads=(rid,), writes=(wid,) if wid else ())

    def tt(self, e, out, a, b, op, r, w):
        self.op(e, lambda g: g.tensor_tensor(out=out, in0=a, in1=b, op=op), reads=r, writes=w)

    def ts(self, e, out, a, s1, s2, op0, op1, r, w):
        if op1 is None:
            self.op(e, lambda g: g.tensor_scalar(out=out, in0=a, scalar1=s1, scalar2=None, op0=op0), reads=r, writes=w)
        else:
            self.op(e, lambda g: g.tensor_scalar(out=out, in0=a, scalar1=s1, scalar2=s2, op0=op0, op1=op1), reads=r, writes=w)

    def stt(self, out, a, s, b, op0, op1, r, w):
        self.op('dve', lambda g: g.scalar_tensor_tensor(out=out, in0=a, scalar=s, in1=b, op0=op0, op1=op1), reads=r, writes=w)

    def actf(self, out, a, func, r, w, bias=None, scale=None, accum=None):
        kw = {}
        if bias is not None:
            kw['bias'] = bias
        if scale is not None:
            kw['scale'] = scale
        if accum is not None:
            kw['accum_out'] = accum
        self.op('act', lambda g: g.activation(out=out, in_=a, func=func, **kw), reads=r, writes=w)

    def cp(self, e, out, a, r, w):
        if e == 'act':
            self.actf(out, a, AF.Copy, r, w)
        else:
            self.op(e, lambda g: g.tensor_copy(out=out, in_=a), reads=r, writes=w)

    def mm(self, out, lhsT, rhs, start, stop, r, w):
        self.op('pe', lambda g: g.matmul(out, lhsT, rhs, start=start, stop=stop), reads=r, writes=w)

    def tr(self, out, in_, ident, r, w):
        self.op('pe', lambda g: g.transpose(out, in_, ident), reads=r, writes=w)


class PsumRing:
    def __init__(self, P, banks, dt=F32):
        if not hasattr(P, "banks"):
            P.banks = {}
        for i in banks:
            if i not in P.banks:
                P.banks[i] = P.ps(f"psb{i}", [128, 512], dt)
        self.b = list(banks)
        self.P = P
        self.i = 0

    def get(self):
        j = self.b[self.i]
        self.i = (self.i + 1) % len(self.b)
        return self.P.banks[j], f"psb{j}"


NTB = 66
NROW = NTB * 128
SEQ_T = 256 + 8192
CH = 512
TWO_PI = 2.0 * math.pi
MAGIC = 12582912.0
NEG = -1.0e30
NK = 8448
NKT = 66
ATT_SCALE = 192.0 ** -0.5


def sincos(P, th, n, tmp, sin_out, cos_out, ids):
    for which, outt in ((0, sin_out), (1, cos_out)):
        x, u, k, y, m = (tmp[:, j, :n] for j in range(5))
        r = list(ids)
        if which == 0:
            P.cp('dve', x, th, r, r)
        else:
            P.ts('dve', x, th, math.pi / 2, None, ALU.add, None, r, r)
        P.ts('dve', u, x, 1.0 / TWO_PI, None, ALU.mult, None, r, r)
        P.ts('dve', k, u, MAGIC, None, ALU.add, None, r, r)
        P.ts('dve', k, k, MAGIC, None, ALU.subtract, None, r, r)
        P.stt(y, k, -TWO_PI, x, ALU.mult, ALU.add, r, r)
        P.ts('dve', m, y, math.pi, None, ALU.is_gt, None, r, r)
        P.stt(y, m, -TWO_PI, y, ALU.mult, ALU.add, r, r)
        P.ts('dve', m, y, -math.pi, None, ALU.is_lt, None, r, r)
        P.stt(y, m, TWO_PI, y, ALU.mult, ALU.add, r, r)
        P.ts('dve', y, y, math.pi, -math.pi, ALU.min, ALU.max, r, r)
        P.actf(outt, y, AF.Sin, r, r)


def transpose_tile(P, ring, src, sid, dst, did, nk, ident):
    for kc in range(nk):
        pt, pid = ring.get()
        P.tr(pt[:, 0:128], src[:, kc * 128:(kc + 1) * 128], ident[:], [sid, "ident"], [pid])
        P.cp('act' if kc % 2 == 0 else 'dve', dst[:, kc, :], pt[:, 0:128], [pid], [did])


def linear(P, ring, xT, xid, W, wid, nk, N, cb, bs=512):
    nb = 0
    c0 = 0
    while c0 < N:
        n = min(bs, N - c0)
        pt, pid = ring.get()
        for kc in range(nk):
            P.mm(pt[:, 0:n], xT[:, kc, :], W[:, kc, c0:c0 + n], kc == 0, kc == nk - 1, [xid, wid], [pid])
        cb(nb, c0, pt[:, 0:n], pid, n)
        c0 += n
        nb += 1


def layernorm(P, x, xid, gbc, bbc, out, oid, stats, mv):
    for c in range(2):
        P.op('dve', lambda g: g.bn_stats(out=stats[:, c, :], in_=x[:, c * 512:(c + 1) * 512]), reads=[xid], writes=["lnst"])
    P.op('dve', lambda g: g.bn_aggr(out=mv[:, 0:2], in_=stats[:].rearrange("p a b -> p (a b)")), reads=["lnst"], writes=["lnmv"])
    P.ts('dve', mv[:, 2:3], mv[:, 1:2], LN_EPS, None, ALU.add, None, ["lnmv"], ["lnmv"])
    P.actf(mv[:, 2:3], mv[:, 2:3], AF.Sqrt, ["lnmv"], ["lnmv"])
    P.op('dve', lambda g: g.reciprocal(out=mv[:, 3:4], in_=mv[:, 2:3]), reads=["lnmv"], writes=["lnmv"])
    P.ts('dve', x, x, mv[:, 0:1], mv[:, 3:4], ALU.subtract, ALU.mult, [xid, "lnmv"], [xid])
    P.tt('pool', x, x, gbc, ALU.mult, [xid, "lng"], [xid])
    P.tt('pool', out, x, bbc, ALU.add, [xid, "lnb"], [oid])


def rmsnorm(P, x, xid, n, gbc, gid, out, oid, junk, ss):
    P.actf(junk, x, AF.Square, [xid], ["rjunk"], accum=ss[:, 0:1])
    P.actf(ss[:, 3:4], junk[:, 0:1], AF.Copy, ["rjunk"], ["rjunk"])
    P.ts('dve', ss[:, 1:2], ss[:, 0:1], 1.0 / n, RMS_EPS, ALU.mult, ALU.add, ["rjunk"], ["rss"])
    P.actf(ss[:, 1:2], ss[:, 1:2], AF.Sqrt, ["rss"], ["rss"])
    P.op('dve', lambda g: g.reciprocal(out=ss[:, 2:3], in_=ss[:, 1:2]), reads=["rss"], writes=["rss"])
    P.stt(out, x, ss[:, 2:3], gbc, ALU.mult, ALU.mult, [xid, "rss", gid], [oid])


def rope(P, x1, x2, cosb, sinb, o1, o2, t, rid, wid):
    P.tt('dve', t[0], x1, cosb, ALU.mult, rid, ["ropet"])
    P.tt('dve', t[1], x2, sinb, ALU.mult, rid, ["ropet"])
    P.tt('dve', t[2], x2, cosb, ALU.mult, rid, ["ropet"])
    P.tt('dve', t[3], x1, sinb, ALU.mult, rid, ["ropet"])
    P.tt('dve', o1, t[0], t[1], ALU.subtract, ["ropet"], wid)
    P.tt('dve', o2, t[2], t[3], ALU.add, ["ropet"], wid)


def emit_mods(P, cT_d, aw_d, abB_d, modsB):
    with P.scope("md"):
        s = P.sb("s", [128, 8, 2]); srep = P.sb("srep", [128, 8, 2, 128])
        P.load("ld_c", s[:], cT_d, "s")
        P.actf(s[:], s[:], AF.Silu, ["s"], ["s"])
        P.cp('dve', srep[:], s[:].unsqueeze(3).to_broadcast([128, 8, 2, 128]), ["s"], ["srep"])
        w = [P.sb(f"w{i}", [128, 8, 512]) for i in range(2)]
        bb = [P.sb(f"bb{i}", [128, 512]) for i in range(2)]
        ot = [[P.sb(f"ot{v}{i}", [128, 512]) for i in range(2)] for v in range(2)]
        ring = PsumRing(P, range(8))
        it = 0
        for layer in range(2):
            for nb in range(12):
                sl = it % 2
                it += 1
                cols = slice(nb * 512, (nb + 1) * 512)
                P.load(f"ld_w{sl}", w[sl][:], aw_d[layer, :, cols].rearrange("(kc p) n -> p kc n", p=128), f"w{sl}")
                P.load(f"ld_b{sl}", bb[sl][:], abB_d[:, layer, cols], f"bb{sl}")
                for v in range(2):
                    pt, pid = ring.get()
                    for kc in range(8):
                        P.mm(pt[:, 0:512], srep[:, kc, v, :], w[sl][:, kc, :], kc == 0, kc == 7, ["srep", f"w{sl}"], [pid])
                    P.tt('dve', ot[v][sl][:], pt[:, 0:512], bb[sl][:], ALU.add, [pid, f"bb{sl}"], [f"ot{v}{sl}"])
                    P.store(f"st_m{v}{sl}", modsB[layer, v, :, cols], ot[v][sl][:], f"ot{v}{sl}")


def emit_s5(P, I, YF, YB):
    with P.scope("s5"):
        ident = P.sb("ident", [128, 128]); s = P.sb("s", [128, 8, 2])
        P.load("ld_par", ident[:], I["ident"], "ident")
        P.load("ld_par", s[:], I["cT"], "s")
        P.actf(s[:], s[:], AF.Silu, ["s"], ["s"])
        abT = P.sb("abT", [128, 16]); P.load("ld_par", abT[:], I["abT"], "abT")
        wsl = P.sb("wsl", [128, 2, 8, 128])
        sc1 = P.sb("sc1", [128, 2]); sh = P.sb("sh", [128, 2]); dv = P.sb("dv", [128, 1])
        are = P.sb("are", [128, 8]); aim = P.sb("aim", [128, 8]); ldt = P.sb("ldt", [128, 8])
        bre = P.sb("bre", [128, 8, 16]); bim = P.sb("bim", [128, 8, 16])
        cre = P.sb("cre", [128, 8, 16]); cim = P.sb("cim", [128, 8, 16])
        prm = P.sb("prm", [128, 16, 8]); tmp5 = P.sb("tmp5", [128, 5, 8])
        Wre = P.sb("Wre", [128, 8, 128]); Wim = P.sb("Wim", [128, 8, 128])
        Cre = P.sb("Cre", [128, 8, 128]); nCre = P.sb("nCre", [128, 8, 128]); nCim = P.sb("nCim", [128, 8, 128])
        bfull = P.sb("bfull", [128, 2, 128]); tb = P.sb("tb", [128, 2, 16])
        ctab = P.sb("ctab", [128, 8, CH]); stab = P.sb("stab", [128, 8, CH]); rB = P.sb("rB", [128, 8, CH])
        ones = P.sb("ones", [128, CH]); En = P.sb("En", [128, 8, 2, 2]); cur = P.sb("cur", [128, 4]); st = P.sb("st", [128, 4, 2])
        xs = [P.sb(f"xs{i}", [128, CH]) for i in range(2)]
        ms = [P.sb(f"ms{i}", [128, CH]) for i in range(2)]
        ys = [P.sb(f"ys{i}", [128, CH]) for i in range(2)]
        yt = [P.sb(f"yt{i}", [128, 4, 128]) for i in range(2)]
        NW = 3
        va = [P.sb(f"va{i}", [128, 4, CH]) for i in range(NW)]
        G = [P.sb(f"G{i}", [128, 2, CH]) for i in range(NW)]
        X = [P.sb(f"X{i}", [128, 4, CH]) for i in range(NW)]
        ring = PsumRing(P, range(2, 6)); yring = PsumRing(P, range(0, 2)); trring = PsumRing(P, range(6, 8))
        P.op('dve', lambda g: g.memset(ones[:], 1.0), writes=["ones"])
        R = ["prm"]
        it = 0
        wk = 0
        for fc in range(8):
            for dst, key, nm in ((are, "a_re", "are"), (aim, "a_im", "aim"), (ldt, "ldt", "ldt"), (bre, "bre", "bre"),
                                 (bim, "bim", "bim"), (cre, "cre", "cre"), (cim, "cim", "cim"), (dv, "dvec", "dv")):
                P.load("ld_par", dst[:], I[key][fc], nm)
            for which in range(2):
                c0 = which * 1024 + fc * 128
                P.load("ld_wsl", wsl[:, which], I["aw"][0, :, c0:c0 + 128].rearrange("(kc p) n -> p kc n", p=128), "wsl")
            for which, dstt, addc in ((0, sh, 0.0), (1, sc1, 1.0)):
                pt, pid = ring.get()
                for kc in range(8):
                    P.mm(pt[:, 0:2], wsl[:, which, kc, :], s[:, kc, :], kc == 0, kc == 7, ["wsl", "s"], [pid])
                P.ts('dve', dstt[:], pt[:, 0:2], abT[:, which * 8 + fc:which * 8 + fc + 1], addc, ALU.add, ALU.add,
                     [pid, "abT"], ["scsh"])
            dt_, mag, th, sn, cs, abr, abi, den, fre, fim, t0, t1 = (prm[:, j, :] for j in range(12))
            P.actf(dt_, ldt[:], AF.Exp, ["ldt"], R)
            P.tt('dve', t0, are[:], dt_, ALU.mult, ["are"] + R, R)
            P.actf(mag, t0, AF.Exp, R, R)
            P.tt('dve', th, aim[:], dt_, ALU.mult, ["aim"] + R, R)
            sincos(P, th, 8, tmp5, sn, cs, R + ["tmp5"])
            P.tt('dve', abr, mag, cs, ALU.mult, R, R)
            P.tt('dve', abi, mag, sn, ALU.mult, R, R)
            P.tt('dve', den, are[:], are[:], ALU.mult, ["are"] + R, R)
            P.tt('dve', t0, aim[:], aim[:], ALU.mult, ["aim"] + R, R)
            P.tt('dve', den, den, t0, ALU.add, R, R)
            P.op('dve', lambda g: g.reciprocal(out=den, in_=den), reads=R, writes=R)
            P.ts('dve', t0, abr, -1.0, None, ALU.add, None, R, R)
            P.tt('dve', fre, t0, are[:], ALU.mult, ["are"] + R, R)
            P.tt('dve', t1, abi, aim[:], ALU.mult, ["aim"] + R, R)
            P.tt('dve', fre, fre, t1, ALU.add, R, R)
            P.tt('dve', fre, fre, den, ALU.mult, R, R)
            P.tt('dve', fim, abi, are[:], ALU.mult, ["are"] + R, R)
            P.tt('dve', t1, t0, aim[:], ALU.mult, ["aim"] + R, R)
            P.tt('dve', fim, fim, t1, ALU.subtract, R, R)
            P.tt('dve', fim, fim, den, ALU.mult, R, R)
            for j in range(8):
                pair = j % 4
                for which, Wdst in ((0, Wre), (1, Wim)):
                    P.op('dve', lambda g: g.memset(bfull[:, which, :], 0.0), writes=["bfull"])
                    if which == 0:
                        P.ts('dve', tb[:, 0, :], bim[:, j, :], fim[:, j:j + 1], None, ALU.mult, None, ["bim"] + R, ["tb"])
                        src2, op1 = bre, ALU.subtract
                    else:
                        P.ts('dve', tb[:, 0, :], bre[:, j, :], fim[:, j:j + 1], None, ALU.mult, None, ["bre"] + R, ["tb"])
                        src2, op1 = bim, ALU.add
                    P.stt(tb[:, 1, :], src2[:, j, :], fre[:, j:j + 1], tb[:, 0, :], ALU.mult, op1, ["bre", "bim", "tb"] + R, ["tb"])
                    for g2 in range(2):
                        c0 = 32 * pair + 16 * g2
                        P.cp('dve', bfull[64 * g2:64 * g2 + 64, which, c0:c0 + 16], tb[64 * g2:64 * g2 + 64, 1, :], ["tb"], ["bfull"])
                    pt, pid = ring.get()
                    P.tr(pt[:, 0:128], bfull[:, which, :], ident[:], ["bfull", "ident"], [pid])
                    P.cp('act', Wdst[:, j, :], pt[:, 0:128], [pid], ["W"])
                for srcc, dsts in ((cre, (Cre, nCre)), (cim, (None, nCim))):
                    for dd, sgn in zip(dsts, (1.0, -1.0)):
                        if dd is None:
                            continue
                        P.op('dve', lambda g: g.memset(dd[:, j, :], 0.0), writes=["W"])
                        for g2 in range(2):
                            c0 = 32 * pair + 16 * g2
                            P.ts('dve', dd[64 * g2:64 * g2 + 64, j, c0:c0 + 16], srcc[64 * g2:64 * g2 + 64, j, :], sgn, None,
                                 ALU.mult, None, ["cre", "cim"], ["W"])
                P.op('dve', lambda g: g.memset(ctab[:, j, 0:1], 1.0), writes=["tab"])
                P.op('dve', lambda g: g.memset(stab[:, j, 0:1], 0.0), writes=["tab"])
                P.cp('dve', cur[:, 0:1], cs[:, j:j + 1], R, ["cur"])
                P.cp('dve', cur[:, 1:2], sn[:, j:j + 1], R, ["cur"])
                n = 1
                while n <= CH:
                    if n in (256, 512):
                        wi = 0 if n == 256 else 1
                        P.cp('dve', En[:, j, wi, :], cur[:, 0:2], ["cur"], ["En"])
                    if n == CH:
                        break
                    cn, snn = cur[:, 0:1], cur[:, 1:2]
                    T = ["tab", "cur", "rB"]
                    P.ts('dve', rB[:, j, 0:n], stab[:, j, 0:n], snn, None, ALU.mult, None, T, ["rB"])
                    P.stt(ctab[:, j, n:2 * n], ctab[:, j, 0:n], cn, rB[:, j, 0:n], ALU.mult, ALU.subtract, T, ["tab"])
                    P.ts('dve', rB[:, j, 0:n], ctab[:, j, 0:n], snn, None, ALU.mult, None, T, ["rB"])
                    P.stt(stab[:, j, n:2 * n], stab[:, j, 0:n], cn, rB[:, j, 0:n], ALU.mult, ALU.add, T, ["tab"])
                    P.tt('dve', cur[:, 2:3], cn, cn, ALU.mult, ["cur"], ["cur"])
                    P.tt('dve', cur[:, 3:4], snn, snn, ALU.mult, ["cur"], ["cur"])
                    P.tt('dve', cur[:, 3:4], cur[:, 2:3], cur[:, 3:4], ALU.subtract, ["cur"], ["cur"])
                    P.tt('dve', cur[:, 2:3], cn, snn, ALU.mult, ["cur"], ["cur"])
                    P.ts('dve', cur[:, 1:2], cur[:, 2:3], 2.0, None, ALU.mult, None, ["cur"], ["cur"])
                    P.cp('dve', cur[:, 0:1], cur[:, 3:4], ["cur"], ["cur"])
                    n *= 2
                P.ts('dve', rB[:, j, :], ones[:], mag[:, j:j + 1], None, ALU.mult, None, ["ones", "rB"] + R, ["rB"])
            for d_ in range(2):
                Yd = YF if d_ == 0 else YB
                P.op('dve', lambda g: g.memset(st[:], 0.0), writes=["st"])
                items = [(ci, pair) for ci in range(17) for pair in range(4)]
                cinfo = {}

                def chunk_setup(ci):
                    nonlocal it
                    n = 256 if ci == 0 else 512
                    if ci == 0:
                        t0_, row0, mcol = 0, 8192, 1
                    elif d_ == 0:
                        t0_, row0, mcol = 256 + 512 * (ci - 1), 512 * (ci - 1), 0
                    else:
                        t0_, row0, mcol = 256 + 8192 - 512 * ci, 8192 - 512 * ci, 0
                    sl = it % 2
                    it += 1
                    P.load(f"ld_x{sl}", xs[sl][:, :n], I["xT"][fc, :, t0_:t0_ + n], f"xs{sl}")
                    xin = xs[sl][:, :n] if d_ == 0 else xs[sl][:, n - 1::-1]
                    P.ts('dve', ms[sl][:, :n], xin, sc1[:, mcol:mcol + 1], sh[:, mcol:mcol + 1], ALU.mult, ALU.add,
                         [f"xs{sl}", "scsh"], [f"ms{sl}"])
                    yp, yid = yring.get()
                    cinfo[ci] = (n, row0, sl, yp, yid)

                def drive(ci, pair):
                    n, row0, sl, yp, yid = cinfo[ci]
                    j = d_ * 4 + pair
                    pa, aid = ring.get()
                    pb, bid = ring.get()
                    P.mm(pa[:, :n], Wre[:, j, :], ms[sl][:, :n], True, True, ["W", f"ms{sl}"], [aid])
                    P.mm(pb[:, :n], Wim[:, j, :], ms[sl][:, :n], True, True, ["W", f"ms{sl}"], [bid])
                    return pa, aid, pb, bid

                chunk_setup(0)
                pend = drive(0, 0)
                for ii, (ci, pair) in enumerate(items):
                    n, row0, sl, yp, yid = cinfo[ci]
                    pa, aid, pb, bid = pend
                    if ii + 1 < len(items):
                        nci, npair = items[ii + 1]
                        if npair == 0:
                            chunk_setup(nci)
                        pend = drive(nci, npair)
                    j = d_ * 4 + pair
                    w_ = wk % NW
                    wk += 1
                    c_, s_ = ctab[:, j, :n], stab[:, j, :n]
                    V, VI = va[w_], f"va{w_}"
                    P.tt('dve', V[:, 0, :n], pa[:, :n], c_, ALU.mult, [aid, "tab"], [VI])
                    P.tt('dve', V[:, 1, :n], pb[:, :n], s_, ALU.mult, [bid, "tab"], [VI])
                    P.tt('dve', V[:, 0, :n], V[:, 0, :n], V[:, 1, :n], ALU.add, [VI], [VI])
                    P.tt('dve', V[:, 2, :n], pb[:, :n], c_, ALU.mult, [bid, "tab"], [VI])
                    P.tt('dve', V[:, 3, :n], pa[:, :n], s_, ALU.mult, [aid, "tab"], [VI])
                    P.tt('dve', V[:, 2, :n], V[:, 2, :n], V[:, 3, :n], ALU.subtract, [VI], [VI])
                    GG, GI = G[w_], f"G{w_}"
                    P.op('dve', lambda g: g.tensor_tensor_scan(out=GG[:, 0, :n], data0=rB[:, j, :n], data1=V[:, 0, :n],
                                                               initial=st[:, pair, 0:1], op0=ALU.mult, op1=ALU.add),
                         reads=["rB", VI, "st"], writes=[GI])
                    P.op('dve', lambda g: g.tensor_tensor_scan(out=GG[:, 1, :n], data0=rB[:, j, :n], data1=V[:, 2, :n],
                                                               initial=st[:, pair, 1:2], op0=ALU.mult, op1=ALU.add),
                         reads=["rB", VI, "st"], writes=[GI])
                    wi = 0 if n == 256 else 1
                    cn, snn = En[:, j, wi, 0:1], En[:, j, wi, 1:2]
                    P.ts('dve', cur[:, 0:1], GG[:, 1, n - 1:n], snn, None, ALU.mult, None, [GI, "En"], ["cur"])
                    P.stt(st[:, pair, 0:1], GG[:, 0, n - 1:n], cn, cur[:, 0:1], ALU.mult, ALU.subtract, [GI, "En", "cur"], ["st"])
                    P.ts('dve', cur[:, 1:2], GG[:, 0, n - 1:n], snn, None, ALU.mult, None, [GI, "En"], ["cur"])
                    P.stt(st[:, pair, 1:2], GG[:, 1, n - 1:n], cn, cur[:, 1:2], ALU.mult, ALU.add, [GI, "En", "cur"], ["st"])
                    XX, XI = X[w_], f"X{w_}"
                    P.tt('pool', XX[:, 0, :n], GG[:, 0, :n], c_, ALU.mult, [GI, "tab"], [XI])
                    P.tt('pool', XX[:, 1, :n], GG[:, 1, :n], s_, ALU.mult, [GI, "tab"], [XI])
                    P.tt('pool', XX[:, 2, :n], GG[:, 1, :n], c_, ALU.mult, [GI, "tab"], [XI])
                    P.tt('pool', XX[:, 3, :n], GG[:, 0, :n], s_, ALU.mult, [GI, "tab"], [XI])
                    for q_, Wm in enumerate((Cre, nCre, nCim, nCim)):
                        P.mm(yp[:, :n], Wm[:, j, :], XX[:, q_, :n], pair == 0 and q_ == 0, pair == 3 and q_ == 3,
                             ["W", XI], [yid])
                    if pair == 3:
                        if d_ == 0:
                            P.stt(ys[sl][:, :n], ms[sl][:, :n], dv[:, 0:1], yp[:, :n], ALU.mult, ALU.add, [f"ms{sl}", "dv", yid], [f"ys{sl}"])
                        else:
                            P.cp('dve', ys[sl][:, :n], yp[:, n - 1::-1], [yid], [f"ys{sl}"])
                        nblk = n // 128
                        for bk in range(nblk):
                            pt, pid = trring.get()
                            P.tr(pt[:, 0:128], ys[sl][:, bk * 128:(bk + 1) * 128], ident[:], [f"ys{sl}", "ident"], [pid])
                            P.cp('act', yt[sl][:, bk, :], pt[:, 0:128], [pid], [f"yt{sl}"])
                        P.store(f"st_y{sl}", Yd[row0:row0 + n, fc * 128:(fc + 1) * 128].rearrange("(k p) f -> p k f", p=128),
                                yt[sl][:, 0:nblk, :], f"yt{sl}")


def emit_s5out(P, I, modsB, YF, YB, H1, cvt_src=None, cvt_dst=None):
    nt = NTB
    with P.scope("so"):
        cgen = cvt_steps(P, cvt_src, cvt_dst, "c0") if cvt_src is not None else None
        ident = P.sb("ident", [128, 128]); gL = P.sb("gL", [128, D]); gC = P.sb("gC", [128, D])
        wg = P.sb("wg", [128, 8, D], BF16); wo = P.sb("wo", [128, 8, D], BF16); wstage = P.sb("wstage", [128, 8, D]); lng = P.sb("lng", [128, D]); lnb = P.sb("lnb", [128, D])
        P.load("ld_c", ident[:], I["ident"], "ident")
        P.load("ld_c", gL[:], modsB[0, 0, :, 2048:3072], "gL"); P.load("ld_c", gC[:], modsB[0, 1, :, 2048:3072], "gC")
        P.load("ld_c", lng[:], I["lng"][0], "lng"); P.load("ld_c", lnb[:], I["lnb"][0], "lnb")
        P.load("ld_wg", wstage[:], I["wglu"].rearrange("(kc p) n -> p kc n", p=128), "wstage")
        P.cp('dve', wg[:], wstage[:], ["wstage"], ["wg"])
        P.load("ld_wg", wstage[:], I["wo_s5"].rearrange("(kc p) n -> p kc n", p=128), "wstage")
        P.cp('act', wo[:], wstage[:], ["wstage"], ["wo"])
        xs = [P.sb(f"x{i}", [128, D]) for i in range(2)]
        ya = [P.sb(f"ya{i}", [128, D]) for i in range(2)]
        yb_ = [P.sb(f"yb{i}", [128, D]) for i in range(2)]
        A = P.sb("A", [128, D]); B = P.sb("B", [128, D]); C = P.sb("C", [128, D]); T = P.sb("T", [128, 8, 128], BF16)
        O = [P.sb(f"O{i}", [128, D]) for i in range(2)]
        stats = P.sb("stats", [128, 2, 6]); mv = P.sb("mv", [128, 4])
        tring = PsumRing(P, range(0, 4)); lring = PsumRing(P, range(4, 8))
        for t in range(nt):
            s = t % 2
            rows = slice(t * 128, (t + 1) * 128)
            P.load(f"ld_x{s}", xs[s][:], I["x_tok"][rows, :], f"x{s}")
            P.load(f"ld_ya{s}", ya[s][:], YF[rows, :], f"ya{s}")
            P.load(f"ld_yb{s}", yb_[s][:], YB[rows, :], f"yb{s}")
            gate, gid = (gC, "gC") if t >= 64 else (gL, "gL")
            P.tt('dve', A[:], ya[s][:], yb_[s][:], ALU.add, [f"ya{s}", f"yb{s}"], ["A"])
            P.actf(B[:], A[:], AF.Gelu, ["A"], ["B"])
            transpose_tile(P, tring, B, "B", T, "T", 8, ident)
            linear(P, lring, T, "T", wg, "wg", 8, D,
                   lambda nb, c0, ps, pid, n: P.actf(C[:, c0:c0 + n], ps, AF.Sigmoid, [pid], ["C"]))
            P.tt('dve', C[:], C[:], B[:], ALU.mult, ["B", "C"], ["C"])
            transpose_tile(P, tring, C, "C", T, "T", 8, ident)
            linear(P, lring, T, "T", wo, "wo", 8, D,
                   lambda nb, c0, ps, pid, n: P.tt('dve', A[:, c0:c0 + n], ps, gate[:, c0:c0 + n], ALU.mult, [pid, gid], ["A"]))
            P.stt(A[:], xs[s][:], ALPHA, A[:], ALU.mult, ALU.add, [f"x{s}", "A"], ["A"])
            layernorm(P, A[:], "A", lng[:], lnb[:], O[s][:], f"O{s}", stats, mv)
            P.store(f"st_o{s}", H1[rows, :], O[s][:], f"O{s}")
            cvt_advance(cgen, 2)
        cvt_advance(cgen, 128)


def emit_peer(P, I, modsB, layer, Hin, Hout, nt, pfx, uv_d):
    with P.scope(pfx):
        ident = P.sb("ident", [128, 128]); mL = P.sb("mL", [128, 3, D])
        wq = P.sb("wq", [128, 8, D]); kbd = P.sb("kbd", [128, 8, 256]); lng = P.sb("lng", [128, D]); lnb = P.sb("lnb", [128, D])
        iota = P.sb("iota", [128, 16])
        P.load("ld_c", ident[:], I["ident"], "ident")
        P.load("ld_c", mL[:], modsB[layer, 0, :, 3072:6144].rearrange("p (m d) -> p m d", d=D), "mL")
        P.load("ld_c", kbd[:], I["kbd"][layer], "kbd")
        P.load("ld_c", lng[:], I["lng"][2 * layer + 1], "lng"); P.load("ld_c", lnb[:], I["lnb"][2 * layer + 1], "lnb")
        P.load("ld_c", iota[:], I["iota16"], "iota")
        P.load("ld_wq", wq[:], I["peer_wq"][layer].rearrange("(kc p) n -> p kc n", p=128), "wq")
        P.ts('dve', mL[:, 1, :], mL[:, 1, :], 1.0, None, ALU.add, None, ["mL"], ["mL"])
        hs = [P.sb("h0", [128, D])] * 2
        identb = P.sb("identb", [128, 128], BF16)
        Pk = [P.sb(f"Pk{i}", [128, D], BF16) for i in range(4)]
        M = P.sb("M", [128, D]); T = P.sb("T", [128, 8, 128]); Q = P.sb("Q", [128, D])
        sc = P.sb("sc", [128, 16, 128])
        sv = P.sb("sv", [128, 16, 16]); si = P.sb("si", [128, 16, 16], U32); sif = P.sb("sif", [128, 16, 16])

        cv = P.sb("cv", [128, 8, 16]); ci = P.sb("ci", [128, 8, 16], U32); cab = P.sb("cab", [128, 2, 8, 16], U32)
        cabf = P.sb("cabf", [128, 2, 8, 16]); OH = sc[:].rearrange("p a b -> p (a b)").rearrange("p (h c) -> p h c", c=256)
        i12 = P.sb("i12", [128, 2, 8, 16]); ef = P.sb("ef", [128, 128]); eidx = P.sb("eidx", [128, 128], U32)
        gt = P.sb("gt", [128, 8, 16]); gs = P.sb("gs", [128, 8]); dots = P.sb("dots", [128, 128]); actv = P.sb("actv", [128, 128])
        NS = 12
        uvg = [P.sb(f"uvg{i}", [128, 2 * D], BF16) for i in range(NS)]
        junk = Q
        acc = P.sb("acc", [128, D])
        sc2t = P.sb("sc2", [128, 16, 128]); candt = P.sb("cand", [128, 8, 256])
        sc2 = sc2t[:]
        cand2 = sc2t[:].rearrange("p a b -> p (a b)").rearrange("p (h c) -> p h c", c=256)
        cand = candt[:]
        O = [Q] * 2
        stats = P.sb("stats", [128, 2, 6]); mv = P.sb("mv", [128, 4])
        tring = PsumRing(P, range(0, 4)); lring = PsumRing(P, range(4, 6)); aring = PsumRing(P, range(6, 8))
        P.cp('dve', identb[:], ident[:], ["ident"], ["identb"])
        pa = [P.banks[6], P.banks[7]]
        sv4 = sv[:].rearrange("p (h s) k -> p h s k", s=2)
        sif4 = sif[:].rearrange("p (h s) k -> p h s k", s=2)
        B4 = [128, 8, 16, 16]
        for t in range(nt):
            s = t % 2
            rows = slice(t * 128, (t + 1) * 128)
            mod, mid = mL, "mL"
            if t == 64:
                P.load("ld_c", mL[:], modsB[layer, 1, :, 3072:6144].rearrange("p (m d) -> p m d", d=D), "mL")
                P.ts('dve', mL[:, 1, :], mL[:, 1, :], 1.0, None, ALU.add, None, ["mL"], ["mL"])
            P.load("ld_h0", hs[s][:], Hin[rows, :], "h0")
            P.tt('dve', M[:], hs[s][:], mod[:, 1, :], ALU.mult, ["h0", mid], ["M"])
            P.tt('dve', M[:], M[:], mod[:, 0, :], ALU.add, ["M", mid], ["M"])
            transpose_tile(P, tring, M, "M", T, "T", 8, ident)
            linear(P, lring, T, "T", wq, "wq", 8, D,
                   lambda nb, c0, ps, pid, n: P.cp('act', Q[:, c0:c0 + n], ps, [pid], ["Q"]))
            transpose_tile(P, tring, Q, "Q", T, "T", 8, ident)
            for hh in range(8):
                pt, pid = lring.get()
                P.mm(pt[:, 0:256], T[:, hh, :], kbd[:, hh, :], True, True, ["T", "kbd"], [pid])
                P.cp('act', sc[:, 2 * hh:2 * hh + 2, :], pt[:, 0:256].rearrange("p (s n) -> p s n", s=2), [pid], ["sc"])
            for blk in range(16):
                P.op('dve', lambda g: g.max(out=sv[:, blk, 0:8], in_=sc[:, blk, :]), reads=["sc"], writes=["sv"])
                P.op('dve', lambda g: g.max_index(out=si[:, blk, 0:8], in_max=sv[:, blk, 0:8], in_values=sc[:, blk, :]),
                     reads=["sc", "sv"], writes=["si"])
                P.op('dve', lambda g: g.match_replace(out=sc2[:, blk, :], in_to_replace=sv[:, blk, 0:8], in_values=sc[:, blk, :],
                                                      imm_value=NEG), reads=["sc", "sv"], writes=["sc2"])
                P.op('dve', lambda g: g.max(out=sv[:, blk, 8:16], in_=sc2[:, blk, :]), reads=["sc2"], writes=["sv"])
                P.op('dve', lambda g: g.max_index(out=si[:, blk, 8:16], in_max=sv[:, blk, 8:16], in_values=sc2[:, blk, :]),
                     reads=["sc2", "sv"], writes=["si"])
            P.cp('dve', sif[:], si[:], ["si"], ["sif"])
            P.tt('dve', cand[:].rearrange("p h (a b) -> p h a b", b=16), sv4[:, :, 0, :].unsqueeze(3).to_broadcast(B4),
                 sv4[:, :, 1, :].unsqueeze(2).to_broadcast(B4), ALU.add, ["sv"], ["cand"])
            for hh in range(8):
                P.op('dve', lambda g: g.max(out=cv[:, hh, 0:8], in_=cand[:, hh, :]), reads=["cand"], writes=["cv"])
                P.op('dve', lambda g: g.max_index(out=ci[:, hh, 0:8], in_max=cv[:, hh, 0:8], in_values=cand[:, hh, :]),
                     reads=["cand", "cv"], writes=["ci"])
                P.op('dve', lambda g: g.match_replace(out=cand2[:, hh, :], in_to_replace=cv[:, hh, 0:8], in_values=cand[:, hh, :],
                                                      imm_value=NEG), reads=["cand", "cv"], writes=["sc2"])
                P.op('dve', lambda g: g.max(out=cv[:, hh, 8:16], in_=cand2[:, hh, :]), reads=["sc2"], writes=["cv"])
                P.op('dve', lambda g: g.max_index(out=ci[:, hh, 8:16], in_max=cv[:, hh, 8:16], in_values=cand2[:, hh, :]),
                     reads=["sc2", "cv"], writes=["ci"])
            P.op('dve', lambda g: g.tensor_single_scalar(out=cab[:, 0], in_=ci[:], scalar=4, op=ALU.logical_shift_right),
                 reads=["ci"], writes=["cab"])
            P.op('dve', lambda g: g.tensor_single_scalar(out=cab[:, 1], in_=ci[:], scalar=15, op=ALU.bitwise_and),
                 reads=["ci"], writes=["cab"])
            P.cp('dve', cabf[:], cab[:], ["cab"], ["cabf"])
            OH4 = OH.rearrange("p h (a b) -> p h a b", b=16)
            for w_ in range(2):
                P.tt('dve', OH4, iota[:].unsqueeze(1).unsqueeze(1).to_broadcast(B4), cabf[:, w_].unsqueeze(3).to_broadcast(B4),
                     ALU.is_equal, ["iota", "cabf"], ["sc"])
                P.tt('dve', OH4, OH4, sif4[:, :, w_, :].unsqueeze(2).to_broadcast(B4), ALU.mult, ["sc", "sif"], ["sc"])
                P.op('dve', lambda g: g.tensor_reduce(out=i12[:, w_], in_=OH4, axis=AX.X, op=ALU.add), reads=["sc"], writes=["i12"])
            P.stt(ef[:].rearrange("p (h k) -> p h k", k=16), i12[:, 0], 128.0, i12[:, 1], ALU.mult, ALU.add, ["i12"], ["ef"])
            P.cp('dve', eidx[:], ef[:], ["ef"], ["eidx"])
            P.tt('dve', gt[:], cv[:], cv[:, :, 0:1].to_broadcast([128, 8, 16]), ALU.subtract, ["cv"], ["gt"])
            P.actf(gt[:], gt[:], AF.Exp, ["gt"], ["gt"])
            P.op('dve', lambda g: g.tensor_reduce(out=gs[:], in_=gt[:], axis=AX.X, op=ALU.add), reads=["gt"], writes=["gs"])
            P.op('dve', lambda g: g.reciprocal(out=gs[:], in_=gs[:]), reads=["gs"], writes=["gs"])
            P.tt('dve', gt[:], gt[:], gs[:].unsqueeze(2).to_broadcast([128, 8, 16]), ALU.mult, ["gt", "gs"], ["gt"])
            GS = 2
            NG = 128 // GS
            gtf = gt[:].rearrange("p h k -> p (h k)")
            for g in range(NG + 2):
                if g < NG:
                    for i in range(GS):
                        k = g * GS + i
                        sl = (g % 6) * GS + i
                        P.dma('pool', f"guv{sl}", lambda e: e.indirect_dma_start(
                            out=uvg[sl][:], out_offset=None, in_=uv_d[:, :],
                            in_offset=bass.IndirectOffsetOnAxis(ap=eidx[:, k:k + 1], axis=0)), reads=["eidx"], writes=[f"uvg{sl}"])
                        P.op('dve', lambda g_: g_.scalar_tensor_tensor(out=junk[:], in0=uvg[sl][:, 0:D], scalar=1.0, in1=M[:],
                                                                       op0=ALU.mult, op1=ALU.mult, accum_out=dots[:, k:k + 1]),
                             reads=[f"uvg{sl}", "M"], writes=[f"dots{g % 4}"])
                elif g == NG:
                    P.op('dve', lambda g_: g_.memset(gs[:, 0:1], 0.0), writes=["dfence"])
                if 1 <= g <= NG:
                    gq = g - 1
                    ks = slice(gq * GS, (gq + 1) * GS)
                    fence = [f"dots{g % 4}"] if g < NG else ["dfence"]
                    P.actf(actv[:, ks], dots[:, ks], AF.Gelu, [f"dots{gq % 4}"] + fence, [f"actv{gq % 4}"])
                if g >= 2:
                    gp = g - 2
                    ks = slice(gp * GS, (gp + 1) * GS)
                    P.tt('dve', actv[:, ks], actv[:, ks], gtf[:, ks], ALU.mult, [f"actv{gp % 4}", "gt"], [f"actv{gp % 4}"])
                    for i in range(GS):
                        k = gp * GS + i
                        sl = (gp % 6) * GS + i
                        pk, pkid = Pk[k % 4], f"Pk{k % 4}"
                        P.actf(pk[:], uvg[sl][:, D:2 * D], AF.Identity, [f"uvg{sl}", f"actv{gp % 4}"], [pkid], scale=actv[:, k:k + 1])
                        for hf in range(2):
                            P.mm(pa[hf][:, 0:512], identb[:], pk[:, hf * 512:(hf + 1) * 512], k == 0, k == 127,
                                 [pkid, "identb"], [f"psb{6 + hf}"])
            for hf in range(2):
                P.tt('dve', acc[:, hf * 512:(hf + 1) * 512], pa[hf][:, 0:512], mod[:, 2, hf * 512:(hf + 1) * 512], ALU.mult,
                     [f"psb{6 + hf}", mid], ["acc"])
            P.stt(acc[:], hs[s][:], ALPHA, acc[:], ALU.mult, ALU.add, ["h0", "acc"], ["acc"])
            layernorm(P, acc[:], "acc", lng[:], lnb[:], O[s][:], "Q", stats, mv)
            P.store("st_o0", Hout[rows, :], O[s][:], "Q")


def cvt_steps(P, src, dst, pfx):
    NSL = 2
    fin = [P.sb(f"{pfx}fin{i}", [128, 2 * D]) for i in range(NSL)]
    fout = [P.sb(f"{pfx}fout{i}", [128, 2 * D], BF16) for i in range(NSL)]
    for r in range(128):
        sl = r % NSL
        rows = slice(r * 128, (r + 1) * 128)
        P.load(f"ld_f{sl}", fin[sl][:], src[rows, :], f"{pfx}fin{sl}")
        P.cp('pool', fout[sl][:], fin[sl][:], [f"{pfx}fin{sl}"], [f"{pfx}fout{sl}"])
        P.store(f"st_f{sl}", dst[rows, :], fout[sl][:], f"{pfx}fout{sl}")
        yield r


def cvt_advance(gen, n):
    if gen is None:
        return
    for _ in range(n):
        try:
            next(gen)
        except StopIteration:
            return


def emit_mlaproj(P, I, modsB, H2, kT_s, krT_s, v_s, qT_s, qrT_s, cvt_src=None, cvt_dst=None):
    with P.scope("mp"):
        cgen = cvt_steps(P, cvt_src, cvt_dst, "c1") if cvt_src is not None else None
        ident = P.sb("ident", [128, 128]); mL = P.sb("mL", [128, 2, D]); mC = P.sb("mC", [128, 2, D])
        wdq = P.sb("wdq", [128, 8, 384], BF16); qn = P.sb("qn", [128, 384]); wuq = P.sb("wuq", [128, 3, 1536], BF16)
        wdkv = P.sb("wdkv", [128, 8, 320], BF16); kvn = P.sb("kvn", [128, 256]); wukv = P.sb("wukv", [128, 2, 2048], BF16)
        wstage = P.sb("wstage", [128, 4608])
        own = P.sb("own", [128, 16], U32)
        P.load("ld_c", ident[:], I["ident"], "ident"); P.load("ld_c", own[:], I["ownidx"], "own")
        P.load("ld_c", mL[:], modsB[1, 0, :, 0:2048].rearrange("p (m d) -> p m d", d=D), "mL")
        P.load("ld_c", mC[:], modsB[1, 1, :, 0:2048].rearrange("p (m d) -> p m d", d=D), "mC")
        P.load("ld_c", qn[:], I["qn"], "qn"); P.load("ld_c", kvn[:], I["kvn"], "kvn")
        for wi_, (dst, key, nm, kcs, ncol) in enumerate(((wdq, "wdq", "wdq", 8, 384), (wuq, "wuq", "wuq", 3, 1536),
                                                         (wdkv, "wdkv", "wdkv", 8, 320), (wukv, "wukv", "wukv", 2, 2048))):
            stg = wstage[:, 0:kcs * ncol].rearrange("p (kc n) -> p kc n", n=ncol)
            P.load("ld_w", stg, I[key].rearrange("(kc p) n -> p kc n", p=128), "wstage")
            P.cp('dve' if wi_ % 2 == 0 else 'act', dst[:], stg, ["wstage"], [nm])
        P.ts('dve', mL[:, 1, :], mL[:, 1, :], 1.0, None, ALU.add, None, ["mL"], ["mL"])
        P.ts('dve', mC[:, 1, :], mC[:, 1, :], 1.0, None, ALU.add, None, ["mC"], ["mC"])
        hs = [P.sb(f"h{i}", [128, D]) for i in range(2)]
        cs = [P.sb(f"cs{i}", [128, 2, 32]) for i in range(2)]
        M = P.sb("M", [128, D]); T = P.sb("T", [128, 8, 128], BF16); T2 = P.sb("T2", [128, 3, 128], BF16)
        cq = P.sb("cq", [128, 384]); cqn = P.sb("cqn", [128, 384]); junk = P.sb("junk", [128, 384]); ss = P.sb("ss", [128, 4])
        Qf = P.sb("Qf", [128, 1536]); QR = P.sb("QR", [128, 8, 64]); kv = P.sb("kv", [128, 320]); ckn = P.sb("ckn", [128, 256])
        KVf = P.sb("KVf", [128, 2048]); KRf = P.sb("KRf", [128, 64])
        rt = P.sb("rt", [128, 4, 8, 32])
        KT = [P.sb(f"KT{i}", [128, 8, 128], BF16) for i in range(2)]
        KRT = [P.sb(f"KRT{i}", [64, 128], BF16) for i in range(2)]
        V9 = [P.sb(f"V9{i}", [128, 8, 129], BF16) for i in range(2)]
        QT = [P.sb(f"QT{i}", [128, 8, 128], BF16) for i in range(2)]
        QRT = [P.sb(f"QRT{i}", [64, 8, 128], BF16) for i in range(2)]
        for i in range(2):
            P.op('dve', lambda g: g.memset(V9[i][:], 1.0), writes=[f"V9{i}"])
        tring = PsumRing(P, range(0, 4)); lring = PsumRing(P, range(4, 8))
        KVf4 = KVf[:].rearrange("p (h d) -> p h d", d=256)
        for t in range(NTB):
            s = t % 2
            rows = slice(t * 128, (t + 1) * 128)
            cols = slice(t * 128, (t + 1) * 128)
            mod, mid = (mC, "mC") if t >= 64 else (mL, "mL")
            P.load(f"ld_h{s}", hs[s][:], H2[rows, :], f"h{s}")
            P.load(f"ld_cs{s}", cs[s][:, 0, :], I["cos"][rows, :], f"cs{s}")
            P.load(f"ld_cs{s}", cs[s][:, 1, :], I["sin"][rows, :], f"cs{s}")
            P.tt('dve', M[:], hs[s][:], mod[:, 1, :], ALU.mult, [f"h{s}", mid], ["M"])
            P.tt('dve', M[:], M[:], mod[:, 0, :], ALU.add, ["M", mid], ["M"])
            transpose_tile(P, tring, M, "M", T, "T", 8, ident)
            linear(P, lring, T, "T", wdkv, "wdkv", 8, 320, lambda nb, c0, ps, pid, n: P.cp('act', kv[:, c0:c0 + n], ps, [pid], ["kv"]))
            rmsnorm(P, kv[:, 0:256], "kv", 256, kvn[:], "kvn", ckn[:], "ckn", junk[:, 0:256], ss)
            rope(P, kv[:, 256:288], kv[:, 288:320], cs[s][:, 0, :], cs[s][:, 1, :], KRf[:, 0:32], KRf[:, 32:64],
                 [rt[:, i, 0, :] for i in range(4)], ["kv", f"cs{s}"], ["KRf"])
            pt, pid = tring.get()
            P.tr(pt[0:64, 0:128], KRf[:, 0:64], ident[:], ["KRf", "ident"], [pid])
            P.cp('act', KRT[s][:], pt[0:64, 0:128], [pid], [f"KRT{s}"])
            P.store(f"st_kr{s}", krT_s[:, cols], KRT[s][:], f"KRT{s}")
            transpose_tile(P, tring, ckn, "ckn", T2, "T2", 2, ident)
            linear(P, lring, T2, "T2", wukv, "wukv", 2, 2048,
                   lambda nb, c0, ps, pid, n: P.cp('act' if nb % 2 else 'dve', KVf[:, c0:c0 + n], ps, [pid], ["KVf"]))
            for hh in range(8):
                pt, pid = tring.get()
                P.tr(pt[:, 0:128], KVf[:, hh * 256:hh * 256 + 128], ident[:], ["KVf", "ident"], [pid])
                P.cp('act' if hh % 2 else 'dve', KT[s][:, hh, :], pt[:, 0:128], [pid], [f"KT{s}"])
            P.store(f"st_kt{s}", kT_s[:, :, cols].rearrange("h d t -> d h t"), KT[s][:], f"KT{s}")
            P.cp('dve', V9[s][:, :, 0:128], KVf4[:, :, 128:256], ["KVf"], [f"V9{s}"])
            P.store(f"st_v{s}", v_s[:, :, t * 129:(t + 1) * 129].rearrange("h p c -> p h c"), V9[s][:], f"V9{s}")
            cvt_advance(cgen, 2)
        cvt_advance(cgen, 128)
        for i in range(16):
            s = i % 2
            rows = slice(i * 128, (i + 1) * 128)
            P.dma('pool', f"gh{s}", lambda e: e.indirect_dma_start(
                out=hs[s][:], out_offset=None, in_=H2[:, :],
                in_offset=bass.IndirectOffsetOnAxis(ap=own[:, i:i + 1], axis=0)), reads=["own", "H2"], writes=[f"h{s}"])
            P.load(f"ld_cs{s}", cs[s][:, 0, :], I["cos_own"][rows, :], f"cs{s}")
            P.load(f"ld_cs{s}", cs[s][:, 1, :], I["sin_own"][rows, :], f"cs{s}")
            P.tt('dve', M[:], hs[s][:], mL[:, 1, :], ALU.mult, [f"h{s}", "mL"], ["M"])
            P.tt('dve', M[:], M[:], mL[:, 0, :], ALU.add, ["M", "mL"], ["M"])
            transpose_tile(P, tring, M, "M", T, "T", 8, ident)
            linear(P, lring, T, "T", wdq, "wdq", 8, 384, lambda nb, c0, ps, pid, n: P.cp('act', cq[:, c0:c0 + n], ps, [pid], ["cq"]))
            rmsnorm(P, cq[:], "cq", 384, qn[:], "qn", cqn[:], "cqn", junk[:], ss)
            transpose_tile(P, tring, cqn, "cqn", T2, "T2", 3, ident)
            linear(P, lring, T2, "T2", wuq, "wuq", 3, 1536,
                   lambda nb, c0, ps, pid, n: P.cp('act' if nb % 2 else 'dve', Qf[:, c0:c0 + n], ps, [pid], ["Qf"]))
            Qf4 = Qf[:].rearrange("p (h d) -> p h d", d=192)
            cosb = cs[s][:, 0, :].unsqueeze(1).to_broadcast([128, 8, 32])
            sinb = cs[s][:, 1, :].unsqueeze(1).to_broadcast([128, 8, 32])
            rope(P, Qf4[:, :, 128:160], Qf4[:, :, 160:192], cosb, sinb, QR[:, :, 0:32], QR[:, :, 32:64],
                 [rt[:, j] for j in range(4)], ["Qf", f"cs{s}"], ["QR"])
            for hh in range(8):
                pt, pid = tring.get()
                P.tr(pt[:, 0:128], Qf[:, hh * 192:hh * 192 + 128], ident[:], ["Qf", "ident"], [pid])
                P.cp('act' if hh % 2 else 'dve', QT[s][:, hh, :], pt[:, 0:128], [pid], [f"QT{s}"])
                pt, pid = tring.get()
                P.tr(pt[0:64, 0:128], QR[:, hh, :], ident[:], ["QR", "ident"], [pid])
                P.cp('dve' if hh % 2 else 'act', QRT[s][:, hh, :], pt[0:64, 0:128], [pid], [f"QRT{s}"])
            P.store(f"st_qt{s}", qT_s[:, :, rows].rearrange("h d t -> d h t"), QT[s][:], f"QT{s}")
            P.store(f"st_qr{s}", qrT_s[:, :, rows].rearrange("h d t -> d h t"), QRT[s][:], f"QRT{s}")


def emit_attn(P, I, modsB, H2, kT_d, krT_d, v_d, qT_d, qrT_d, H3):
    with P.scope("at"):
        ident = P.sb("ident", [128, 128]); gate = P.sb("gate", [128, D]); wo = P.sb("wo", [128, 8, D])
        lng = P.sb("lng", [128, D]); lnb = P.sb("lnb", [128, D]); own = P.sb("own", [128, 16], U32)
        krTa = P.sb("krTa", [65, NK], BF16)
        P.load("ld_c", ident[:], I["ident"], "ident"); P.load("ld_c", own[:], I["ownidx"], "own")
        P.load("ld_c", gate[:], modsB[1, 0, :, 2048:3072], "gate")
        P.load("ld_c", lng[:], I["lng"][2], "lng"); P.load("ld_c", lnb[:], I["lnb"][2], "lnb")
        P.load("ld_wo", wo[:], I["wo_mla"].rearrange("(kc p) n -> p kc n", p=128), "wo")
        P.load("ld_kr", krTa[0:64, :], krT_d, "krTa")
        P.op('dve', lambda g: g.memset(krTa[64:65, :], 1.0), writes=["krTa"])
        kT = [P.sb(f"kT{i}", [128, NK], BF16) for i in range(2)]
        vh = [P.sb(f"vh{i}", [128, NKT * 129], BF16) for i in range(2)]
        qT = [P.sb(f"qT{i}", [128, 512], BF16) for i in range(2)]
        qrTa = [P.sb(f"qrTa{i}", [65, 512], BF16) for i in range(2)]
        NPT = 3
        PT = [P.sb(f"PT{i}", [128, 512], BF16) for i in range(NPT)]
        mx = P.sb("mx", [128, 20]); nmp = P.sb("nmp", [128, 65]); rden = P.sb("rden", [128, 4])
        otok = P.sb("otok", [128, 4, D])
        hs = [P.sb("hs0", [128, D])] * 2
        A = P.sb("A", [128, D]); T = P.sb("T", [128, 8, 128])
        O = [P.sb("O0", [128, D])] * 2
        stats = P.sb("stats", [128, 2, 6]); mv = P.sb("mv", [128, 4])
        sring = PsumRing(P, range(0, 3)); mring = PsumRing(P, range(3, 4)); oring = PsumRing(P, range(4, 8))
        P.op('dve', lambda g: g.memset(nmp[:], 0.0), writes=["nmp"])
        kblocks = [(512 * i, 512) for i in range(16)] + [(8192, 256)]
        it = 0
        ipt = 0
        tcount = 0
        for qb in range(4):
            for hh in range(8):
                s = it % 2
                it += 1
                P.load(f"ld_k{s}", kT[s][:], kT_d[hh], f"kT{s}")
                P.load(f"ld_v{s}", vh[s][:], v_d[hh], f"vh{s}")
                P.load(f"ld_q{s}", qT[s][:], qT_d[hh, :, qb * 512:(qb + 1) * 512], f"qT{s}")
                P.load(f"ld_qr{s}", qrTa[s][0:64, :], qrT_d[hh, :, qb * 512:(qb + 1) * 512], f"qrTa{s}")
                for qs in range(4):
                    qc = slice(qs * 128, (qs + 1) * 128)
                    for kb, (k0, n) in enumerate(kblocks):
                        pt, pid = sring.get()
                        P.mm(pt[:, 0:n], qT[s][:, qc], kT[s][:, k0:k0 + n], True, False, [f"qT{s}", f"kT{s}"], [pid])
                        P.mm(pt[:, 0:n], qrTa[s][0:64, qc], krTa[0:64, k0:k0 + n], False, True, [f"qrTa{s}", "krTa"], [pid])
                        P.op('dve', lambda g: g.tensor_reduce(out=mx[:, kb:kb + 1], in_=pt[:, 0:n], axis=AX.X, op=ALU.max),
                             reads=[pid], writes=["mx"])
                    P.op('dve', lambda g: g.tensor_reduce(out=mx[:, 17:18], in_=mx[:, 0:17], axis=AX.X, op=ALU.max),
                         reads=["mx"], writes=["mx"])
                    P.ts('dve', nmp[:, 64:65], mx[:, 17:18], -1.0, None, ALU.mult, None, ["mx"], ["nmp"])
                    pt, pid = mring.get()
                    P.mm(pt[0:65, 0:128], nmp[:, :], ident[:], True, True, ["nmp", "ident"], [pid])
                    P.cp('act', qrTa[s][64:65, qc], pt[64:65, 0:128], [pid], [f"qrTa{s}"])
                ops = [oring.get() for _ in range(4)]
                for kt in range(NKT):
                    kc = slice(kt * 128, (kt + 1) * 128)
                    pt, pid = sring.get()
                    P.mm(pt[:, 0:512], kT[s][:, kc], qT[s][:, :], True, False, [f"kT{s}", f"qT{s}"], [pid])
                    P.mm(pt[:, 0:512], krTa[0:65, kc], qrTa[s][0:65, :], False, True, ["krTa", f"qrTa{s}"], [pid])
                    pi = ipt % NPT
                    ipt += 1
                    P.actf(PT[pi][:], pt[:, 0:512], AF.Exp, [pid], [f"PT{pi}"], scale=ATT_SCALE)
                    for qs in range(4):
                        ot, oid = ops[qs]
                        P.mm(ot[:, 0:129], PT[pi][:, qs * 128:(qs + 1) * 128], vh[s][:, kt * 129:(kt + 1) * 129],
                             kt == 0, kt == NKT - 1, [f"PT{pi}", f"vh{s}"], [oid])
                for qs in range(4):
                    ot, oid = ops[qs]
                    P.op('dve', lambda g: g.reciprocal(out=rden[:, qs:qs + 1], in_=ot[:, 128:129]), reads=[oid], writes=["rden"])
                    P.ts('dve', otok[:, qs, hh * 128:(hh + 1) * 128], ot[:, 0:128], rden[:, qs:qs + 1], None, ALU.mult, None,
                         [oid, "rden"], ["otok"])
            for qs in range(4):
                s2 = tcount % 2
                ti = qb * 4 + qs
                tcount += 1
                rows = slice(ti * 128, (ti + 1) * 128)
                P.dma('pool', "gh0", lambda e: e.indirect_dma_start(
                    out=hs[s2][:], out_offset=None, in_=H2[:, :],
                    in_offset=bass.IndirectOffsetOnAxis(ap=own[:, ti:ti + 1], axis=0)), reads=["own"], writes=["hs0"])
                for kc in range(8):
                    pt, pid = sring.get()
                    P.tr(pt[:, 0:128], otok[:, qs, kc * 128:(kc + 1) * 128], ident[:], ["otok", "ident"], [pid])
                    P.cp('act' if kc % 2 == 0 else 'dve', T[:, kc, :], pt[:, 0:128], [pid], ["T"])
                linear(P, sring, T, "T", wo, "wo", 8, D,
                       lambda nb, c0, ps, pid, n: P.tt('dve', A[:, c0:c0 + n], ps, gate[:, c0:c0 + n], ALU.mult, [pid, "gate"], ["A"]))
                P.stt(A[:], hs[s2][:], ALPHA, A[:], ALU.mult, ALU.add, ["hs0", "A"], ["A"])
                layernorm(P, A[:], "A", lng[:], lnb[:], O[s2][:], "O0", stats, mv)
                P.store("st_o0", H3[rows, :], O[s2][:], "O0")


IN_SPECS = [
    ("cT", [128, 8, 2], F32), ("aw", [2, 1024, 6144], F32), ("abB", [128, 2, 6144], F32), ("abT", [128, 16], F32),
    ("xT", [8, 128, SEQ_T], F32), ("x_tok", [NROW, D], F32),
    ("a_re", [8, 128, 8], F32), ("a_im", [8, 128, 8], F32), ("ldt", [8, 128, 8], F32),
    ("bre", [8, 128, 8, 16], F32), ("bim", [8, 128, 8, 16], F32), ("cre", [8, 128, 8, 16], F32), ("cim", [8, 128, 8, 16], F32),
    ("dvec", [8, 128, 1], F32), ("ident", [128, 128], F32), ("iota16", [128, 16], F32),
    ("wglu", [D, D], F32), ("wo_s5", [D, D], F32), ("lng", [4, 128, D], F32), ("lnb", [4, 128, D], F32),
    ("peer_wq", [2, D, D], F32), ("kbd", [2, 128, 8, 256], F32), ("peer_uv0", [16384, 2 * D], F32), ("peer_uv1", [16384, 2 * D], F32),
    ("wdq", [D, 384], F32), ("qn", [128, 384], F32), ("wuq", [384, 1536], F32), ("wdkv", [D, 320], F32),
    ("kvn", [128, 256], F32), ("wukv", [256, 2048], F32), ("wo_mla", [D, D], F32),
    ("cos", [NROW, 32], F32), ("sin", [NROW, 32], F32), ("cos_own", [2048, 32], F32), ("sin_own", [2048, 32], F32),
    ("ownidx", [128, 16], U32),
]


def build_fused():
    P = Prog()
    I = {name: P.din(name, shape, dt) for name, shape, dt in IN_SPECS}
    out = P.dout("out", [2048, D])
    modsB = P.scratch("modsB", [2, 2, 128, 6144])
    YF = P.scratch("YF", [NROW, D]); YB = P.scratch("YB", [NROW, D])
    H1 = P.scratch("H1", [NROW, D]); H2 = P.scratch("H2", [NROW, D]); H3 = P.scratch("H3", [2048, D])
    kT_s = P.scratch("kT_s", [8, 128, NK], BF16); krT_s = P.scratch("krT_s", [64, NK], BF16)
    v_s = P.scratch("v_s", [8, 128, NKT * 129], BF16)
    qT_s = P.scratch("qT_s", [8, 128, 2048], BF16); qrT_s = P.scratch("qrT_s", [8, 64, 2048], BF16)
    uvb = [P.scratch(f"uvb{l}", [16384, 2 * D], BF16) for l in range(2)]
    emit_mods(P, I["cT"], I["aw"], I["abB"], modsB)
    emit_s5(P, I, YF, YB)
    emit_s5out(P, I, modsB, YF, YB, H1, I["peer_uv0"], uvb[0])
    emit_peer(P, I, modsB, 0, H1, H2, NTB, "p0", uvb[0])
    emit_mlaproj(P, I, modsB, H2, kT_s, krT_s, v_s, qT_s, qrT_s, I["peer_uv1"], uvb[1])
    emit_attn(P, I, modsB, H2, kT_s, krT_s, v_s, qT_s, qrT_s, H3)
    emit_peer(P, I, modsB, 1, H3, out, 16, "p1", uvb[1])
    P.finish()
    return P


def bc(v):
    return np.ascontiguousarray(np.broadcast_to(v[None, :], (128, v.shape[0])).astype(np.float32))


def rope_tables():
    n_freq = 16
    inv = (10000.0 ** (-np.arange(n_freq, dtype=np.float32) / n_freq)).astype(np.float32)
    r, col = np.meshgrid(np.arange(128, dtype=np.float32), np.arange(64, dtype=np.float32), indexing='ij')
    ang = np.concatenate([r.reshape(-1, 1) * inv, col.reshape(-1, 1) * inv], axis=-1).astype(np.float32)
    return np.cos(ang).astype(np.float32), np.sin(ang).astype(np.float32)


def kernel(x, c, ctx, c_ctx, ada_w, ada_b, ln_g, ln_b,
           s5_a_re, s5_a_im, s5_log_dt, s5_b_re, s5_b_im, s5_c_re, s5_c_im, s5_d, s5_w_glu, s5_w_o,
           mla_w_dq, mla_q_norm, mla_w_uq, mla_w_dkv, mla_kv_norm, mla_w_ukv, mla_w_o,
           peer_w_q, peer_keys, peer_u, peer_v):
    f = lambda a: np.ascontiguousarray(np.asarray(a, dtype=np.float32))
    x, c, ctx, c_ctx, ada_w, ada_b, ln_g, ln_b = map(f, (x, c, ctx, c_ctx, ada_w, ada_b, ln_g, ln_b))
    P = build_fused()
    def st(a):
        return np.ascontiguousarray(f(a).reshape(2, 8, 4, 2, 64).transpose(1, 3, 4, 0, 2).reshape(8, 128, 8))

    def bl(a):
        return np.ascontiguousarray(f(a).reshape(2, 8, 4, 2, 64, 16).transpose(1, 3, 4, 0, 2, 5).reshape(8, 128, 8, 16))
    kbd = np.zeros((2, 128, 8, 256), dtype=np.float32)
    pk = f(peer_keys)
    for l in range(2):
        for s_ in range(2):
            kbd[l, 64 * s_:64 * s_ + 64, :, 128 * s_:128 * s_ + 128] = pk[l, :, s_].transpose(2, 0, 1)
    cosl, sinl = rope_tables()
    cos_all = np.ones((NROW, 32), np.float32); sin_all = np.zeros((NROW, 32), np.float32)
    cos_all[:8192] = cosl; sin_all[:8192] = sinl
    common = {
        "aw": ada_w, "abB": np.ascontiguousarray(np.broadcast_to(ada_b[None], (128, 2, 6144))),
        "abT": np.ascontiguousarray(ada_b[0, :2048].reshape(2, 8, 128).transpose(2, 0, 1).reshape(128, 16)),
        "a_re": st(s5_a_re[0]), "a_im": st(s5_a_im[0]),
        "ldt": st(np.broadcast_to(f(s5_log_dt)[0][:, :, None], (2, 64, 64))),
        "bre": bl(s5_b_re[0]), "bim": bl(s5_b_im[0]),
        "cre": bl(f(s5_c_re)[0].transpose(0, 1, 3, 2)), "cim": bl(f(s5_c_im)[0].transpose(0, 1, 3, 2)),
        "dvec": np.ascontiguousarray(f(s5_d)[0].reshape(8, 128, 1)),
        "ident": np.eye(128, dtype=np.float32), "iota16": bc(np.arange(16, dtype=np.float32)),
        "wglu": f(s5_w_glu)[0], "wo_s5": f(s5_w_o)[0],
        "lng": np.stack([bc(ln_g[0, 0]), bc(ln_g[0, 1]), bc(ln_g[1, 0]), bc(ln_g[1, 1])]),
        "lnb": np.stack([bc(ln_b[0, 0]), bc(ln_b[0, 1]), bc(ln_b[1, 0]), bc(ln_b[1, 1])]),
        "peer_wq": f(peer_w_q), "kbd": kbd, "peer_uv0": np.ascontiguousarray(np.concatenate([f(peer_u)[0], f(peer_v)[0]], axis=1)),
        "peer_uv1": np.ascontiguousarray(np.concatenate([f(peer_u)[1], f(peer_v)[1]], axis=1)),
        "wdq": f(mla_w_dq)[0], "qn": bc(f(mla_q_norm)[0]), "wuq": f(mla_w_uq)[0], "wdkv": f(mla_w_dkv)[0],
        "kvn": bc(f(mla_kv_norm)[0]), "wukv": f(mla_w_ukv)[0], "wo_mla": f(mla_w_o)[0],
        "cos": cos_all, "sin": sin_all,
    }
    per_b = []
    for b in range(2):
        seq = np.concatenate([ctx[b], x[b]], axis=0)
        per_b.append({
            "cT": np.ascontiguousarray(np.stack([c[b], c_ctx], axis=1).reshape(8, 128, 2).transpose(1, 0, 2)),
            "xT": np.ascontiguousarray(seq.T.reshape(8, 128, SEQ_T)),
            "x_tok": np.ascontiguousarray(np.concatenate([x[b], ctx[b]], axis=0)),
        })
    maps = []
    for k in range(NCORES):
        b, j = k // 4, k % 4
        m = dict(common)
        m.update(per_b[b])
        m["cos_own"] = np.ascontiguousarray(cosl[2048 * j:2048 * (j + 1)])
        m["sin_own"] = np.ascontiguousarray(sinl[2048 * j:2048 * (j + 1)])
        m["ownidx"] = np.ascontiguousarray((2048 * j + np.arange(2048, dtype=np.uint32)).reshape(16, 128).T)
        maps.append(m)
    res = P.run(maps)
    out = np.zeros((2, 8192, D), np.float32)
    for k in range(NCORES):
        b, j = k // 4, k % 4
        out[b, 2048 * j:2048 * (j + 1)] = res[k]["out"]
    return out
```

```python
import math
import numpy as np
import ml_dtypes
from contextlib import ExitStack
import concourse.bass as bass
import concourse.mybir as mybir
from concourse.bass_utils import run_bass_kernel_spmd

F32 = mybir.dt.float32
BF16 = mybir.dt.bfloat16
U32 = mybir.dt.uint32
I32 = mybir.dt.int32
AF = mybir.ActivationFunctionType
ALU = mybir.AluOpType
AX = mybir.AxisListType

NCORES = 8
D = 1024
ALPHA = (2 * 2) ** 0.25
LN_EPS = 1e-5
RMS_EPS = 1e-6
NT = 17
TPC = NT * 128


class Prog:
    def __init__(self):
        self.nc = bass.Bass("TRN2", target_bir_lowering=False)
        self.es = ExitStack()
        self.semes = ExitStack()
        nc = self.nc
        self.eng = {'pe': nc.tensor, 'dve': nc.vector, 'act': nc.scalar, 'pool': nc.gpsimd, 'sp': nc.sync}
        self.sems, self.cnt = {}, {}
        self.seen = {e: {} for e in self.eng}
        self.lastw, self.readers = {}, {}
        for e in ('pe', 'dve', 'act', 'pool'):
            self._sem('E_' + e)
        self.npsum = 0

    def _sem(self, key):
        if key not in self.sems:
            self.sems[key] = self.semes.enter_context(self.nc.semaphore(key))
            self.cnt[key] = 0
        return self.sems[key]

    def din(self, name, shape, dt=F32):
        return self.nc.dram_tensor(name, list(shape), dt, kind="ExternalInput").ap()

    def dout(self, name, shape, dt=F32):
        return self.nc.dram_tensor(name, list(shape), dt, kind="ExternalOutput").ap()

    def sb(self, name, shape, dt=F32):
        return self.es.enter_context(self.nc.sbuf_tensor("sb_" + getattr(self, "pfx", "") + name, list(shape), dt))

    def scratch(self, name, shape, dt=F32):
        return self.nc.dram_tensor(name, list(shape), dt).ap()

    def scope(self, pfx):
        prog = self

        class _S:
            def __enter__(s_):
                s_.old = (prog.es, getattr(prog, "pfx", ""))
                prog.es = ExitStack()
                prog.pfx = pfx + "_"
                return prog

            def __exit__(s_, *a):
                prog.barrier()
                prog.es.close()
                prog.es, prog.pfx = s_.old
                return False
        return _S()

    def barrier(self):
        for e in self.eng:
            for k, v in self.cnt.items():
                if v > 0 and self.seen[e].get(k, 0) < v:
                    self.eng[e].wait_ge(self.sems[k], v)
                    self.seen[e][k] = v

    def ps(self, name, shape, dt=F32):
        return self.es.enter_context(self.nc.psum_tensor(name, list(shape), dt))

    def _deps(self, reads, writes):
        deps = {}

        def add(k, v):
            if not k.startswith('E_'):
                v = max(v, self.cnt[k])
            if deps.get(k, 0) < v:
                deps[k] = v
        for b in reads:
            if b in self.lastw:
                add(*self.lastw[b])
        for b in writes:
            if b in self.lastw:
                add(*self.lastw[b])
            for k, v in self.readers.get(b, {}).items():
                add(k, v)
        return deps

    def _wait(self, e, deps, skip=None):
        for k, v in deps.items():
            if k == skip or self.seen[e].get(k, 0) >= v:
                continue
            self.eng[e].wait_ge(self.sems[k], v)
            self.seen[e][k] = v

    def _commit(self, k, v, reads, writes):
        for b in writes:
            self.lastw[b] = (k, v)
            self.readers[b] = {}
        for b in reads:
            r = self.readers.setdefault(b, {})
            if r.get(k, 0) < v:
                r[k] = v

    def op(self, e, fn, reads=(), writes=()):
        key = 'E_' + e
        self._wait(e, self._deps(reads, writes), skip=key if e == 'pe' else None)
        ins = fn(self.eng[e])
        self.cnt[key] += 1
        ins.then_inc(self.sems[key], 1)
        self._commit(key, self.cnt[key], reads, writes)

    def dma(self, q, semkey, fn, reads=(), writes=()):
        self._sem(semkey)
        self._wait(q, self._deps(reads, writes))
        ins = fn(self.eng[q])
        self.cnt[semkey] += 16
        ins.then_inc(self.sems[semkey], 16)
        self._commit(semkey, self.cnt[semkey], reads, writes)

    def finish(self):
        for k, v in self.cnt.items():
            if v > 0 and self.seen['sp'].get(k, 0) < v:
                self.nc.sync.wait_ge(self.sems[k], v)

    def run(self, in_maps):
        res = run_bass_kernel_spmd(self.nc, in_maps, core_ids=list(range(NCORES)))
        return res.results

    def load(self, semkey, dst, src, wid, rid=None, q='sp'):
        self.dma(q, semkey, lambda e: e.dma_start(out=dst, in_=src), reads=(rid,) if rid else (), writes=(wid,))

    def store(self, semkey, dst, src, rid, wid=None, q='sp'):
        self.dma(q, semkey, lambda e: e.dma_start(out=dst, in_=src), reads=(rid,), writes=(wid,) if wid else ())

    def tt(self, e, out, a, b, op, r, w):
        self.op(e, lambda g: g.tensor_tensor(out=out, in0=a, in1=b, op=op), reads=r, writes=w)

    def ts(self, e, out, a, s1, s2, op0, op1, r, w):
        if op1 is None:
            self.op(e, lambda g: g.tensor_scalar(out=out, in0=a, scalar1=s1, scalar2=None, op0=op0), reads=r, writes=w)
        else:
            self.op(e, lambda g: g.tensor_scalar(out=out, in0=a, scalar1=s1, scalar2=s2, op0=op0, op1=op1), reads=r, writes=w)

    def stt(self, out, a, s, b, op0, op1, r, w):
        self.op('dve', lambda g: g.scalar_tensor_tensor(out=out, in0=a, scalar=s, in1=b, op0=op0, op1=op1), reads=r, writes=w)

    def actf(self, out, a, func, r, w, bias=None, scale=None, accum=None):
        kw = {}
        if bias is not None:
            kw['bias'] = bias
        if scale is not None:
            kw['scale'] = scale
        if accum is not None:
            kw['accum_out'] = accum
        self.op('act', lambda g: g.activation(out=out, in_=a, func=func, **kw), reads=r, writes=w)

    def cp(self, e, out, a, r, w):
        if e == 'act':
            self.actf(out, a, AF.Copy, r, w)
        else:
            self.op(e, lambda g: g.tensor_copy(out=out, in_=a), reads=r, writes=w)

    def mm(self, out, lhsT, rhs, start, stop, r, w):
        self.op('pe', lambda g: g.matmul(out, lhsT, rhs, start=start, stop=stop), reads=r, writes=w)

    def tr(self, out, in_, ident, r, w):
        self.op('pe', lambda g: g.transpose(out, in_, ident), reads=r, writes=w)


class PsumRing:
    def __init__(self, P, banks, dt=F32):
        if not hasattr(P, "banks"):
            P.banks = {}
        for i in banks:
            if i not in P.banks:
                P.banks[i] = P.ps(f"psb{i}", [128, 512], dt)
        self.b = list(banks)
        self.P = P
        self.i = 0

    def get(self):
        j = self.b[self.i]
        self.i = (self.i + 1) % len(self.b)
        return self.P.banks[j], f"psb{j}"


NTB = 66
NROW = NTB * 128
SEQ_T = 256 + 8192
CH = 512
TWO_PI = 2.0 * math.pi
MAGIC = 12582912.0
NEG = -1.0e30
NK = 8448
NKT = 66
ATT_SCALE = 192.0 ** -0.5


def sincos(P, th, n, tmp, sin_out, cos_out, ids):
    for which, outt in ((0, sin_out), (1, cos_out)):
        x, u, k, y, m = (tmp[:, j, :n] for j in range(5))
        r = list(ids)
        if which == 0:
            P.cp('dve', x, th, r, r)
        else:
            P.ts('dve', x, th, math.pi / 2, None, ALU.add, None, r, r)
        P.ts('dve', u, x, 1.0 / TWO_PI, None, ALU.mult, None, r, r)
        P.ts('dve', k, u, MAGIC, None, ALU.add, None, r, r)
        P.ts('dve', k, k, MAGIC, None, ALU.subtract, None, r, r)
        P.stt(y, k, -TWO_PI, x, ALU.mult, ALU.add, r, r)
        P.ts('dve', m, y, math.pi, None, ALU.is_gt, None, r, r)
        P.stt(y, m, -TWO_PI, y, ALU.mult, ALU.add, r, r)
        P.ts('dve', m, y, -math.pi, None, ALU.is_lt, None, r, r)
        P.stt(y, m, TWO_PI, y, ALU.mult, ALU.add, r, r)
        P.ts('dve', y, y, math.pi, -math.pi, ALU.min, ALU.max, r, r)
        P.actf(outt, y, AF.Sin, r, r)


def transpose_tile(P, ring, src, sid, dst, did, nk, ident):
    for kc in range(nk):
        pt, pid = ring.get()
        P.tr(pt[:, 0:128], src[:, kc * 128:(kc + 1) * 128], ident[:], [sid, "ident"], [pid])
        P.cp('act' if kc % 2 == 0 else 'dve', dst[:, kc, :], pt[:, 0:128], [pid], [did])


def linear(P, ring, xT, xid, W, wid, nk, N, cb, bs=512):
    nb = 0
    c0 = 0
    while c0 < N:
        n = min(bs, N - c0)
        pt, pid = ring.get()
        for kc in range(nk):
            P.mm(pt[:, 0:n], xT[:, kc, :], W[:, kc, c0:c0 + n], kc == 0, kc == nk - 1, [xid, wid], [pid])
        cb(nb, c0, pt[:, 0:n], pid, n)
        c0 += n
        nb += 1


def layernorm(P, x, xid, gbc, bbc, out, oid, stats, mv):
    for c in range(2):
        P.op('dve', lambda g: g.bn_stats(out=stats[:, c, :], in_=x[:, c * 512:(c + 1) * 512]), reads=[xid], writes=["lnst"])
    P.op('dve', lambda g: g.bn_aggr(out=mv[:, 0:2], in_=stats[:].rearrange("p a b -> p (a b)")), reads=["lnst"], writes=["lnmv"])
    P.ts('dve', mv[:, 2:3], mv[:, 1:2], LN_EPS, None, ALU.add, None, ["lnmv"], ["lnmv"])
    P.actf(mv[:, 2:3], mv[:, 2:3], AF.Sqrt, ["lnmv"], ["lnmv"])
    P.op('dve', lambda g: g.reciprocal(out=mv[:, 3:4], in_=mv[:, 2:3]), reads=["lnmv"], writes=["lnmv"])
    P.ts('dve', x, x, mv[:, 0:1], mv[:, 3:4], ALU.subtract, ALU.mult, [xid, "lnmv"], [xid])
    P.tt('pool', x, x, gbc, ALU.mult, [xid, "lng"], [xid])
    P.tt('pool', out, x, bbc, ALU.add, [xid, "lnb"], [oid])


def rmsnorm(P, x, xid, n, gbc, gid, out, oid, junk, ss):
    P.actf(junk, x, AF.Square, [xid], ["rjunk"], accum=ss[:, 0:1])
    P.actf(ss[:, 3:4], junk[:, 0:1], AF.Copy, ["rjunk"], ["rjunk"])
    P.ts('dve', ss[:, 1:2], ss[:, 0:1], 1.0 / n, RMS_EPS, ALU.mult, ALU.add, ["rjunk"], ["rss"])
    P.actf(ss[:, 1:2], ss[:, 1:2], AF.Sqrt, ["rss"], ["rss"])
    P.op('dve', lambda g: g.reciprocal(out=ss[:, 2:3], in_=ss[:, 1:2]), reads=["rss"], writes=["rss"])
    P.stt(out, x, ss[:, 2:3], gbc, ALU.mult, ALU.mult, [xid, "rss", gid], [oid])


def rope(P, x1, x2, cosb, sinb, o1, o2, t, rid, wid):
    P.tt('dve', t[0], x1, cosb, ALU.mult, rid, ["ropet"])
    P.tt('dve', t[1], x2, sinb, ALU.mult, rid, ["ropet"])
    P.tt('dve', t[2], x2, cosb, ALU.mult, rid, ["ropet"])
    P.tt('dve', t[3], x1, sinb, ALU.mult, rid, ["ropet"])
    P.tt('dve', o1, t[0], t[1], ALU.subtract, ["ropet"], wid)
    P.tt('dve', o2, t[2], t[3], ALU.add, ["ropet"], wid)


def emit_mods(P, cT_d, aw_d, abB_d, modsB):
    with P.scope("md"):
        s = P.sb("s", [128, 8, 2]); srep = P.sb("srep", [128, 8, 2, 128])
        P.load("ld_c", s[:], cT_d, "s")
        P.actf(s[:], s[:], AF.Silu, ["s"], ["s"])
        P.cp('dve', srep[:], s[:].unsqueeze(3).to_broadcast([128, 8, 2, 128]), ["s"], ["srep"])
        w = [P.sb(f"w{i}", [128, 8, 512]) for i in range(2)]
        bb = [P.sb(f"bb{i}", [128, 512]) for i in range(2)]
        ot = [[P.sb(f"ot{v}{i}", [128, 512]) for i in range(2)] for v in range(2)]
        ring = PsumRing(P, range(8))
        it = 0
        for layer in range(2):
            for nb in range(12):
                sl = it % 2
                it += 1
                cols = slice(nb * 512, (nb + 1) * 512)
                P.load(f"ld_w{sl}", w[sl][:], aw_d[layer, :, cols].rearrange("(kc p) n -> p kc n", p=128), f"w{sl}")
                P.load(f"ld_b{sl}", bb[sl][:], abB_d[:, layer, cols], f"bb{sl}")
                for v in range(2):
                    pt, pid = ring.get()
                    for kc in range(8):
                        P.mm(pt[:, 0:512], srep[:, kc, v, :], w[sl][:, kc, :], kc == 0, kc == 7, ["srep", f"w{sl}"], [pid])
                    P.tt('dve', ot[v][sl][:], pt[:, 0:512], bb[sl][:], ALU.add, [pid, f"bb{sl}"], [f"ot{v}{sl}"])
                    P.store(f"st_m{v}{sl}", modsB[layer, v, :, cols], ot[v][sl][:], f"ot{v}{sl}")


def emit_s5(P, I, YF, YB):
    with P.scope("s5"):
        ident = P.sb("ident", [128, 128]); s = P.sb("s", [128, 8, 2])
        abT = P.sb("abT", [128, 16])
        P.load("ld_par0", ident[:], I["ident"], "ident")
        P.load("ld_par0", s[:], I["cT"], "s")
        P.load("ld_par0", abT[:], I["abT"], "abT")
        P.actf(s[:], s[:], AF.Silu, ["s", "ident", "abT"], ["s"])
        wsl = P.sb("wsl", [128, 2, 8, 128])
        sc1 = P.sb("sc1", [128, 2]); sh = P.sb("sh", [128, 2]); dv = P.sb("dv", [128, 1])
        are = P.sb("are", [128, 8]); aim = P.sb("aim", [128, 8]); ldt = P.sb("ldt", [128, 8])
        bre = P.sb("bre", [128, 8, 16]); bim = P.sb("bim", [128, 8, 16])
        cre = P.sb("cre", [128, 8, 16]); cim = P.sb("cim", [128, 8, 16])
        prm = P.sb("prm", [128, 16, 8]); tmp5 = P.sb("tmp5", [128, 5, 8])
        Wre = P.sb("Wre", [128, 8, 128]); Wim = P.sb("Wim", [128, 8, 128])
        Cre = P.sb("Cre", [128, 8, 128]); nCre = P.sb("nCre", [128, 8, 128]); nCim = P.sb("nCim", [128, 8, 128])
        bfull = P.sb("bfull", [128, 2, 128]); tb = P.sb("tb", [128, 2, 16])
        ctab = P.sb("ctab", [128, 8, CH]); stab = P.sb("stab", [128, 8, CH]); rB = P.sb("rB", [128, 8, CH])
        ones = P.sb("ones", [128, CH]); En = P.sb("En", [128, 8, 2, 2]); cur = P.sb("cur", [128, 4]); st = P.sb("st", [128, 4, 2])
        xs = [P.sb(f"xs{i}", [128, CH]) for i in range(2)]
        ms = [P.sb(f"ms{i}", [128, CH]) for i in range(2)]
        ys = [P.sb(f"ys{i}", [128, CH]) for i in range(2)]
        yt = [P.sb(f"yt{i}", [128, 4, 128]) for i in range(2)]
        NW = 3
        va = [P.sb(f"va{i}", [128, 4, CH]) for i in range(NW)]
        G = [P.sb(f"G{i}", [128, 2, CH]) for i in range(NW)]
        X = [P.sb(f"X{i}", [128, 4, CH]) for i in range(NW)]
        ring = PsumRing(P, range(2, 6)); yring = PsumRing(P, range(0, 2)); trring = PsumRing(P, range(6, 8))
        P.op('dve', lambda g: g.memset(ones[:], 1.0), writes=["ones"])
        R = ["prm"]
        it = 0
        wk = 0
        for fc in range(8):
            for dst, key, nm in ((are, "a_re", "are"), (aim, "a_im", "aim"), (ldt, "ldt", "ldt"), (bre, "bre", "bre"),
                                 (bim, "bim", "bim"), (cre, "cre", "cre"), (cim, "cim", "cim"), (dv, "dvec", "dv")):
                P.load("ld_par", dst[:], I[key][fc], nm)
            for which in range(2):
                c0 = which * 1024 + fc * 128
                P.load("ld_wsl", wsl[:, which], I["aw"][0, :, c0:c0 + 128].rearrange("(kc p) n -> p kc n", p=128), "wsl")
            for which, dstt, addc in ((0, sh, 0.0), (1, sc1, 1.0)):
                pt, pid = ring.get()
                for kc in range(8):
                    P.mm(pt[:, 0:2], wsl[:, which, kc, :], s[:, kc, :], kc == 0, kc == 7, ["wsl", "s"], [pid])
                P.ts('dve', dstt[:], pt[:, 0:2], abT[:, which * 8 + fc:which * 8 + fc + 1], addc, ALU.add, ALU.add,
                     [pid, "abT"], ["scsh"])
            dt_, mag, th, sn, cs, abr, abi, den, fre, fim, t0, t1 = (prm[:, j, :] for j in range(12))
            P.actf(dt_, ldt[:], AF.Exp, ["ldt"], R)
            P.tt('dve', t0, are[:], dt_, ALU.mult, ["are"] + R, R)
            P.actf(mag, t0, AF.Exp, R, R)
            P.tt('dve', th, aim[:], dt_, ALU.mult, ["aim"] + R, R)
            sincos(P, th, 8, tmp5, sn, cs, R + ["tmp5"])
            P.tt('dve', abr, mag, cs, ALU.mult, R, R)
            P.tt('dve', abi, mag, sn, ALU.mult, R, R)
            P.tt('dve', den, are[:], are[:], ALU.mult, ["are"] + R, R)
            P.tt('dve', t0, aim[:], aim[:], ALU.mult, ["aim"] + R, R)
            P.tt('dve', den, den, t0, ALU.add, R, R)
            P.op('dve', lambda g: g.reciprocal(out=den, in_=den), reads=R, writes=R)
            P.ts('dve', t0, abr, -1.0, None, ALU.add, None, R, R)
            P.tt('dve', fre, t0, are[:], ALU.mult, ["are"] + R, R)
            P.tt('dve', t1, abi, aim[:], ALU.mult, ["aim"] + R, R)
            P.tt('dve', fre, fre, t1, ALU.add, R, R)
            P.tt('dve', fre, fre, den, ALU.mult, R, R)
            P.tt('dve', fim, abi, are[:], ALU.mult, ["are"] + R, R)
            P.tt('dve', t1, t0, aim[:], ALU.mult, ["aim"] + R, R)
            P.tt('dve', fim, fim, t1, ALU.subtract, R, R)
            P.tt('dve', fim, fim, den, ALU.mult, R, R)
            for j in range(8):
                pair = j % 4
                for which, Wdst in ((0, Wre), (1, Wim)):
                    P.op('dve', lambda g: g.memset(bfull[:, which, :], 0.0), writes=["bfull"])
                    if which == 0:
                        P.ts('dve', tb[:, 0, :], bim[:, j, :], fim[:, j:j + 1], None, ALU.mult, None, ["bim"] + R, ["tb"])
                        src2, op1 = bre, ALU.subtract
                    else:
                        P.ts('dve', tb[:, 0, :], bre[:, j, :], fim[:, j:j + 1], None, ALU.mult, None, ["bre"] + R, ["tb"])
                        src2, op1 = bim, ALU.add
                    P.stt(tb[:, 1, :], src2[:, j, :], fre[:, j:j + 1], tb[:, 0, :], ALU.mult, op1, ["bre", "bim", "tb"] + R, ["tb"])
                    for g2 in range(2):
                        c0 = 32 * pair + 16 * g2
                        P.cp('dve', bfull[64 * g2:64 * g2 + 64, which, c0:c0 + 16], tb[64 * g2:64 * g2 + 64, 1, :], ["tb"], ["bfull"])
                    pt, pid = ring.get()
                    P.tr(pt[:, 0:128], bfull[:, which, :], ident[:], ["bfull", "ident"], [pid])
                    P.cp('act', Wdst[:, j, :], pt[:, 0:128], [pid], ["W"])
                for srcc, dsts in ((cre, (Cre, nCre)), (cim, (None, nCim))):
                    for dd, sgn in zip(dsts, (1.0, -1.0)):
                        if dd is None:
                            continue
                        P.op('dve', lambda g: g.memset(dd[:, j, :], 0.0), writes=["W"])
                        for g2 in range(2):
                            c0 = 32 * pair + 16 * g2
                            P.ts('dve', dd[64 * g2:64 * g2 + 64, j, c0:c0 + 16], srcc[64 * g2:64 * g2 + 64, j, :], sgn, None,
                                 ALU.mult, None, ["cre", "cim"], ["W"])
                P.op('dve', lambda g: g.memset(ctab[:, j, 0:1], 1.0), writes=["tab"])
                P.op('dve', lambda g: g.memset(stab[:, j, 0:1], 0.0), writes=["tab"])
                P.cp('dve', cur[:, 0:1], cs[:, j:j + 1], R, ["cur"])
                P.cp('dve', cur[:, 1:2], sn[:, j:j + 1], R, ["cur"])
                n = 1
                while n <= CH:
                    if n in (256, 512):
                        wi = 0 if n == 256 else 1
                        P.cp('dve', En[:, j, wi, :], cur[:, 0:2], ["cur"], ["En"])
                    if n == CH:
                        break
                    cn, snn = cur[:, 0:1], cur[:, 1:2]
                    T = ["tab", "cur", "rB"]
                    P.ts('dve', rB[:, j, 0:n], stab[:, j, 0:n], snn, None, ALU.mult, None, T, ["rB"])
                    P.stt(ctab[:, j, n:2 * n], ctab[:, j, 0:n], cn, rB[:, j, 0:n], ALU.mult, ALU.subtract, T, ["tab"])
                    P.ts('dve', rB[:, j, 0:n], ctab[:, j, 0:n], snn, None, ALU.mult, None, T, ["rB"])
                    P.stt(stab[:, j, n:2 * n], stab[:, j, 0:n], cn, rB[:, j, 0:n], ALU.mult, ALU.add, T, ["tab"])
                    P.tt('dve', cur[:, 2:3], cn, cn, ALU.mult, ["cur"], ["cur"])
                    P.tt('dve', cur[:, 3:4], snn, snn, ALU.mult, ["cur"], ["cur"])
                    P.tt('dve', cur[:, 3:4], cur[:, 2:3], cur[:, 3:4], ALU.subtract, ["cur"], ["cur"])
                    P.tt('dve', cur[:, 2:3], cn, snn, ALU.mult, ["cur"], ["cur"])
                    P.ts('dve', cur[:, 1:2], cur[:, 2:3], 2.0, None, ALU.mult, None, ["cur"], ["cur"])
                    P.cp('dve', cur[:, 0:1], cur[:, 3:4], ["cur"], ["cur"])
                    n *= 2
                P.ts('dve', rB[:, j, :], ones[:], mag[:, j:j + 1], None, ALU.mult, None, ["ones", "rB"] + R, ["rB"])
            for d_ in range(2):
                Yd = YF if d_ == 0 else YB
                P.op('dve', lambda g: g.memset(st[:], 0.0), writes=["st"])
                items = [(ci, pair) for ci in range(17) for pair in range(4)]
                cinfo = {}

                def chunk_setup(ci):
                    nonlocal it
                    n = 256 if ci == 0 else 512
                    if ci == 0:
                        t0_, row0, mcol = 0, 8192, 1
                    elif d_ == 0:
                        t0_, row0, mcol = 256 + 512 * (ci - 1), 512 * (ci - 1), 0
                    else:
                        t0_, row0, mcol = 256 + 8192 - 512 * ci, 8192 - 512 * ci, 0
                    sl = it % 2
                    it += 1
                    P.load(f"ld_x{sl}", xs[sl][:, :n], I["xT"][fc, :, t0_:t0_ + n], f"xs{sl}")
                    xin = xs[sl][:, :n] if d_ == 0 else xs[sl][:, n - 1::-1]
                    P.ts('dve', ms[sl][:, :n], xin, sc1[:, mcol:mcol + 1], sh[:, mcol:mcol + 1], ALU.mult, ALU.add,
                         [f"xs{sl}", "scsh"], [f"ms{sl}"])
                    yp, yid = yring.get()
                    cinfo[ci] = (n, row0, sl, yp, yid)

                def drive(ci, pair):
                    n, row0, sl, yp, yid = cinfo[ci]
                    j = d_ * 4 + pair
                    pa, aid = ring.get()
                    pb, bid = ring.get()
                    P.mm(pa[:, :n], Wre[:, j, :], ms[sl][:, :n], True, True, ["W", f"ms{sl}"], [aid])
                    P.mm(pb[:, :n], Wim[:, j, :], ms[sl][:, :n], True, True, ["W", f"ms{sl}"], [bid])
                    return pa, aid, pb, bid

                chunk_setup(0)
                pend = drive(0, 0)
                for ii, (ci, pair) in enumerate(items):
                    n, row0, sl, yp, yid = cinfo[ci]
                    pa, aid, pb, bid = pend
                    if ii + 1 < len(items):
                        nci, npair = items[ii + 1]
                        if npair == 0:
                            chunk_setup(nci)
                        pend = drive(nci, npair)
                    j = d_ * 4 + pair
                    w_ = wk % NW
                    wk += 1
                    c_, s_ = ctab[:, j, :n], stab[:, j, :n]
                    V, VI = va[w_], f"va{w_}"
                    P.tt('dve', V[:, 0, :n], pa[:, :n], c_, ALU.mult, [aid, "tab"], [VI])
                    P.tt('dve', V[:, 1, :n], pb[:, :n], s_, ALU.mult, [bid, "tab"], [VI])
                    P.tt('dve', V[:, 0, :n], V[:, 0, :n], V[:, 1, :n], ALU.add, [VI], [VI])
                    P.tt('dve', V[:, 2, :n], pb[:, :n], c_, ALU.mult, [bid, "tab"], [VI])
                    P.tt('dve', V[:, 3, :n], pa[:, :n], s_, ALU.mult, [aid, "tab"], [VI])
                    P.tt('dve', V[:, 2, :n], V[:, 2, :n], V[:, 3, :n], ALU.subtract, [VI], [VI])
                    GG, GI = G[w_], f"G{w_}"
                    P.op('dve', lambda g: g.tensor_tensor_scan(out=GG[:, 0, :n], data0=rB[:, j, :n], data1=V[:, 0, :n],
                                                               initial=st[:, pair, 0:1], op0=ALU.mult, op1=ALU.add),
                         reads=["rB", VI, "st"], writes=[GI])
                    P.op('dve', lambda g: g.tensor_tensor_scan(out=GG[:, 1, :n], data0=rB[:, j, :n], data1=V[:, 2, :n],
                                                               initial=st[:, pair, 1:2], op0=ALU.mult, op1=ALU.add),
                         reads=["rB", VI, "st"], writes=[GI])
                    wi = 0 if n == 256 else 1
                    cn, snn = En[:, j, wi, 0:1], En[:, j, wi, 1:2]
                    P.ts('dve', cur[:, 0:1], GG[:, 1, n - 1:n], snn, None, ALU.mult, None, [GI, "En"], ["cur"])
                    P.stt(st[:, pair, 0:1], GG[:, 0, n - 1:n], cn, cur[:, 0:1], ALU.mult, ALU.subtract, [GI, "En", "cur"], ["st"])
                    P.ts('dve', cur[:, 1:2], GG[:, 0, n - 1:n], snn, None, ALU.mult, None, [GI, "En"], ["cur"])
                    P.stt(st[:, pair, 1:2], GG[:, 1, n - 1:n], cn, cur[:, 1:2], ALU.mult, ALU.add, [GI, "En", "cur"], ["st"])
                    XX, XI = X[w_], f"X{w_}"
                    P.tt('pool', XX[:, 0, :n], GG[:, 0, :n], c_, ALU.mult, [GI, "tab"], [XI])
                    P.tt('pool', XX[:, 1, :n], GG[:, 1, :n], s_, ALU.mult, [GI, "tab"], [XI])
                    P.tt('pool', XX[:, 2, :n], GG[:, 1, :n], c_, ALU.mult, [GI, "tab"], [XI])
                    P.tt('pool', XX[:, 3, :n], GG[:, 0, :n], s_, ALU.mult, [GI, "tab"], [XI])
                    for q_, Wm in enumerate((Cre, nCre, nCim, nCim)):
                        P.mm(yp[:, :n], Wm[:, j, :], XX[:, q_, :n], pair == 0 and q_ == 0, pair == 3 and q_ == 3,
                             ["W", XI], [yid])
                    if pair == 3:
                        if d_ == 0:
                            P.stt(ys[sl][:, :n], ms[sl][:, :n], dv[:, 0:1], yp[:, :n], ALU.mult, ALU.add, [f"ms{sl}", "dv", yid], [f"ys{sl}"])
                        else:
                            P.cp('dve', ys[sl][:, :n], yp[:, n - 1::-1], [yid], [f"ys{sl}"])
                        nblk = n // 128
                        for bk in range(nblk):
                            pt, pid = trring.get()
                            P.tr(pt[:, 0:128], ys[sl][:, bk * 128:(bk + 1) * 128], ident[:], [f"ys{sl}", "ident"], [pid])
                            P.cp('act', yt[sl][:, bk, :], pt[:, 0:128], [pid], [f"yt{sl}"])
                        P.store(f"st_y{sl}", Yd[row0:row0 + n, fc * 128:(fc + 1) * 128].rearrange("(k p) f -> p k f", p=128),
                                yt[sl][:, 0:nblk, :], f"yt{sl}")


def emit_s5out(P, I, modsB, YF, YB, H1):
    nt = NTB
    with P.scope("so"):
        ident = P.sb("ident", [128, 128]); gL = P.sb("gL", [128, D]); gC = P.sb("gC", [128, D])
        wg = P.sb("wg", [128, 8, D], BF16); wo = P.sb("wo", [128, 8, D], BF16); wstage = P.sb("wstage", [128, 8, D]); lng = P.sb("lng", [128, D]); lnb = P.sb("lnb", [128, D])
        P.load("ld_c", ident[:], I["ident"], "ident")
        P.load("ld_c", gL[:], modsB[0, 0, :, 2048:3072], "gL"); P.load("ld_c", gC[:], modsB[0, 1, :, 2048:3072], "gC")
        P.load("ld_c", lng[:], I["lng"][0], "lng"); P.load("ld_c", lnb[:], I["lnb"][0], "lnb")
        P.load("ld_wg", wstage[:], I["wglu"].rearrange("(kc p) n -> p kc n", p=128), "wstage")
        P.cp('dve', wg[:], wstage[:], ["wstage"], ["wg"])
        P.load("ld_wg", wstage[:], I["wo_s5"].rearrange("(kc p) n -> p kc n", p=128), "wstage")
        P.cp('act', wo[:], wstage[:], ["wstage"], ["wo"])
        xs = [P.sb(f"x{i}", [128, D]) for i in range(2)]
        ya = [P.sb(f"ya{i}", [128, D]) for i in range(2)]
        yb_ = [P.sb(f"yb{i}", [128, D]) for i in range(2)]
        A = P.sb("A", [128, D]); B = P.sb("B", [128, D]); C = P.sb("C", [128, D]); T = P.sb("T", [128, 8, 128], BF16)
        O = [P.sb(f"O{i}", [128, D]) for i in range(2)]
        stats = P.sb("stats", [128, 2, 6]); mv = P.sb("mv", [128, 4])
        tring = PsumRing(P, range(0, 4)); lring = PsumRing(P, range(4, 8))
        for t in range(nt):
            s = t % 2
            rows = slice(t * 128, (t + 1) * 128)
            P.load(f"ld_x{s}", xs[s][:], I["x_tok"][rows, :], f"x{s}")
            P.load(f"ld_ya{s}", ya[s][:], YF[rows, :], f"ya{s}")
            P.load(f"ld_yb{s}", yb_[s][:], YB[rows, :], f"yb{s}")
            gate, gid = (gC, "gC") if t >= 64 else (gL, "gL")
            P.tt('dve', A[:], ya[s][:], yb_[s][:], ALU.add, [f"ya{s}", f"yb{s}"], ["A"])
            P.actf(B[:], A[:], AF.Gelu, ["A"], ["B"])
            transpose_tile(P, tring, B, "B", T, "T", 8, ident)
            linear(P, lring, T, "T", wg, "wg", 8, D,
                   lambda nb, c0, ps, pid, n: P.actf(C[:, c0:c0 + n], ps, AF.Sigmoid, [pid], ["C"]))
            P.tt('dve', C[:], C[:], B[:], ALU.mult, ["B", "C"], ["C"])
            transpose_tile(P, tring, C, "C", T, "T", 8, ident)
            linear(P, lring, T, "T", wo, "wo", 8, D,
                   lambda nb, c0, ps, pid, n: P.tt('dve', A[:, c0:c0 + n], ps, gate[:, c0:c0 + n], ALU.mult, [pid, gid], ["A"]))
            P.stt(A[:], xs[s][:], ALPHA, A[:], ALU.mult, ALU.add, [f"x{s}", "A"], ["A"])
            layernorm(P, A[:], "A", lng[:], lnb[:], O[s][:], f"O{s}", stats, mv)
            P.store(f"st_o{s}", H1[rows, :], O[s][:], f"O{s}")


def emit_peer(P, I, modsB, layer, Hin, Hout, nt, pfx, uv_d):
    with P.scope(pfx):
        ident = P.sb("ident", [128, 128]); mL = P.sb("mL", [128, 3, D])
        wq = P.sb("wq", [128, 8, D]); kbd = P.sb("kbd", [128, 8, 256]); lng = P.sb("lng", [128, D]); lnb = P.sb("lnb", [128, D])
        iota = P.sb("iota", [128, 16])
        P.load("ld_c", ident[:], I["ident"], "ident")
        P.load("ld_c", mL[:], modsB[layer, 0, :, 3072:6144].rearrange("p (m d) -> p m d", d=D), "mL")
        P.load("ld_c", kbd[:], I["kbd"][layer], "kbd")
        P.load("ld_c", lng[:], I["lng"][2 * layer + 1], "lng"); P.load("ld_c", lnb[:], I["lnb"][2 * layer + 1], "lnb")
        P.load("ld_c", iota[:], I["iota16"], "iota")
        P.load("ld_wq", wq[:], I["peer_wq"][layer].rearrange("(kc p) n -> p kc n", p=128), "wq")
        P.ts('dve', mL[:, 1, :], mL[:, 1, :], 1.0, None, ALU.add, None, ["mL"], ["mL"])
        hs = [P.sb("h0", [128, D])] * 2
        identb = P.sb("identb", [128, 128], BF16)
        Pk = [P.sb(f"Pk{i}", [128, D], BF16) for i in range(4)]
        M = P.sb("M", [128, D]); T = P.sb("T", [128, 8, 128]); Q = P.sb("Q", [128, D])
        sc = P.sb("sc", [128, 16, 128])
        sv = P.sb("sv", [128, 16, 16]); si = P.sb("si", [128, 16, 16], U32); sif = P.sb("sif", [128, 16, 16])

        cv = P.sb("cv", [128, 8, 16]); ci = P.sb("ci", [128, 8, 16], U32); cab = P.sb("cab", [128, 2, 8, 16], U32)
        cabf = P.sb("cabf", [128, 2, 8, 16]); OH = sc[:].rearrange("p a b -> p (a b)").rearrange("p (h c) -> p h c", c=256)
        i12 = P.sb("i12", [128, 2, 8, 16]); ef = P.sb("ef", [128, 128]); eidx = P.sb("eidx", [128, 128], U32)
        gt = P.sb("gt", [128, 8, 16]); gs = P.sb("gs", [128, 8]); dots = P.sb("dots", [128, 128]); actv = P.sb("actv", [128, 128])
        NS = 12
        uvg = [P.sb(f"uvg{i}", [128, 2 * D], BF16) for i in range(NS)]
        junk = Q
        acc = P.sb("acc", [128, D])
        sc2t = P.sb("sc2", [128, 16, 128]); candt = P.sb("cand", [128, 8, 256])
        sc2 = sc2t[:]
        cand2 = sc2t[:].rearrange("p a b -> p (a b)").rearrange("p (h c) -> p h c", c=256)
        cand = candt[:]
        O = [Q] * 2
        stats = P.sb("stats", [128, 2, 6]); mv = P.sb("mv", [128, 4])
        tring = PsumRing(P, range(0, 4)); lring = PsumRing(P, range(4, 6)); aring = PsumRing(P, range(6, 8))
        P.cp('dve', identb[:], ident[:], ["ident"], ["identb"])
        pa = [P.banks[6], P.banks[7]]
        sv4 = sv[:].rearrange("p (h s) k -> p h s k", s=2)
        sif4 = sif[:].rearrange("p (h s) k -> p h s k", s=2)
        B4 = [128, 8, 16, 16]
        for t in range(nt):
            s = t % 2
            rows = slice(t * 128, (t + 1) * 128)
            mod, mid = mL, "mL"
            if t == 64:
                P.load("ld_c", mL[:], modsB[layer, 1, :, 3072:6144].rearrange("p (m d) -> p m d", d=D), "mL")
                P.ts('dve', mL[:, 1, :], mL[:, 1, :], 1.0, None, ALU.add, None, ["mL"], ["mL"])
            P.load("ld_h0", hs[s][:], Hin[rows, :], "h0")
            P.tt('dve', M[:], hs[s][:], mod[:, 1, :], ALU.mult, ["h0", mid], ["M"])
            P.tt('dve', M[:], M[:], mod[:, 0, :], ALU.add, ["M", mid], ["M"])
            transpose_tile(P, tring, M, "M", T, "T", 8, ident)
            linear(P, lring, T, "T", wq, "wq", 8, D,
                   lambda nb, c0, ps, pid, n: P.cp('act', Q[:, c0:c0 + n], ps, [pid], ["Q"]))
            transpose_tile(P, tring, Q, "Q", T, "T", 8, ident)
            for hh in range(8):
                pt, pid = lring.get()
                P.mm(pt[:, 0:256], T[:, hh, :], kbd[:, hh, :], True, True, ["T", "kbd"], [pid])
                P.cp('act', sc[:, 2 * hh:2 * hh + 2, :], pt[:, 0:256].rearrange("p (s n) -> p s n", s=2), [pid], ["sc"])
            for blk in range(16):
                P.op('dve', lambda g: g.max(out=sv[:, blk, 0:8], in_=sc[:, blk, :]), reads=["sc"], writes=["sv"])
                P.op('dve', lambda g: g.max_index(out=si[:, blk, 0:8], in_max=sv[:, blk, 0:8], in_values=sc[:, blk, :]),
                     reads=["sc", "sv"], writes=["si"])
                P.op('dve', lambda g: g.match_replace(out=sc2[:, blk, :], in_to_replace=sv[:, blk, 0:8], in_values=sc[:, blk, :],
                                                      imm_value=NEG), reads=["sc", "sv"], writes=["sc2"])
                P.op('dve', lambda g: g.max(out=sv[:, blk, 8:16], in_=sc2[:, blk, :]), reads=["sc2"], writes=["sv"])
                P.op('dve', lambda g: g.max_index(out=si[:, blk, 8:16], in_max=sv[:, blk, 8:16], in_values=sc2[:, blk, :]),
                     reads=["sc2", "sv"], writes=["si"])
            P.cp('dve', sif[:], si[:], ["si"], ["sif"])
            P.tt('dve', cand[:].rearrange("p h (a b) -> p h a b", b=16), sv4[:, :, 0, :].unsqueeze(3).to_broadcast(B4),
                 sv4[:, :, 1, :].unsqueeze(2).to_broadcast(B4), ALU.add, ["sv"], ["cand"])
            for hh in range(8):
                P.op('dve', lambda g: g.max(out=cv[:, hh, 0:8], in_=cand[:, hh, :]), reads=["cand"], writes=["cv"])
                P.op('dve', lambda g: g.max_index(out=ci[:, hh, 0:8], in_max=cv[:, hh, 0:8], in_values=cand[:, hh, :]),
                     reads=["cand", "cv"], writes=["ci"])
                P.op('dve', lambda g: g.match_replace(out=cand2[:, hh, :], in_to_replace=cv[:, hh, 0:8], in_values=cand[:, hh, :],
                                                      imm_value=NEG), reads=["cand", "cv"], writes=["sc2"])
                P.op('dve', lambda g: g.max(out=cv[:, hh, 8:16], in_=cand2[:, hh, :]), reads=["sc2"], writes=["cv"])
                P.op('dve', lambda g: g.max_index(out=ci[:, hh, 8:16], in_max=cv[:, hh, 8:16], in_values=cand2[:, hh, :]),
                     reads=["sc2", "cv"], writes=["ci"])
            P.op('dve', lambda g: g.tensor_single_scalar(out=cab[:, 0], in_=ci[:], scalar=4, op=ALU.logical_shift_right),
                 reads=["ci"], writes=["cab"])
            P.op('dve', lambda g: g.tensor_single_scalar(out=cab[:, 1], in_=ci[:], scalar=15, op=ALU.bitwise_and),
                 reads=["ci"], writes=["cab"])
            P.cp('dve', cabf[:], cab[:], ["cab"], ["cabf"])
            OH4 = OH.rearrange("p h (a b) -> p h a b", b=16)
            for w_ in range(2):
                P.tt('dve', OH4, iota[:].unsqueeze(1).unsqueeze(1).to_broadcast(B4), cabf[:, w_].unsqueeze(3).to_broadcast(B4),
                     ALU.is_equal, ["iota", "cabf"], ["sc"])
                P.tt('dve', OH4, OH4, sif4[:, :, w_, :].unsqueeze(2).to_broadcast(B4), ALU.mult, ["sc", "sif"], ["sc"])
                P.op('dve', lambda g: g.tensor_reduce(out=i12[:, w_], in_=OH4, axis=AX.X, op=ALU.add), reads=["sc"], writes=["i12"])
            P.stt(ef[:].rearrange("p (h k) -> p h k", k=16), i12[:, 0], 128.0, i12[:, 1], ALU.mult, ALU.add, ["i12"], ["ef"])
            P.cp('dve', eidx[:], ef[:], ["ef"], ["eidx"])
            P.tt('dve', gt[:], cv[:], cv[:, :, 0:1].to_broadcast([128, 8, 16]), ALU.subtract, ["cv"], ["gt"])
            P.actf(gt[:], gt[:], AF.Exp, ["gt"], ["gt"])
            P.op('dve', lambda g: g.tensor_reduce(out=gs[:], in_=gt[:], axis=AX.X, op=ALU.add), reads=["gt"], writes=["gs"])
            P.op('dve', lambda g: g.reciprocal(out=gs[:], in_=gs[:]), reads=["gs"], writes=["gs"])
            P.tt('dve', gt[:], gt[:], gs[:].unsqueeze(2).to_broadcast([128, 8, 16]), ALU.mult, ["gt", "gs"], ["gt"])
            GS = 2
            NG = 128 // GS
            gtf = gt[:].rearrange("p h k -> p (h k)")
            for g in range(NG + 2):
                if g < NG:
                    for i in range(GS):
                        k = g * GS + i
                        sl = (g % 6) * GS + i
                        P.dma('pool', f"guv{sl}", lambda e: e.indirect_dma_start(
                            out=uvg[sl][:], out_offset=None, in_=uv_d[:, :],
                            in_offset=bass.IndirectOffsetOnAxis(ap=eidx[:, k:k + 1], axis=0)), reads=["eidx"], writes=[f"uvg{sl}"])
                        P.op('dve', lambda g_: g_.scalar_tensor_tensor(out=junk[:], in0=uvg[sl][:, 0:D], scalar=1.0, in1=M[:],
                                                                       op0=ALU.mult, op1=ALU.mult, accum_out=dots[:, k:k + 1]),
                             reads=[f"uvg{sl}", "M"], writes=[f"dots{g % 4}"])
                elif g == NG:
                    P.op('dve', lambda g_: g_.memset(gs[:, 0:1], 0.0), writes=["dfence"])
                if 1 <= g <= NG:
                    gq = g - 1
                    ks = slice(gq * GS, (gq + 1) * GS)
                    fence = [f"dots{g % 4}"] if g < NG else ["dfence"]
                    P.actf(actv[:, ks], dots[:, ks], AF.Gelu, [f"dots{gq % 4}"] + fence, [f"actv{gq % 4}"])
                if g >= 2:
                    gp = g - 2
                    ks = slice(gp * GS, (gp + 1) * GS)
                    P.tt('dve', actv[:, ks], actv[:, ks], gtf[:, ks], ALU.mult, [f"actv{gp % 4}", "gt"], [f"actv{gp % 4}"])
                    for i in range(GS):
                        k = gp * GS + i
                        sl = (gp % 6) * GS + i
                        pk, pkid = Pk[k % 4], f"Pk{k % 4}"
                        P.actf(pk[:], uvg[sl][:, D:2 * D], AF.Identity, [f"uvg{sl}", f"actv{gp % 4}"], [pkid], scale=actv[:, k:k + 1])
                        for hf in range(2):
                            P.mm(pa[hf][:, 0:512], identb[:], pk[:, hf * 512:(hf + 1) * 512], k == 0, k == 127,
                                 [pkid, "identb"], [f"psb{6 + hf}"])
            for hf in range(2):
                P.tt('dve', acc[:, hf * 512:(hf + 1) * 512], pa[hf][:, 0:512], mod[:, 2, hf * 512:(hf + 1) * 512], ALU.mult,
                     [f"psb{6 + hf}", mid], ["acc"])
            P.stt(acc[:], hs[s][:], ALPHA, acc[:], ALU.mult, ALU.add, ["h0", "acc"], ["acc"])
            layernorm(P, acc[:], "acc", lng[:], lnb[:], O[s][:], "Q", stats, mv)
            P.store("st_o0", Hout[rows, :], O[s][:], "Q")


def emit_cvt(P, src, dst, pfx):
    with P.scope(pfx):
        NSL = 3
        fin = [P.sb(f"fin{i}", [128, 2 * D]) for i in range(NSL)]
        fout = [P.sb(f"fout{i}", [128, 2 * D], BF16) for i in range(NSL)]
        engs = ('dve', 'act', 'pool')
        for r in range(128):
            sl = r % NSL
            rows = slice(r * 128, (r + 1) * 128)
            P.load(f"ld_f{sl}", fin[sl][:], src[rows, :], f"fin{sl}")
            P.cp(engs[r % 3], fout[sl][:], fin[sl][:], [f"fin{sl}"], [f"fout{sl}"])
            P.store(f"st_f{sl}", dst[rows, :], fout[sl][:], f"fout{sl}")


def emit_mlaproj(P, I, modsB, H2, kT_s, krT_s, v_s, qT_s, qrT_s):
    with P.scope("mp"):
        ident = P.sb("ident", [128, 128]); mL = P.sb("mL", [128, 2, D]); mC = P.sb("mC", [128, 2, D])
        wdq = P.sb("wdq", [128, 8, 384], BF16); qn = P.sb("qn", [128, 384]); wuq = P.sb("wuq", [128, 3, 1536], BF16)
        wdkv = P.sb("wdkv", [128, 8, 320], BF16); kvn = P.sb("kvn", [128, 256]); wukv = P.sb("wukv", [128, 2, 2048], BF16)
        wstage = P.sb("wstage", [128, 4608])
        own = P.sb("own", [128, 16], U32)
        P.load("ld_c", ident[:], I["ident"], "ident"); P.load("ld_c", own[:], I["ownidx"], "own")
        P.load("ld_c", mL[:], modsB[1, 0, :, 0:2048].rearrange("p (m d) -> p m d", d=D), "mL")
        P.load("ld_c", mC[:], modsB[1, 1, :, 0:2048].rearrange("p (m d) -> p m d", d=D), "mC")
        P.load("ld_c", qn[:], I["qn"], "qn"); P.load("ld_c", kvn[:], I["kvn"], "kvn")
        for wi_, (dst, key, nm, kcs, ncol) in enumerate(((wdq, "wdq", "wdq", 8, 384), (wuq, "wuq", "wuq", 3, 1536),
                                                         (wdkv, "wdkv", "wdkv", 8, 320), (wukv, "wukv", "wukv", 2, 2048))):
            stg = wstage[:, 0:kcs * ncol].rearrange("p (kc n) -> p kc n", n=ncol)
            P.load("ld_w", stg, I[key].rearrange("(kc p) n -> p kc n", p=128), "wstage")
            P.cp('dve' if wi_ % 2 == 0 else 'act', dst[:], stg, ["wstage"], [nm])
        P.ts('dve', mL[:, 1, :], mL[:, 1, :], 1.0, None, ALU.add, None, ["mL"], ["mL"])
        P.ts('dve', mC[:, 1, :], mC[:, 1, :], 1.0, None, ALU.add, None, ["mC"], ["mC"])
        hs = [P.sb(f"h{i}", [128, D]) for i in range(2)]
        cs = [P.sb(f"cs{i}", [128, 2, 32]) for i in range(2)]
        M = P.sb("M", [128, D]); T = P.sb("T", [128, 8, 128], BF16); T2 = P.sb("T2", [128, 3, 128], BF16)
        cq = P.sb("cq", [128, 384]); cqn = P.sb("cqn", [128, 384]); junk = P.sb("junk", [128, 384]); ss = P.sb("ss", [128, 4])
        Qf = P.sb("Qf", [128, 1536]); QR = P.sb("QR", [128, 8, 64]); kv = P.sb("kv", [128, 320]); ckn = P.sb("ckn", [128, 256])
        KVf = P.sb("KVf", [128, 2048]); KRf = P.sb("KRf", [128, 64])
        rt = P.sb("rt", [128, 4, 8, 32])
        KT = [P.sb(f"KT{i}", [128, 8, 128], BF16) for i in range(2)]
        KRT = [P.sb(f"KRT{i}", [64, 128], BF16) for i in range(2)]
        V9 = [P.sb(f"V9{i}", [128, 8, 129], BF16) for i in range(2)]
        QT = [P.sb(f"QT{i}", [128, 8, 128], BF16) for i in range(2)]
        QRT = [P.sb(f"QRT{i}", [64, 8, 128], BF16) for i in range(2)]
        for i in range(2):
            P.op('dve', lambda g: g.memset(V9[i][:], 1.0), writes=[f"V9{i}"])
        tring = PsumRing(P, range(0, 4)); lring = PsumRing(P, range(4, 8))
        KVf4 = KVf[:].rearrange("p (h d) -> p h d", d=256)
        for t in range(NTB):
            s = t % 2
            rows = slice(t * 128, (t + 1) * 128)
            cols = slice(t * 128, (t + 1) * 128)
            mod, mid = (mC, "mC") if t >= 64 else (mL, "mL")
            P.load(f"ld_h{s}", hs[s][:], H2[rows, :], f"h{s}")
            P.load(f"ld_cs{s}", cs[s][:, 0, :], I["cos"][rows, :], f"cs{s}")
            P.load(f"ld_cs{s}", cs[s][:, 1, :], I["sin"][rows, :], f"cs{s}")
            P.tt('dve', M[:], hs[s][:], mod[:, 1, :], ALU.mult, [f"h{s}", mid], ["M"])
            P.tt('dve', M[:], M[:], mod[:, 0, :], ALU.add, ["M", mid], ["M"])
            transpose_tile(P, tring, M, "M", T, "T", 8, ident)
            linear(P, lring, T, "T", wdkv, "wdkv", 8, 320, lambda nb, c0, ps, pid, n: P.cp('act', kv[:, c0:c0 + n], ps, [pid], ["kv"]))
            rmsnorm(P, kv[:, 0:256], "kv", 256, kvn[:], "kvn", ckn[:], "ckn", junk[:, 0:256], ss)
            rope(P, kv[:, 256:288], kv[:, 288:320], cs[s][:, 0, :], cs[s][:, 1, :], KRf[:, 0:32], KRf[:, 32:64],
                 [rt[:, i, 0, :] for i in range(4)], ["kv", f"cs{s}"], ["KRf"])
            pt, pid = tring.get()
            P.tr(pt[0:64, 0:128], KRf[:, 0:64], ident[:], ["KRf", "ident"], [pid])
            P.cp('act', KRT[s][:], pt[0:64, 0:128], [pid], [f"KRT{s}"])
            P.store(f"st_kr{s}", krT_s[:, cols], KRT[s][:], f"KRT{s}")
            transpose_tile(P, tring, ckn, "ckn", T2, "T2", 2, ident)
            linear(P, lring, T2, "T2", wukv, "wukv", 2, 2048,
                   lambda nb, c0, ps, pid, n: P.cp('act' if nb % 2 else 'dve', KVf[:, c0:c0 + n], ps, [pid], ["KVf"]))
            for hh in range(8):
                pt, pid = tring.get()
                P.tr(pt[:, 0:128], KVf[:, hh * 256:hh * 256 + 128], ident[:], ["KVf", "ident"], [pid])
                P.cp('act' if hh % 2 else 'dve', KT[s][:, hh, :], pt[:, 0:128], [pid], [f"KT{s}"])
            P.store(f"st_kt{s}", kT_s[:, :, cols].rearrange("h d t -> d h t"), KT[s][:], f"KT{s}")
            P.cp('dve', V9[s][:, :, 0:128], KVf4[:, :, 128:256], ["KVf"], [f"V9{s}"])
            P.store(f"st_v{s}", v_s[:, :, t * 129:(t + 1) * 129].rearrange("h p c -> p h c"), V9[s][:], f"V9{s}")
        for i in range(16):
            s = i % 2
            rows = slice(i * 128, (i + 1) * 128)
            P.dma('pool', f"gh{s}", lambda e: e.indirect_dma_start(
                out=hs[s][:], out_offset=None, in_=H2[:, :],
                in_offset=bass.IndirectOffsetOnAxis(ap=own[:, i:i + 1], axis=0)), reads=["own", "H2"], writes=[f"h{s}"])
            P.load(f"ld_cs{s}", cs[s][:, 0, :], I["cos_own"][rows, :], f"cs{s}")
            P.load(f"ld_cs{s}", cs[s][:, 1, :], I["sin_own"][rows, :], f"cs{s}")
            P.tt('dve', M[:], hs[s][:], mL[:, 1, :], ALU.mult, [f"h{s}", "mL"], ["M"])
            P.tt('dve', M[:], M[:], mL[:, 0, :], ALU.add, ["M", "mL"], ["M"])
            transpose_tile(P, tring, M, "M", T, "T", 8, ident)
            linear(P, lring, T, "T", wdq, "wdq", 8, 384, lambda nb, c0, ps, pid, n: P.cp('act', cq[:, c0:c0 + n], ps, [pid], ["cq"]))
            rmsnorm(P, cq[:], "cq", 384, qn[:], "qn", cqn[:], "cqn", junk[:], ss)
            transpose_tile(P, tring, cqn, "cqn", T2, "T2", 3, ident)
            linear(P, lring, T2, "T2", wuq, "wuq", 3, 1536,
                   lambda nb, c0, ps, pid, n: P.cp('act' if nb % 2 else 'dve', Qf[:, c0:c0 + n], ps, [pid], ["Qf"]))
            Qf4 = Qf[:].rearrange("p (h d) -> p h d", d=192)
            cosb = cs[s][:, 0, :].unsqueeze(1).to_broadcast([128, 8, 32])
            sinb = cs[s][:, 1, :].unsqueeze(1).to_broadcast([128, 8, 32])
            rope(P, Qf4[:, :, 128:160], Qf4[:, :, 160:192], cosb, sinb, QR[:, :, 0:32], QR[:, :, 32:64],
                 [rt[:, j] for j in range(4)], ["Qf", f"cs{s}"], ["QR"])
            for hh in range(8):
                pt, pid = tring.get()
                P.tr(pt[:, 0:128], Qf[:, hh * 192:hh * 192 + 128], ident[:], ["Qf", "ident"], [pid])
                P.cp('act' if hh % 2 else 'dve', QT[s][:, hh, :], pt[:, 0:128], [pid], [f"QT{s}"])
                pt, pid = tring.get()
                P.tr(pt[0:64, 0:128], QR[:, hh, :], ident[:], ["QR", "ident"], [pid])
                P.cp('dve' if hh % 2 else 'act', QRT[s][:, hh, :], pt[0:64, 0:128], [pid], [f"QRT{s}"])
            P.store(f"st_qt{s}", qT_s[:, :, rows].rearrange("h d t -> d h t"), QT[s][:], f"QT{s}")
            P.store(f"st_qr{s}", qrT_s[:, :, rows].rearrange("h d t -> d h t"), QRT[s][:], f"QRT{s}")


def emit_attn(P, I, modsB, H2, kT_d, krT_d, v_d, qT_d, qrT_d, H3):
    with P.scope("at"):
        ident = P.sb("ident", [128, 128]); gate = P.sb("gate", [128, D]); wo = P.sb("wo", [128, 8, D])
        lng = P.sb("lng", [128, D]); lnb = P.sb("lnb", [128, D]); own = P.sb("own", [128, 16], U32)
        krTa = P.sb("krTa", [65, NK], BF16)
        P.load("ld_c", ident[:], I["ident"], "ident"); P.load("ld_c", own[:], I["ownidx"], "own")
        P.load("ld_c", gate[:], modsB[1, 0, :, 2048:3072], "gate")
        P.load("ld_c", lng[:], I["lng"][2], "lng"); P.load("ld_c", lnb[:], I["lnb"][2], "lnb")
        P.load("ld_wo", wo[:], I["wo_mla"].rearrange("(kc p) n -> p kc n", p=128), "wo")
        P.load("ld_kr", krTa[0:64, :], krT_d, "krTa")
        P.op('dve', lambda g: g.memset(krTa[64:65, :], 1.0), writes=["krTa"])
        kT = [P.sb(f"kT{i}", [128, NK], BF16) for i in range(2)]
        vh = [P.sb(f"vh{i}", [128, NKT * 129], BF16) for i in range(2)]
        qT = [P.sb(f"qT{i}", [128, 512], BF16) for i in range(2)]
        qrTa = [P.sb(f"qrTa{i}", [65, 512], BF16) for i in range(2)]
        NPT = 3
        PT = [P.sb(f"PT{i}", [128, 512], BF16) for i in range(NPT)]
        mx = P.sb("mx", [128, 20]); nmp = P.sb("nmp", [128, 65]); rden = P.sb("rden", [128, 4])
        otok = P.sb("otok", [128, 4, D])
        hs = [P.sb("hs0", [128, D])] * 2
        A = P.sb("A", [128, D]); T = P.sb("T", [128, 8, 128])
        O = [P.sb("O0", [128, D])] * 2
        stats = P.sb("stats", [128, 2, 6]); mv = P.sb("mv", [128, 4])
        sring = PsumRing(P, range(0, 3)); mring = PsumRing(P, range(3, 4)); oring = PsumRing(P, range(4, 8))
        P.op('dve', lambda g: g.memset(nmp[:], 0.0), writes=["nmp"])
        kblocks = [(512 * i, 512) for i in range(16)] + [(8192, 256)]
        it = 0
        ipt = 0
        tcount = 0
        for qb in range(4):
            for hh in range(8):
                s = it % 2
                it += 1
                P.load(f"ld_k{s}", kT[s][:], kT_d[hh], f"kT{s}")
                P.load(f"ld_v{s}", vh[s][:], v_d[hh], f"vh{s}")
                P.load(f"ld_q{s}", qT[s][:], qT_d[hh, :, qb * 512:(qb + 1) * 512], f"qT{s}")
                P.load(f"ld_qr{s}", qrTa[s][0:64, :], qrT_d[hh, :, qb * 512:(qb + 1) * 512], f"qrTa{s}")
                for qs in range(4):
                    qc = slice(qs * 128, (qs + 1) * 128)
                    for kb, (k0, n) in enumerate(kblocks):
                        pt, pid = sring.get()
                        P.mm(pt[:, 0:n], qT[s][:, qc], kT[s][:, k0:k0 + n], True, False, [f"qT{s}", f"kT{s}"], [pid])
                        P.mm(pt[:, 0:n], qrTa[s][0:64, qc], krTa[0:64, k0:k0 + n], False, True, [f"qrTa{s}", "krTa"], [pid])
                        P.op('dve', lambda g: g.tensor_reduce(out=mx[:, kb:kb + 1], in_=pt[:, 0:n], axis=AX.X, op=ALU.max),
                             reads=[pid], writes=["mx"])
                    P.op('dve', lambda g: g.tensor_reduce(out=mx[:, 17:18], in_=mx[:, 0:17], axis=AX.X, op=ALU.max),
                         reads=["mx"], writes=["mx"])
                    P.ts('dve', nmp[:, 64:65], mx[:, 17:18], -1.0, None, ALU.mult, None, ["mx"], ["nmp"])
                    pt, pid = mring.get()
                    P.mm(pt[0:65, 0:128], nmp[:, :], ident[:], True, True, ["nmp", "ident"], [pid])
                    P.cp('act', qrTa[s][64:65, qc], pt[64:65, 0:128], [pid], [f"qrTa{s}"])
                ops = [oring.get() for _ in range(4)]
                for kt in range(NKT):
                    kc = slice(kt * 128, (kt + 1) * 128)
                    pt, pid = sring.get()
                    P.mm(pt[:, 0:512], kT[s][:, kc], qT[s][:, :], True, False, [f"kT{s}", f"qT{s}"], [pid])
                    P.mm(pt[:, 0:512], krTa[0:65, kc], qrTa[s][0:65, :], False, True, ["krTa", f"qrTa{s}"], [pid])
                    pi = ipt % NPT
                    ipt += 1
                    P.actf(PT[pi][:], pt[:, 0:512], AF.Exp, [pid], [f"PT{pi}"], scale=ATT_SCALE)
                    for qs in range(4):
                        ot, oid = ops[qs]
                        P.mm(ot[:, 0:129], PT[pi][:, qs * 128:(qs + 1) * 128], vh[s][:, kt * 129:(kt + 1) * 129],
                             kt == 0, kt == NKT - 1, [f"PT{pi}", f"vh{s}"], [oid])
                for qs in range(4):
                    ot, oid = ops[qs]
                    P.op('dve', lambda g: g.reciprocal(out=rden[:, qs:qs + 1], in_=ot[:, 128:129]), reads=[oid], writes=["rden"])
                    P.ts('dve', otok[:, qs, hh * 128:(hh + 1) * 128], ot[:, 0:128], rden[:, qs:qs + 1], None, ALU.mult, None,
                         [oid, "rden"], ["otok"])
            for qs in range(4):
                s2 = tcount % 2
                ti = qb * 4 + qs
                tcount += 1
                rows = slice(ti * 128, (ti + 1) * 128)
                P.dma('pool', "gh0", lambda e: e.indirect_dma_start(
                    out=hs[s2][:], out_offset=None, in_=H2[:, :],
                    in_offset=bass.IndirectOffsetOnAxis(ap=own[:, ti:ti + 1], axis=0)), reads=["own"], writes=["hs0"])
                for kc in range(8):
                    pt, pid = sring.get()
                    P.tr(pt[:, 0:128], otok[:, qs, kc * 128:(kc + 1) * 128], ident[:], ["otok", "ident"], [pid])
                    P.cp('act' if kc % 2 == 0 else 'dve', T[:, kc, :], pt[:, 0:128], [pid], ["T"])
                linear(P, sring, T, "T", wo, "wo", 8, D,
                       lambda nb, c0, ps, pid, n: P.tt('dve', A[:, c0:c0 + n], ps, gate[:, c0:c0 + n], ALU.mult, [pid, "gate"], ["A"]))
                P.stt(A[:], hs[s2][:], ALPHA, A[:], ALU.mult, ALU.add, ["hs0", "A"], ["A"])
                layernorm(P, A[:], "A", lng[:], lnb[:], O[s2][:], "O0", stats, mv)
                P.store("st_o0", H3[rows, :], O[s2][:], "O0")


IN_SPECS = [
    ("cT", [128, 8, 2], F32), ("aw", [2, 1024, 6144], F32), ("abB", [128, 2, 6144], F32), ("abT", [128, 16], F32),
    ("xT", [8, 128, SEQ_T], F32), ("x_tok", [NROW, D], F32),
    ("a_re", [8, 128, 8], F32), ("a_im", [8, 128, 8], F32), ("ldt", [8, 128, 8], F32),
    ("bre", [8, 128, 8, 16], F32), ("bim", [8, 128, 8, 16], F32), ("cre", [8, 128, 8, 16], F32), ("cim", [8, 128, 8, 16], F32),
    ("dvec", [8, 128, 1], F32), ("ident", [128, 128], F32), ("iota16", [128, 16], F32),
    ("wglu", [D, D], F32), ("wo_s5", [D, D], F32), ("lng", [4, 128, D], F32), ("lnb", [4, 128, D], F32),
    ("peer_wq", [2, D, D], F32), ("kbd", [2, 128, 8, 256], F32), ("peer_uv0", [16384, 2 * D], F32), ("peer_uv1", [16384, 2 * D], F32),
    ("wdq", [D, 384], F32), ("qn", [128, 384], F32), ("wuq", [384, 1536], F32), ("wdkv", [D, 320], F32),
    ("kvn", [128, 256], F32), ("wukv", [256, 2048], F32), ("wo_mla", [D, D], F32),
    ("cos", [NROW, 32], F32), ("sin", [NROW, 32], F32), ("cos_own", [2048, 32], F32), ("sin_own", [2048, 32], F32),
    ("ownidx", [128, 16], U32),
]


def build_fused():
    P = Prog()
    I = {name: P.din(name, shape, dt) for name, shape, dt in IN_SPECS}
    out = P.dout("out", [2048, D])
    modsB = P.scratch("modsB", [2, 2, 128, 6144])
    YF = P.scratch("YF", [NROW, D]); YB = P.scratch("YB", [NROW, D])
    H1 = P.scratch("H1", [NROW, D]); H2 = P.scratch("H2", [NROW, D]); H3 = P.scratch("H3", [2048, D])
    kT_s = P.scratch("kT_s", [8, 128, NK], BF16); krT_s = P.scratch("krT_s", [64, NK], BF16)
    v_s = P.scratch("v_s", [8, 128, NKT * 129], BF16)
    qT_s = P.scratch("qT_s", [8, 128, 2048], BF16); qrT_s = P.scratch("qrT_s", [8, 64, 2048], BF16)
    uvb = [P.scratch(f"uvb{l}", [16384, 2 * D], BF16) for l in range(2)]
    for l in range(2):
        emit_cvt(P, I[f"peer_uv{l}"], uvb[l], f"cv{l}")
    emit_mods(P, I["cT"], I["aw"], I["abB"], modsB)
    emit_s5(P, I, YF, YB)
    emit_s5out(P, I, modsB, YF, YB, H1)
    emit_peer(P, I, modsB, 0, H1, H2, NTB, "p0", uvb[0])
    emit_mlaproj(P, I, modsB, H2, kT_s, krT_s, v_s, qT_s, qrT_s)
    emit_attn(P, I, modsB, H2, kT_s, krT_s, v_s, qT_s, qrT_s, H3)
    emit_peer(P, I, modsB, 1, H3, out, 16, "p1", uvb[1])
    P.finish()
    return P


def bc(v):
    return np.ascontiguousarray(np.broadcast_to(v[None, :], (128, v.shape[0])).astype(np.float32))


def rope_tables():
    n_freq = 16
    inv = (10000.0 ** (-np.arange(n_freq, dtype=np.float32) / n_freq)).astype(np.float32)
    r, col = np.meshgrid(np.arange(128, dtype=np.float32), np.arange(64, dtype=np.float32), indexing='ij')
    ang = np.concatenate([r.reshape(-1, 1) * inv, col.reshape(-1, 1) * inv], axis=-1).astype(np.float32)
    return np.cos(ang).astype(np.float32), np.sin(ang).astype(np.float32)


def kernel(x, c, ctx, c_ctx, ada_w, ada_b, ln_g, ln_b,
           s5_a_re, s5_a_im, s5_log_dt, s5_b_re, s5_b_im, s5_c_re, s5_c_im, s5_d, s5_w_glu, s5_w_o,
           mla_w_dq, mla_q_norm, mla_w_uq, mla_w_dkv, mla_kv_norm, mla_w_ukv, mla_w_o,
           peer_w_q, peer_keys, peer_u, peer_v):
    f = lambda a: np.ascontiguousarray(np.asarray(a, dtype=np.float32))
    x, c, ctx, c_ctx, ada_w, ada_b, ln_g, ln_b = map(f, (x, c, ctx, c_ctx, ada_w, ada_b, ln_g, ln_b))
    P = build_fused()
    def st(a):
        return np.ascontiguousarray(f(a).reshape(2, 8, 4, 2, 64).transpose(1, 3, 4, 0, 2).reshape(8, 128, 8))

    def bl(a):
        return np.ascontiguousarray(f(a).reshape(2, 8, 4, 2, 64, 16).transpose(1, 3, 4, 0, 2, 5).reshape(8, 128, 8, 16))
    kbd = np.zeros((2, 128, 8, 256), dtype=np.float32)
    pk = f(peer_keys)
    for l in range(2):
        for s_ in range(2):
            kbd[l, 64 * s_:64 * s_ + 64, :, 128 * s_:128 * s_ + 128] = pk[l, :, s_].transpose(2, 0, 1)
    cosl, sinl = rope_tables()
    cos_all = np.ones((NROW, 32), np.float32); sin_all = np.zeros((NROW, 32), np.float32)
    cos_all[:8192] = cosl; sin_all[:8192] = sinl
    common = {
        "aw": ada_w, "abB": np.ascontiguousarray(np.broadcast_to(ada_b[None], (128, 2, 6144))),
        "abT": np.ascontiguousarray(ada_b[0, :2048].reshape(2, 8, 128).transpose(2, 0, 1).reshape(128, 16)),
        "a_re": st(s5_a_re[0]), "a_im": st(s5_a_im[0]),
        "ldt": st(np.broadcast_to(f(s5_log_dt)[0][:, :, None], (2, 64, 64))),
        "bre": bl(s5_b_re[0]), "bim": bl(s5_b_im[0]),
        "cre": bl(f(s5_c_re)[0].transpose(0, 1, 3, 2)), "cim": bl(f(s5_c_im)[0].transpose(0, 1, 3, 2)),
        "dvec": np.ascontiguousarray(f(s5_d)[0].reshape(8, 128, 1)),
        "ident": np.eye(128, dtype=np.float32), "iota16": bc(np.arange(16, dtype=np.float32)),
        "wglu": f(s5_w_glu)[0], "wo_s5": f(s5_w_o)[0],
        "lng": np.stack([bc(ln_g[0, 0]), bc(ln_g[0, 1]), bc(ln_g[1, 0]), bc(ln_g[1, 1])]),
        "lnb": np.stack([bc(ln_b[0, 0]), bc(ln_b[0, 1]), bc(ln_b[1, 0]), bc(ln_b[1, 1])]),
        "peer_wq": f(peer_w_q), "kbd": kbd, "peer_uv0": np.ascontiguousarray(np.concatenate([f(peer_u)[0], f(peer_v)[0]], axis=1)),
        "peer_uv1": np.ascontiguousarray(np.concatenate([f(peer_u)[1], f(peer_v)[1]], axis=1)),
        "wdq": f(mla_w_dq)[0], "qn": bc(f(mla_q_norm)[0]), "wuq": f(mla_w_uq)[0], "wdkv": f(mla_w_dkv)[0],
        "kvn": bc(f(mla_kv_norm)[0]), "wukv": f(mla_w_ukv)[0], "wo_mla": f(mla_w_o)[0],
        "cos": cos_all, "sin": sin_all,
    }
    per_b = []
    for b in range(2):
        seq = np.concatenate([ctx[b], x[b]], axis=0)
        per_b.append({
            "cT": np.ascontiguousarray(np.stack([c[b], c_ctx], axis=1).reshape(8, 128, 2).transpose(1, 0, 2)),
            "xT": np.ascontiguousarray(seq.T.reshape(8, 128, SEQ_T)),
            "x_tok": np.ascontiguousarray(np.concatenate([x[b], ctx[b]], axis=0)),
        })
    maps = []
    for k in range(NCORES):
        b, j = k // 4, k % 4
        m = dict(common)
        m.update(per_b[b])
        m["cos_own"] = np.ascontiguousarray(cosl[2048 * j:2048 * (j + 1)])
        m["sin_own"] = np.ascontiguousarray(sinl[2048 * j:2048 * (j + 1)])
        m["ownidx"] = np.ascontiguousarray((2048 * j + np.arange(2048, dtype=np.uint32)).reshape(16, 128).T)
        maps.append(m)
    res = P.run(maps)
    out = np.zeros((2, 8192, D), np.float32)
    for k in range(NCORES):
        b, j = k // 4, k % 4
        out[b, 2048 * j:2048 * (j + 1)] = res[k]["out"]
    return out
```
